# Optimizing a Trainium2 kernel written in Bass

```python
import math
import jax, jax.numpy as jnp
from jax import lax
import numpy as np

D_MODEL = 1024
BATCH = 8
SEQ = 4096
DEPTH = 1

RMS_EPS = 1e-6
ROPE_THETA = 500000.0
ROPE_FRACTION = 4
DA_HEADS = 4
DA_HEAD_DIM = 64
DA_V_DIM = 2 * DA_HEAD_DIM
DA_WIDTH = DA_HEADS * DA_V_DIM
DA_QBLOCK = 128
NSA_HEADS = 8
NSA_GROUPS = 2
NSA_HEAD_DIM = 64
NSA_WIDTH = NSA_HEADS * NSA_HEAD_DIM
CMP_BLOCK = 32
CMP_STRIDE = 16
SEL_BLOCK = 64
SEL_TOPK = 16
WINDOW = 512
NSA_QBLOCK = 64
FORCE_BONUS = 1e4
NEG_LARGE = -1e30
PEER_HEADS = 8
PEER_NKEYS = 128
PEER_TOPK = 16
PEER_DQ = 256
N_EXPERTS = PEER_NKEYS * PEER_NKEYS
PEER_TBLOCK = 128
IN_SPLIT_SIZES = (DA_HEADS * 2 * DA_HEAD_DIM, DA_HEADS * 2 * DA_HEAD_DIM, DA_WIDTH, NSA_WIDTH) + (NSA_GROUPS * NSA_HEAD_DIM,) * 6 + (NSA_HEADS * 3, 2 * D_MODEL)
IN_COLS = sum(IN_SPLIT_SIZES)

kernel_name = 'hybrid_diffattn_nsa_peer_block'


def rmsnorm(x, g):
    xf = x.astype(jnp.float32)
    y = xf * lax.rsqrt(jnp.mean(xf * xf, axis=-1, keepdims=True) + RMS_EPS)
    return (y * g.astype(jnp.float32)).astype(x.dtype)


def partial_rope(x, pos):
    d = x.shape[-1]
    rot = d // ROPE_FRACTION
    half = rot // 2
    inv = jnp.power(ROPE_THETA, -jnp.arange(half, dtype=jnp.float32) * 2.0 / rot)
    ang = pos.astype(jnp.float32)[:, None] * inv[None, :]
    cos = jnp.cos(ang)[None, :, None, :].astype(x.dtype)
    sin = jnp.sin(ang)[None, :, None, :].astype(x.dtype)
    x1 = x[..., :half]
    x2 = x[..., half:rot]
    return jnp.concatenate([x1 * cos - x2 * sin, x2 * cos + x1 * sin, x[..., rot:]], axis=-1)


def diff_attention(q, k, v, lq1, lk1, lq2, lk2, subln, lambda_init):
    B, S, H, _, d = q.shape
    scale = d ** -0.5
    lam = (jnp.exp(jnp.sum(lq1.astype(jnp.float32) * lk1.astype(jnp.float32)))
           - jnp.exp(jnp.sum(lq2.astype(jnp.float32) * lk2.astype(jnp.float32))) + lambda_init)
    kh = k.transpose(0, 2, 3, 1, 4)
    vh = v.transpose(0, 2, 1, 3)
    n_blk = S // DA_QBLOCK
    qb = q.transpose(0, 2, 3, 1, 4).reshape(B, H, 2, n_blk, DA_QBLOCK, d).transpose(3, 0, 1, 2, 4, 5)
    kpos = jnp.arange(S)

    def one_block(args):
        qblk, i = args
        qpos = i * DA_QBLOCK + jnp.arange(DA_QBLOCK)
        s = jnp.einsum('bhcqd,bhckd->bhcqk', qblk, kh).astype(jnp.float32) * scale
        mask = kpos[None, :] <= qpos[:, None]
        p = jax.nn.softmax(jnp.where(mask, s, -jnp.inf), axis=-1)
        attn = p[:, :, 0] - lam * p[:, :, 1]
        return jnp.einsum('bhqk,bhkd->bhqd', attn.astype(vh.dtype), vh)

    o = lax.map(one_block, (qb, jnp.arange(n_blk)))
    o = o.transpose(1, 0, 3, 2, 4).reshape(B, S, H, 2 * d)
    o = rmsnorm(o, subln) * (1.0 - lambda_init)
    return o.reshape(B, S, H * 2 * d)


def compress(kv, pe, w1, w2):
    B, S, G, d = kv.shape
    n_cmp = (S - CMP_BLOCK) // CMP_STRIDE + 1
    idx = jnp.arange(n_cmp)[:, None] * CMP_STRIDE + jnp.arange(CMP_BLOCK)[None, :]
    blocks = kv[:, idx] + pe[None, None, :, None, :]
    flat = blocks.transpose(0, 3, 1, 2, 4).reshape(B, G, n_cmp, CMP_BLOCK * d)
    return jax.nn.gelu(flat @ w1, approximate=False) @ w2


def nsa_attention(q, kc_raw, vc_raw, ks, vs, kw, vw, gates, pe_k, pe_v, w1k, w1v, w2k, w2v):
    B, S, Hq, d = q.shape
    G = NSA_GROUPS
    Hg = Hq // G
    QB = NSA_QBLOCK
    scale = d ** -0.5
    kc = compress(kc_raw, pe_k, w1k, w2k)
    vc = compress(vc_raw, pe_v, w1v, w2v)
    n_cmp = kc.shape[2]
    cmp_end = jnp.arange(n_cmp) * CMP_STRIDE + CMP_BLOCK - 1
    n_sel = S // SEL_BLOCK
    k_sel = min(SEL_TOPK, n_sel)
    ci = jnp.arange(n_cmp)[:, None] * CMP_STRIDE
    sj = jnp.arange(n_sel)[None, :] * SEL_BLOCK
    overlap = ((ci < sj + SEL_BLOCK) & (ci + CMP_BLOCK > sj)).astype(jnp.float32)
    ksh = ks.transpose(0, 2, 1, 3)
    vsh = vs.transpose(0, 2, 1, 3)
    pad = ((0, 0), (0, 0), (WINDOW, 0), (0, 0))
    kwp = jnp.pad(kw.transpose(0, 2, 1, 3), pad)
    vwp = jnp.pad(vw.transpose(0, 2, 1, 3), pad)
    n_blk = S // QB
    qb = q.reshape(B, n_blk, QB, G, Hg, d).transpose(1, 0, 3, 4, 2, 5)
    gb = gates.reshape(B, n_blk, QB, G, Hg, 3).transpose(1, 0, 3, 4, 2, 5)
    b_ix = jnp.arange(B)[:, None, None, None]
    g_ix = jnp.arange(G)[None, :, None, None]
    blk_id = jnp.arange(n_sel)

    def one_block(args):
        qblk, gblk, i = args
        qs = i * QB
        qpos = qs + jnp.arange(QB)
        s = jnp.einsum('bghqd,bgnd->bghqn', qblk, kc).astype(jnp.float32) * scale
        cmask = cmp_end[None, :] <= qpos[:, None]
        p_cmp = jax.nn.softmax(jnp.where(cmask, s, NEG_LARGE), axis=-1) * cmask
        o_cmp = jnp.einsum('bghqn,bgnd->bghqd', p_cmp.astype(vc.dtype), vc)
        imp = jnp.einsum('bghqn,ns->bgqs', p_cmp, overlap)
        cur = qpos // SEL_BLOCK
        valid = blk_id[None, :] <= cur[:, None]
        forced = (blk_id[None, :] == 0) | (blk_id[None, :] == cur[:, None]) | (blk_id[None, :] == cur[:, None] - 1)
        score = jnp.where(valid, imp + jnp.where(forced, FORCE_BONUS, 0.0), -jnp.inf)
        _, sel = lax.top_k(score, k_sel)
        tok = (sel[..., None] * SEL_BLOCK + jnp.arange(SEL_BLOCK)).reshape(B, G, QB, k_sel * SEL_BLOCK)
        kg = ksh[b_ix, g_ix, tok]
        vg = vsh[b_ix, g_ix, tok]
        s = jnp.einsum('bghqd,bgqtd->bghqt', qblk, kg).astype(jnp.float32) * scale
        smask = (tok <= qpos[None, None, :, None])[:, :, None]
        p = jax.nn.softmax(jnp.where(smask, s, -jnp.inf), axis=-1)
        o_sel = jnp.einsum('bghqt,bgqtd->bghqd', p.astype(vg.dtype), vg)
        kwin = lax.dynamic_slice_in_dim(kwp, qs, WINDOW + QB, axis=2)
        vwin = lax.dynamic_slice_in_dim(vwp, qs, WINDOW + QB, axis=2)
        kpos = qs - WINDOW + jnp.arange(WINDOW + QB)
        wmask = (kpos[None, :] <= qpos[:, None]) & (kpos[None, :] > qpos[:, None] - WINDOW) & (kpos[None, :] >= 0)
        s = jnp.einsum('bghqd,bgkd->bghqk', qblk, kwin).astype(jnp.float32) * scale
        p = jax.nn.softmax(jnp.where(wmask, s, -jnp.inf), axis=-1)
        o_win = jnp.einsum('bghqk,bgkd->bghqd', p.astype(vwin.dtype), vwin)
        return gblk[..., 0:1] * o_cmp + gblk[..., 1:2] * o_sel + gblk[..., 2:3] * o_win

    o = lax.map(one_block, (qb, gb, jnp.arange(n_blk)))
    return o.transpose(1, 0, 4, 2, 3, 5).reshape(B, S, Hq * d)


def hybrid_mixer(a, w_in, lq1, lk1, lq2, lk2, subln, lambda_init, pe_k, pe_v, w1k, w1v, w2k, w2v, p_da, p_nsa, w_o):
    B, S, _ = a.shape
    pos = jnp.arange(S)
    z = a @ w_in
    splits = np.cumsum(IN_SPLIT_SIZES)[:-1].tolist()
    q_da, k_da, v_da, q_n, kc, vc, ks, vs, kw, vw, g_n, g_m = jnp.split(z, splits, axis=-1)
    q_da = partial_rope(q_da.reshape(B, S, 2 * DA_HEADS, DA_HEAD_DIM), pos).reshape(B, S, DA_HEADS, 2, DA_HEAD_DIM)
    k_da = partial_rope(k_da.reshape(B, S, 2 * DA_HEADS, DA_HEAD_DIM), pos).reshape(B, S, DA_HEADS, 2, DA_HEAD_DIM)
    v_da = v_da.reshape(B, S, DA_HEADS, DA_V_DIM)
    y_da = diff_attention(q_da, k_da, v_da, lq1, lk1, lq2, lk2, subln, lambda_init)
    kv_shape = (B, S, NSA_GROUPS, NSA_HEAD_DIM)
    q_n = partial_rope(q_n.reshape(B, S, NSA_HEADS, NSA_HEAD_DIM), pos)
    kc = partial_rope(kc.reshape(kv_shape), pos)
    ks = partial_rope(ks.reshape(kv_shape), pos)
    kw = partial_rope(kw.reshape(kv_shape), pos)
    gates = jax.nn.sigmoid(g_n.reshape(B, S, NSA_HEADS, 3))
    y_n = nsa_attention(q_n, kc, vc.reshape(kv_shape), ks, vs.reshape(kv_shape), kw, vw.reshape(kv_shape),
                        gates, pe_k, pe_v, w1k, w1v, w2k, w2v)
    g_a, g_b = jnp.split(jax.nn.sigmoid(g_m), 2, axis=-1)
    merged = g_a * (y_da @ p_da) + g_b * (y_n @ p_nsa)
    return merged @ w_o


def peer(c, w_q, sub_k1, sub_k2, u_tab, v_tab):
    B, S, D = c.shape
    T = B * S
    cf = c.reshape(T, D)
    q = (cf @ w_q).reshape(T, PEER_HEADS, 2, PEER_DQ // 2)
    s1 = jnp.einsum('thd,kd->thk', q[:, :, 0], sub_k1).astype(jnp.float32)
    s2 = jnp.einsum('thd,kd->thk', q[:, :, 1], sub_k2).astype(jnp.float32)
    v1, i1 = lax.top_k(s1, PEER_TOPK)
    v2, i2 = lax.top_k(s2, PEER_TOPK)
    cand = (v1[..., :, None] + v2[..., None, :]).reshape(T, PEER_HEADS, PEER_TOPK * PEER_TOPK)
    cidx = (i1[..., :, None] * PEER_NKEYS + i2[..., None, :]).reshape(T, PEER_HEADS, PEER_TOPK * PEER_TOPK)
    sc, pick = lax.top_k(cand, PEER_TOPK)
    eidx = jnp.take_along_axis(cidx, pick, axis=-1)
    gate = jax.nn.softmax(sc, axis=-1)
    E = PEER_HEADS * PEER_TOPK
    n_blk = T // PEER_TBLOCK
    xb = cf.reshape(n_blk, PEER_TBLOCK, D)
    ib = eidx.reshape(n_blk, PEER_TBLOCK, E)
    gb = gate.reshape(n_blk, PEER_TBLOCK, E).astype(c.dtype)

    def one_block(args):
        xblk, iblk, gblk = args
        u = u_tab[iblk]
        h = jax.nn.gelu(jnp.einsum('td,ted->te', xblk, u), approximate=False)
        return jnp.einsum('te,ted->td', h * gblk, v_tab[iblk])

    return lax.map(one_block, (xb, ib, gb)).reshape(B, S, D)


def setup_inputs(seed: int = 0) -> dict:
    key = jax.random.key(seed)
    ks = jax.random.split(key, 24)

    def nrm(k, shape, scale):
        return jax.random.normal(k, shape, jnp.float32) * scale

    d = NSA_HEAD_DIM
    return {
        'x': nrm(ks[0], (BATCH, SEQ, D_MODEL), 1.0),
        'attn_norm': 1.0 + nrm(ks[1], (DEPTH, D_MODEL), 0.01),
        'w_in': nrm(ks[2], (DEPTH, D_MODEL, IN_COLS), D_MODEL ** -0.5),
        'da_lambda_q1': nrm(ks[3], (DEPTH, DA_HEAD_DIM), 0.1),
        'da_lambda_k1': nrm(ks[4], (DEPTH, DA_HEAD_DIM), 0.1),
        'da_lambda_q2': nrm(ks[5], (DEPTH, DA_HEAD_DIM), 0.1),
        'da_lambda_k2': nrm(ks[6], (DEPTH, DA_HEAD_DIM), 0.1),
        'da_subln': 1.0 + nrm(ks[7], (DEPTH, DA_V_DIM), 0.01),
        'cmp_pe_k': nrm(ks[8], (DEPTH, CMP_BLOCK, d), 0.1),
        'cmp_pe_v': nrm(ks[9], (DEPTH, CMP_BLOCK, d), 0.1),
        'cmp_w1_k': nrm(ks[10], (DEPTH, CMP_BLOCK * d, d), (CMP_BLOCK * d) ** -0.5),
        'cmp_w1_v': nrm(ks[11], (DEPTH, CMP_BLOCK * d, d), (CMP_BLOCK * d) ** -0.5),
        'cmp_w2_k': nrm(ks[12], (DEPTH, d, d), d ** -0.5),
        'cmp_w2_v': nrm(ks[13], (DEPTH, d, d), d ** -0.5),
        'p_da': nrm(ks[14], (DEPTH, DA_WIDTH, D_MODEL), DA_WIDTH ** -0.5),
        'p_nsa': nrm(ks[15], (DEPTH, NSA_WIDTH, D_MODEL), NSA_WIDTH ** -0.5),
        'w_o': nrm(ks[16], (DEPTH, D_MODEL, D_MODEL), D_MODEL ** -0.5),
        'ffn_norm': 1.0 + nrm(ks[17], (DEPTH, D_MODEL), 0.01),
        'peer_wq': nrm(ks[18], (DEPTH, D_MODEL, PEER_HEADS * PEER_DQ), D_MODEL ** -0.5),
        'peer_k1': nrm(ks[19], (DEPTH, PEER_NKEYS, PEER_DQ // 2), (PEER_DQ // 2) ** -0.5),
        'peer_k2': nrm(ks[20], (DEPTH, PEER_NKEYS, PEER_DQ // 2), (PEER_DQ // 2) ** -0.5),
        'peer_u': nrm(ks[21], (DEPTH, N_EXPERTS, D_MODEL), D_MODEL ** -0.5),
        'peer_v': nrm(ks[22], (DEPTH, N_EXPERTS, D_MODEL), PEER_HEADS ** -0.5),
        'final_norm': 1.0 + nrm(ks[23], (D_MODEL,), 0.01),
    }


def reference(x, attn_norm, w_in, da_lambda_q1, da_lambda_k1, da_lambda_q2, da_lambda_k2, da_subln,
              cmp_pe_k, cmp_pe_v, cmp_w1_k, cmp_w1_v, cmp_w2_k, cmp_w2_v, p_da, p_nsa, w_o,
              ffn_norm, peer_wq, peer_k1, peer_k2, peer_u, peer_v, final_norm):
    h = x
    for l in range(DEPTH):
        lambda_init = 0.8 - 0.6 * math.exp(-0.3 * l)
        a = rmsnorm(h, attn_norm[l])
        h = h + hybrid_mixer(a, w_in[l], da_lambda_q1[l], da_lambda_k1[l], da_lambda_q2[l], da_lambda_k2[l],
                             da_subln[l], lambda_init, cmp_pe_k[l], cmp_pe_v[l], cmp_w1_k[l], cmp_w1_v[l],
                             cmp_w2_k[l], cmp_w2_v[l], p_da[l], p_nsa[l], w_o[l])
        c = rmsnorm(h, ffn_norm[l])
        h = h + peer(c, peer_wq[l], peer_k1[l], peer_k2[l], peer_u[l], peer_v[l])
    return rmsnorm(h, final_norm)
```

```python
import contextlib
import math
import numpy as np
import ml_dtypes
import concourse.bass as bass
import concourse.mybir as mybir
from concourse.bass_utils import run_bass_kernel_spmd

F32 = mybir.dt.float32
BF16 = mybir.dt.bfloat16
ALU = mybir.AluOpType
AF = mybir.ActivationFunctionType
AX = mybir.AxisListType

D = 1024
NEG = -30000.0


class _Op:
    __slots__ = ("eng", "fn", "deps", "marked", "val", "is_dma", "sem_i", "idx")

    def __init__(self, eng, fn, is_dma):
        self.eng = eng
        self.fn = fn
        self.deps = []
        self.marked = False
        self.val = None
        self.is_dma = is_dma
        self.sem_i = None


class Prog:
    COMPUTE = ("pe", "act", "dve", "pool")
    NDMA_SEM = 8
    SAME_ENG_DIST = 10 ** 9

    def __init__(self, nc):
        self.nc = nc
        self.ops = {e: [] for e in ("pe", "act", "dve", "pool", "sp")}
        self.state = {}
        self.bar = {e: [] for e in self.ops}
        self.dmas_since_bar = []

    suffix = None
    shared = ()

    def _k(self, k):
        if self.suffix is None or k in self.shared or k.startswith("bk") or k.startswith("stg"):
            return k
        return k + self.suffix

    def _add(self, eng, fn, reads, writes, is_dma):
        if self.suffix is not None:
            reads = [self._k(k) for k in reads]
            writes = [self._k(k) for k in writes]
        op = _Op(eng, fn, is_dma)
        deps = list(self.bar[eng])
        self.bar[eng] = []
        for k in reads:
            st = self.state.get(k)
            if st is not None and st[0] is not None:
                deps.append(st[0])
        for k in writes:
            st = self.state.get(k)
            if st is not None:
                if st[0] is not None:
                    deps.append(st[0])
                deps.extend(st[1].values())
                deps.extend(st[2])
        seen = set()
        for d in deps:
            if id(d) in seen:
                continue
            seen.add(id(d))
            if d.eng == "pe" and eng == "pe" and not d.is_dma and not is_dma:
                continue
            op.deps.append(d)
            d.marked = True
        for k in reads:
            st = self.state.setdefault(k, [None, {}, []])
            if is_dma:
                st[2].append(op)
            else:
                st[1][eng] = op
        for k in writes:
            self.state[k] = [op, {}, []]
        op.idx = len(self.ops[eng])
        self.ops[eng].append(op)
        if is_dma:
            self.dmas_since_bar.append(op)
        return op

    def op(self, eng, fn, reads=(), writes=()):
        return self._add(eng, fn, reads, writes, False)

    def dma(self, eng, fn, reads=(), writes=()):
        op = self._add(eng, fn, reads, writes, True)
        op.marked = True
        return op

    def barrier(self):
        deps = []
        for e, lst in self.ops.items():
            for o in reversed(lst):
                if not o.is_dma:
                    deps.append(o)
                    break
        deps.extend(self.dmas_since_bar)
        self.dmas_since_bar = []
        for e in self.bar:
            self.bar[e] = self.bar[e] + deps
        self.state = {}

    def emit(self, es, final_ops):
        nc = self.nc
        csem = {e: es.enter_context(nc.semaphore("s_" + e)) for e in self.COMPUTE}
        dsem = {e: [es.enter_context(nc.semaphore("d_%s%d" % (e, i))) for i in range(self.NDMA_SEM)]
                for e in ("sp", "pool")}
        for e, lst in self.ops.items():
            cnt = 0
            dcnt = 0
            for o in lst:
                if o.is_dma:
                    o.sem_i = dcnt % self.NDMA_SEM
                    o.val = 16 * (dcnt // self.NDMA_SEM + 1)
                    dcnt += 1
                elif o.marked:
                    cnt += 1
                    o.val = cnt
        block = es.enter_context(nc.Block())

        def run(ename, e):
            waited = {}
            for o in self.ops[ename]:
                waits = {}
                for d in o.deps:
                    if (not d.is_dma) and (not o.is_dma) and d.eng == ename and ename in ("dve", "act") and o.idx - d.idx >= self.SAME_ENG_DIST:
                        continue
                    if d.is_dma:
                        key = (d.eng, d.sem_i)
                        sem = dsem[d.eng][d.sem_i]
                    else:
                        key = (d.eng, None)
                        sem = csem[d.eng]
                    if waited.get(key, 0) >= d.val:
                        continue
                    if key not in waits or waits[key][1] < d.val:
                        waits[key] = (sem, d.val)
                if o.is_dma and o.val > 16:
                    key = (ename, o.sem_i)
                    if waited.get(key, 0) < o.val - 16:
                        if key not in waits or waits[key][1] < o.val - 16:
                            waits[key] = (dsem[ename][o.sem_i], o.val - 16)
                for key, (sem, v) in waits.items():
                    e.wait_ge(sem, v)
                    waited[key] = v
                ins = o.fn(e)
                if o.is_dma:
                    ins.then_inc(dsem[ename][o.sem_i], 16)
                elif o.marked:
                    ins.then_inc(csem[ename], 1)
            if ename == "sp":
                for o in final_ops:
                    e.wait_ge(dsem[o.eng][o.sem_i], o.val)

        @block.sync
        def _(e):
            run("sp", e)

        @block.tensor
        def _(e):
            run("pe", e)

        @block.scalar
        def _(e):
            run("act", e)

        @block.vector
        def _(e):
            run("dve", e)

        @block.gpsimd
        def _(e):
            run("pool", e)


C_QDA, C_KDA, C_VDA, C_QN, C_KC, C_VC, C_KS, C_VS, C_KW, C_VW, C_GN, C_GM = (
    0, 512, 1024, 1536, 2048, 2176, 2304, 2432, 2560, 2688, 2816, 2840)
FM_COLS = [C_QDA + 128 * i for i in range(4)] + [C_KDA + 128 * i for i in range(4)] + \
          [C_QN + 128 * i for i in range(4)] + [C_KC, C_KS, C_KW, C_VC]
N_ROPED = 15
LAMBDA_INIT = 0.8 - 0.6 * math.exp(0.0)


class _Stop(Exception):
    pass


def build(S, dbg=False, LIM=99):
    NT = S // 128
    NQC = S // 512
    NCMP = (S - 32) // 16 + 1
    TB = 256
    NB = S // TB
    nc = bass.Bass("TRN2", target_bir_lowering=False)

    def din(name, shape, dt=F32):
        return nc.dram_tensor(name, list(shape), dt, kind="ExternalInput").ap()

    def dscr(name, shape, dt):
        return nc.dram_tensor(name, list(shape), dt, kind="Internal").ap()

    x = din("x", [S, D])
    attn_norm = din("attn_norm", [D])
    w_in = din("w_in", [D, 4888])
    w_rot = din("w_rot", [D, 1920])
    lam4 = din("lam4", [4, 64])
    subln = din("subln", [128])
    peT_k = din("peT_k", [64, 32])
    peT_v = din("peT_v", [64, 32])
    w1k = din("w1k", [2048, 64])
    w1v = din("w1v", [2048, 64])
    w2k = din("w2k", [64, 64])
    w2v = din("w2v", [64, 64])
    p_da = din("p_da", [512, D])
    p_nsa = din("p_nsa", [512, D])
    w_o = din("w_o", [D, D])
    ffn_norm = din("ffn_norm", [D])
    wq = din("wq", [D, 2048])
    k1T = din("k1T", [128, 128])
    k2T = din("k2T", [128, 128])
    peer_u = din("peer_u", [16384, D])
    peer_v = din("peer_v", [16384, D])
    final_norm = din("final_norm", [D])
    ropec = din("ropec", [128, S])
    ropes = din("ropes", [128, S])
    masks = din("masks", [8, 128, 512], BF16)
    cmaskT = din("cmaskT", [256, S], BF16)
    esel = din("esel", [64, S], BF16)
    csel = din("csel", [S, 64])
    ovl = din("ovl", [256, 64], BF16)
    y = nc.dram_tensor("y", [S, D], F32, kind="ExternalOutput").ap()

    FM = dscr("FM", [16, 128, S], BF16)
    VT = dscr("VT", [S, 768], BF16)
    GN = dscr("GN", [S, 24], F32)
    GM = dscr("GM", [S, 2048], F32)
    H = dscr("H", [S, D], F32)
    CT = dscr("CT", [128, 8, S], BF16)
    YDA = dscr("YDA", [S, 512], F32)
    QTs = dscr("QTs", [S // 256, 128, 16 * 256], BF16)
    SVs = dscr("SVs", [S // 256, 3, 128, 256], F32)
    UT = dscr("UT", [128, 128, 1024], BF16)
    VB = dscr("VB", [128, 128, 1024], BF16)
    dbg_out = {}
    if dbg:
        dbg_out["d_fm"] = nc.dram_tensor("d_fm", [16, 128, S], BF16, kind="ExternalOutput").ap()
        dbg_out["d_h"] = nc.dram_tensor("d_h", [S, D], F32, kind="ExternalOutput").ap()
        dbg_out["d_yy"] = nc.dram_tensor("d_yy", [S, 1024], F32, kind="ExternalOutput").ap()
        dbg_out["d_pe"] = nc.dram_tensor("d_pe", [S, D], F32, kind="ExternalOutput").ap()

    es = contextlib.ExitStack()
    with es:
        ARENA_N = 104000
        arena = es.enter_context(nc.sbuf_tensor("arena", [128, ARENA_N], BF16))
        banks = [es.enter_context(nc.psum_tensor("bk%d" % i, [128, 512], F32)) for i in range(8)]
        bk = ["bk%d" % i for i in range(8)]
        off = [0]

        def al(n, dt=BF16):
            k = n * (2 if dt == F32 else 1)
            assert off[0] + k <= ARENA_N, ("arena overflow", off[0], k)
            a = arena[:, off[0]:off[0] + k]
            off[0] += (k + 15) // 16 * 16
            return a.bitcast(dt) if dt != BF16 else a

        P = Prog(nc)
        dq = ["sp", "pool"]
        dqi = [0]

        def dma(out, in_, r=(), w=(), q=None, slow=False):
            if q is None:
                q = dq[dqi[0] % 2]
                dqi[0] += 1
            if slow:
                return P.dma(q, lambda e: e.dma_start(out=out, in_=in_, allow_slow_non_contiguous=True), reads=r, writes=w)
            return P.dma(q, lambda e: e.dma_start(out=out, in_=in_), reads=r, writes=w)

        def mm(out, lhsT, rhs, st, sp_, r, w):
            return P.op("pe", lambda e: e.matmul(out, lhsT=lhsT, rhs=rhs, start=st, stop=sp_), reads=r, writes=w)

        def tr(out, in_, ident_, r, w):
            return P.op("pe", lambda e: e.transpose(out=out, in_=in_, identity=ident_), reads=r, writes=w)

        def act(out, in_, func, r, w, bias=None, scale=None, accum=None):
            kw = {}
            if bias is not None:
                kw["bias"] = bias
            if scale is not None:
                kw["scale"] = scale
            if accum is not None:
                kw["accum_out"] = accum
            return P.op("act", lambda e: e.activation(out=out, in_=in_, func=func, **kw), reads=r, writes=w)

        def ts(eng, out, in0, s1, s2, op0, op1, r, w):
            if op1 is None:
                return P.op(eng, lambda e: e.tensor_scalar(out=out, in0=in0, scalar1=s1, scalar2=None, op0=op0), reads=r, writes=w)
            return P.op(eng, lambda e: e.tensor_scalar(out=out, in0=in0, scalar1=s1, scalar2=s2, op0=op0, op1=op1), reads=r, writes=w)

        def tt(eng, out, in0, in1, op, r, w):
            return P.op(eng, lambda e: e.tensor_tensor(out=out, in0=in0, in1=in1, op=op), reads=r, writes=w)

        def stt(eng, out, in0, scalar, in1, op0, op1, r, w):
            return P.op(eng, lambda e: e.scalar_tensor_tensor(out=out, in0=in0, scalar=scalar, in1=in1, op0=op0, op1=op1), reads=r, writes=w)

        def cp(eng, out, in_, r, w):
            if eng == "act":
                return act(out, in_, AF.Copy, r, w)
            return P.op(eng, lambda e: e.tensor_copy(out=out, in_=in_), reads=r, writes=w)

        def recip(out, in_, r, w):
            return P.op("dve", lambda e: e.reciprocal(out=out, in_=in_), reads=r, writes=w)

        def memset(eng, out, v, w):
            return P.op(eng, lambda e: e.memset(out, v), writes=w)

        try:
            identf = al(128, F32)
            ident = al(128)
            memset("pool", identf, 0.0, ["identf"])
            P.op("pool", lambda e: e.affine_select(out=identf, in_=identf, pattern=[[-1, 128]], compare_op=ALU.not_equal,
                                                   fill=1.0, base=0, channel_multiplier=1), reads=["identf"], writes=["identf"])
            cp("dve", ident, identf, ["identf"], ["ident"])
            stg = [al(1024, F32) for _ in range(2)]
            stgk = ["stg0", "stg1"]
            stgi = [0]
            PERSIST = off[0]

            def load_cast(dst, src, n_p, shape_free, eng=None, p0=0):
                i = stgi[0] % 2
                stgi[0] += 1
                n = int(np.prod(shape_free))
                assert n <= 1024
                sv = stg[i][p0:p0 + n_p, 0:n]
                if len(shape_free) == 2:
                    sv = sv.rearrange("p (a b) -> p a b", a=shape_free[0])
                dma(sv, src, w=[stgk[i]])
                e_ = eng or ("dve" if i == 0 else "pool")
                return sv, stgk[i], e_

            def load_w(dst, dkey, src, ncols, kch=8):
                srcv = src.rearrange("(kc p) n -> p kc n", p=128)
                step = 1024 // kch
                first = True
                for c0 in range(0, ncols, step):
                    c1 = min(ncols, c0 + step)
                    sv, sk, e_ = load_cast(None, srcv[:, :, c0:c1], 128, [kch, c1 - c0])
                    cp(e_, dst[:, :, c0:c1], sv, [sk] + ([] if first else [dkey]), [dkey])
                    first = False

            u_v = peer_u.rearrange("(i j) d -> j i d", j=128)
            v_v = peer_v.rearrange("(i j) d -> j i d", j=128)
            t_state = {"j": 0, "s1": 0}

            def t_stage1(j):
                if j >= 128 or t_state["s1"] > j:
                    return
                t_state["s1"] = j + 1
                a = (2 * j) % 4
                b = (2 * j + 1) % 4
                dma(t_f[a], u_v[j], w=["t_f%d" % a], q="sp")
                dma(t_f[b], v_v[j], w=["t_f%d" % b], q="sp")
                cp("act" if j % 2 else "dve", t_b[a], t_f[a], ["t_f%d" % a], ["t_b%d" % a])
                cp("dve" if j % 2 else "act", t_b[b], t_f[b], ["t_f%d" % b], ["t_b%d" % b])
                dma(VB[j], t_b[b], r=["t_b%d" % b], w=["VB"], q="sp")

            def t_step():
                j = t_state["j"]
                if j >= 128:
                    return
                t_state["j"] = j + 1
                t_stage1(j)
                t_stage1(j + 1)
                a = (2 * j) % 4
                pb = 6 + (j % 2)
                pbv = banks[pb][:, :].bitcast(BF16)
                for kc in range(8):
                    tr(pbv[:, kc * 128:(kc + 1) * 128], t_b[a][:, kc * 128:(kc + 1) * 128], ident, ["t_b%d" % a, "ident"], [bk[pb]])
                o = j % 2
                cp("act" if j % 2 else "dve", t_o[o], pbv, [bk[pb]], ["t_o%d" % o])
                dma(UT[j], t_o[o], r=["t_o%d" % o], w=["UT"], q="sp")

            Wfm = al(8 * 2048).rearrange("p (k n) -> p k n", k=8)
            Wrot = al(8 * 1920).rearrange("p (k n) -> p k n", k=8)
            Wv = al(8 * 768).rearrange("p (k n) -> p k n", k=8)
            Wgn = al(8 * 24).rearrange("p (k n) -> p k n", k=8)
            Wgm = al(8 * 2048).rearrange("p (k n) -> p k n", k=8)
            for i, c0 in enumerate(FM_COLS):
                load_w(Wfm[:, :, i * 128:(i + 1) * 128], "Wfm%d" % i, w_in[:, c0:c0 + 128], 128)
            load_w(Wrot, "Wrot", w_rot, 1920)
            load_w(Wv[:, :, 0:512], "Wv0", w_in[:, C_VDA:C_VDA + 512], 512)
            load_w(Wv[:, :, 512:640], "Wv1", w_in[:, C_VS:C_VS + 128], 128)
            load_w(Wv[:, :, 640:768], "Wv2", w_in[:, C_VW:C_VW + 128], 128)
            load_w(Wgn, "Wgn", w_in[:, C_GN:C_GN + 24], 24)
            load_w(Wgm, "Wgm", w_in[:, C_GM:C_GM + 2048], 2048)
            WFMK = ["Wfm%d" % i for i in range(16)]
            gA = al(8, F32)
            dma(gA, attn_norm.rearrange("(k p) -> p k", p=128), w=["gA"], slow=True)
            xs = [al(1024, F32) for _ in range(2)]
            xb = [al(1024) for _ in range(2)]
            junk = al(1024, F32)
            ssq = [al(1, F32) for _ in range(2)]
            aT = al(8 * 512).rearrange("p (k n) -> p k n", k=8)
            rc = al(512, F32)
            rs_ = al(512, F32)
            tA = [al(512, F32) for _ in range(2)]
            tB = [al(512, F32) for _ in range(2)]
            fmo = [al(512) for _ in range(3)]
            vo = [al(768) for _ in range(2)]
            gno = [al(24, F32) for _ in range(2)]
            gmo = [al(2048, F32) for _ in range(2)]
            t_f = [al(1024, F32) for _ in range(4)]
            t_b = [al(1024) for _ in range(4)]
            t_o = [al(1024) for _ in range(2)]
            T_PER_TILE = -(-128 // (NQC * 16))

            def rms_to_T(xt_ap, xkey, gain, gkey, dstT, dkey, i2, pbank):
                act(junk, xt_ap, AF.Square, [xkey], ["junk", "ssq%d" % i2], scale=1.0 / 32.0, accum=ssq[i2])
                act(ssq[i2], ssq[i2], AF.Sqrt, ["ssq%d" % i2], ["ssq%d" % i2], bias=1e-6, scale=1.0)
                recip(ssq[i2], ssq[i2], ["ssq%d" % i2], ["ssq%d" % i2])
                act(xb[i2], xt_ap, AF.Copy, [xkey, "ssq%d" % i2], ["xb%d" % i2], scale=ssq[i2])
                pv = banks[pbank][:, :].bitcast(BF16)
                for kc in range(8):
                    tr(pv[:, kc * 128:(kc + 1) * 128], xb[i2][:, kc * 128:(kc + 1) * 128], ident, ["xb%d" % i2, "ident"], [bk[pbank]])
                tt("dve", dstT, pv.rearrange("p (k n) -> p k n", k=8), gain.unsqueeze(2).to_broadcast([128, 8, 128]),
                   ALU.mult, [bk[pbank], gkey, dkey], [dkey])

            fm_cnt = 0
            for c in range(NQC):
                q0 = c * 512
                dma(rc, ropec[:, q0:q0 + 512], w=["rc"])
                dma(rs_, ropes[:, q0:q0 + 512], w=["rs"])
                for t4 in range(4):
                    t0 = q0 + t4 * 128
                    i2 = t4 % 2
                    dma(xs[i2], x[t0:t0 + 128, :], w=["xs%d" % i2])
                    rms_to_T(xs[i2], "xs%d" % i2, gA, "gA", aT[:, :, t4 * 128:(t4 + 1) * 128], "aT", i2, 6 + i2)
                for i in range(16):
                    pz = (2 * i) % 4
                    pr = (2 * i + 1) % 4
                    for kc in range(8):
                        mm(banks[pz][:, :], Wfm[:, kc, i * 128:(i + 1) * 128], aT[:, kc, :], kc == 0, kc == 7, ["aT", WFMK[i]], [bk[pz]])
                    fo = fm_cnt % 3
                    fm_cnt += 1
                    if i < N_ROPED:
                        for kc in range(8):
                            mm(banks[pr][:, :], Wrot[:, kc, i * 128:(i + 1) * 128], aT[:, kc, :], kc == 0, kc == 7, ["aT", "Wrot"], [bk[pr]])
                        a2 = i % 2
                        tt("dve", tA[a2], banks[pz][:, :], rc, ALU.mult, [bk[pz], "rc"], ["tA%d" % a2])
                        tt("dve", tB[a2], banks[pr][:, :], rs_, ALU.mult, [bk[pr], "rs"], ["tB%d" % a2])
                        tt("pool", fmo[fo], tA[a2], tB[a2], ALU.add, ["tA%d" % a2, "tB%d" % a2], ["fmo%d" % fo])
                    else:
                        cp("act", fmo[fo], banks[pz][:, :], [bk[pz]], ["fmo%d" % fo])
                    dma(FM[i, :, q0:q0 + 512], fmo[fo], r=["fmo%d" % fo], w=["FM"])
                    for _ in range(T_PER_TILE):
                        t_step()
                    if dbg:
                        dma(dbg_out["d_fm"][i, :, q0:q0 + 512], fmo[fo], r=["fmo%d" % fo], w=["d_fm"])
                for t4 in range(4):
                    t0 = q0 + t4 * 128
                    i2 = t4 % 2
                    lcol = slice(t4 * 128, (t4 + 1) * 128)
                    for (c0, c1, pb) in ((0, 512, 4), (512, 768, 5)):
                        for kc in range(8):
                            mm(banks[pb][:, 0:c1 - c0], aT[:, kc, lcol], Wv[:, kc, c0:c1], kc == 0, kc == 7, ["aT", "Wv0", "Wv1", "Wv2"], [bk[pb]])
                        cp("act", vo[i2][:, c0:c1], banks[pb][:, 0:c1 - c0], [bk[pb], "vo%d" % i2], ["vo%d" % i2])
                    dma(VT[t0:t0 + 128, :], vo[i2], r=["vo%d" % i2], w=["VT"])
                    for kc in range(8):
                        mm(banks[5][:, 0:24], aT[:, kc, lcol], Wgn[:, kc, :], kc == 0, kc == 7, ["aT", "Wgn"], [bk[5]])
                    act(gno[i2], banks[5][:, 0:24], AF.Sigmoid, [bk[5]], ["gno%d" % i2])
                    dma(GN[t0:t0 + 128, :], gno[i2], r=["gno%d" % i2], w=["GN"])
                    for g4 in range(4):
                        pb = g4 % 4
                        for kc in range(8):
                            mm(banks[pb][:, :], aT[:, kc, lcol], Wgm[:, kc, g4 * 512:(g4 + 1) * 512], kc == 0, kc == 7, ["aT", "Wgm"], [bk[pb]])
                        act(gmo[i2][:, g4 * 512:(g4 + 1) * 512], banks[pb][:, :], AF.Sigmoid, [bk[pb], "gmo%d" % i2], ["gmo%d" % i2])
                    dma(GM[t0:t0 + 128, :], gmo[i2], r=["gmo%d" % i2], w=["GM"])
            while t_state["j"] < 128:
                t_step()
            P.barrier()
            if LIM == 2:
                raise _Stop()
            off[0] = PERSIST

            SC = [0, 1, 6]
            ACC = [2, 3, 4, 5]
            sc_i = [0]
            ecnt = [0]
            smc = [0]
            MSK = al(8 * 512).rearrange("p (m n) -> p m n", m=8)
            dma(MSK, masks.rearrange("m p n -> p m n"), w=["MSK"])
            E = [al(512) for _ in range(4)]
            sm = al(64, F32)
            PERSIST2 = off[0]

            def smalloc():
                i = smc[0] % 64
                smc[0] += 1
                return sm[:, i:i + 1], "sm%d" % i

            def score_tile(lhsT, rhs, kkeys, mask_ap):
                b = SC[sc_i[0] % 3]
                sc_i[0] += 1
                mm(banks[b][:, :], lhsT, rhs, True, mask_ap is None, kkeys, [bk[b]])
                if mask_ap is not None:
                    mm(banks[b][:, :], ident, mask_ap, False, True, ["ident", "MSK", "CM0", "CM1"], [bk[b]])
                ei = ecnt[0] % 4
                ecnt[0] += 1
                act(E[ei], banks[b][:, :], AF.Exp, [bk[b]], ["E%d" % ei], scale=0.125)
                return E[ei], "E%d" % ei

            LOOK = 2

            def run_jobs(jobs):
                q_ = []
                for (sf, pf, ef) in jobs:
                    q_.append((sf(), pf, ef))
                    if len(q_) > LOOK:
                        Ee, pf0, ef0 = q_.pop(0)
                        if pf0 is not None:
                            pf0(Ee)
                        if ef0 is not None:
                            ef0()
                for (Ee, pf0, ef0) in q_:
                    if pf0 is not None:
                        pf0(Ee)
                    if ef0 is not None:
                        ef0()

            KDA = [al(S) for _ in range(4)]
            for h in range(4):
                dma(KDA[h], FM[4 + h], r=["FM"], w=["KDA%d" % h])
            VDA = al(NT * 4 * 130).rearrange("p (t h d) -> p t h d", t=NT, h=4)
            memset("pool", VDA[:, :, :, 128:130], 1.0, ["VDA"])
            VTv = VT.rearrange("(t p) c -> p t c", p=128)
            for h in range(4):
                dma(VDA[:, :, h, 0:128], VTv[:, :, h * 128:(h + 1) * 128], r=["VT", "VDA"], w=["VDA"])
            lamt = al(256, F32)
            dma(lamt, lam4.rearrange("a d -> (a d)").partition_broadcast(128), w=["lamt"])
            lamp = al(128, F32)
            lam2 = al(4, F32)
            l4 = lamt.rearrange("p (a d) -> p a d", a=4)
            tt("dve", lamp.rearrange("p (a d) -> p a d", a=2), l4[:, 0:4:2, :], l4[:, 1:4:2, :], ALU.mult, ["lamt"], ["lamp"])
            P.op("dve", lambda e: e.reduce_sum(out=lam2[:, 0:2], in_=lamp.rearrange("p (a d) -> p a d", a=2), axis=AX.X), reads=["lamp"], writes=["lam2"])
            act(lam2[:, 0:2], lam2[:, 0:2], AF.Exp, ["lam2"], ["lam2"])
            tt("dve", lam2[:, 2:3], lam2[:, 1:2], lam2[:, 0:1], ALU.subtract, ["lam2"], ["lam2"])
            ts("dve", lam2[:, 3:4], lam2[:, 2:3], -LAMBDA_INIT, None, ALU.add, None, ["lam2"], ["lam2"])
            neglam = lam2[:, 3:4]
            sublnB = al(128, F32)
            dma(sublnB, subln.partition_broadcast(128), w=["sublnB"])
            ts("dve", sublnB, sublnB, 1.0 - LAMBDA_INIT, None, ALU.mult, None, ["sublnB"], ["sublnB"])
            QDA = [[al(512) for _ in range(2)] for _ in range(4)]
            for h in range(4):
                memset("pool", QDA[h][0][64:128, :], 0.0, ["QDA%d_0" % h])
                memset("pool", QDA[h][1][0:64, :], 0.0, ["QDA%d_1" % h])
            o1 = [al(128, F32) for _ in range(4)]
            yda = [al(512, F32) for _ in range(4)]
            junkb = al(128)
            for c in range(NQC):
                q0 = c * 512
                kt_hi = q0 // 128 + 3
                for h in range(4):
                    dma(QDA[h][0][0:64, :], FM[h, 0:64, q0:q0 + 512], r=["FM", "QDA%d_0" % h], w=["QDA%d_0" % h])
                    dma(QDA[h][1][64:128, :], FM[h, 64:128, q0:q0 + 512], r=["FM", "QDA%d_1" % h], w=["QDA%d_1" % h])
                jobs = []
                for h in range(4):
                    for cc in range(2):
                        rows = slice(cc * 64, (cc + 1) * 64)
                        for kt in range(0, kt_hi + 1):
                            d8 = kt - (q0 // 128 - 4)
                            mk = MSK[:, d8, :] if d8 >= 4 else None

                            def sf(h=h, cc=cc, kt=kt, mk=mk):
                                return score_tile(KDA[h][:, kt * 128:(kt + 1) * 128], QDA[h][cc], ["KDA%d" % h, "QDA%d_%d" % (h, cc)], mk)

                            def pf(Ee, h=h, kt=kt):
                                Ea, Ek = Ee
                                for qt in range(4):
                                    qti = q0 // 128 + qt
                                    if kt > qti:
                                        continue
                                    a = ACC[qt]
                                    mm(banks[a][:, 0:129], Ea[:, qt * 128:(qt + 1) * 128], VDA[:, kt, h, 0:129], kt == 0, kt == qti, [Ek, "VDA"], [bk[a]])

                            def ef(h=h, cc=cc):
                                for qt in range(4):
                                    a = ACC[qt]
                                    rz, rzk = smalloc()
                                    recip(rz, banks[a][:, 128:129], [bk[a]], [rzk])
                                    if cc == 0:
                                        ts("dve", o1[qt], banks[a][:, 0:128], rz, None, ALU.mult, None, [bk[a], rzk], ["o1_%d" % qt])
                                    else:
                                        tt("dve", rz, rz, neglam, ALU.mult, [rzk, "lam2"], [rzk])
                                        stt("dve", o1[qt], banks[a][:, 0:128], rz, o1[qt], ALU.mult, ALU.add, [bk[a], rzk, "o1_%d" % qt], ["o1_%d" % qt])
                                        s2, s2k = smalloc()
                                        act(junkb, o1[qt], AF.Square, ["o1_%d" % qt], ["junkb", s2k], scale=1.0 / math.sqrt(128.0), accum=s2)
                                        act(s2, s2, AF.Sqrt, [s2k], [s2k], bias=1e-6, scale=1.0)
                                        recip(s2, s2, [s2k], [s2k])
                                        stt("dve", yda[qt][:, h * 128:(h + 1) * 128], o1[qt], s2, sublnB, ALU.mult, ALU.mult,
                                            ["o1_%d" % qt, s2k, "sublnB", "yda%d" % qt], ["yda%d" % qt])

                            jobs.append((sf, pf, ef if kt == kt_hi else None))
                run_jobs(jobs)
                for qt in range(4):
                    t0 = q0 + qt * 128
                    dma(YDA[t0:t0 + 128, :], yda[qt], r=["yda%d" % qt], w=["YDA"])
            P.barrier()
            if LIM == 3:
                raise _Stop()
            off[0] = PERSIST2

            KSA = [al(S) for _ in range(2)]
            KW = [al(S) for _ in range(2)]
            for g in range(2):
                dma(KSA[g][0:64, :], FM[13, g * 64:(g + 1) * 64, :], r=["FM"], w=["KSA%d" % g])
                dma(KSA[g][64:128, :], esel, r=["KSA%d" % g], w=["KSA%d" % g])
                memset("pool", KW[g][64:128, :], 0.0, ["KW%d" % g])
                dma(KW[g][0:64, :], FM[14, g * 64:(g + 1) * 64, :], r=["FM", "KW%d" % g], w=["KW%d" % g])
            VS = al(NT * 2 * 66).rearrange("p (t g d) -> p t g d", t=NT, g=2)
            VW = al(NT * 2 * 66).rearrange("p (t g d) -> p t g d", t=NT, g=2)
            memset("pool", VS[:, :, :, 64:66], 1.0, ["VS"])
            memset("pool", VW[:, :, :, 64:66], 1.0, ["VW"])
            for g in range(2):
                dma(VS[:, :, g, 0:64], VTv[:, :, 512 + g * 64:512 + (g + 1) * 64], r=["VT", "VS"], w=["VS"])
                dma(VW[:, :, g, 0:64], VTv[:, :, 640 + g * 64:640 + (g + 1) * 64], r=["VT", "VW"], w=["VW"])
            KCMP = [al(256) for _ in range(2)]
            for g in range(2):
                memset("pool", KCMP[g], 0.0, ["KCMP%d" % g])
            VCO = al(2 * 2 * 130).rearrange("p (g t d) -> p g t d", g=2, t=2)
            memset("pool", VCO, 0.0, ["VCO"])
            memset("pool", VCO[:, :, :, 64:65], 1.0, ["VCO"])
            for g in range(2):
                for nt in range(2):
                    dma(VCO[:, g, nt, 65:129], ovl[nt * 128:(nt + 1) * 128, :], r=["VCO"], w=["VCO"])

            mark_B = off[0]
            kcT = al(S)
            vcT = al(S)
            dma(kcT, FM[12], r=["FM"], w=["kcT"])
            dma(vcT, FM[15], r=["FM"], w=["vcT"])
            W1 = {}
            for nm, src in (("k", w1k), ("v", w1v)):
                W1[nm] = al(2048).rearrange("p (l o) -> p l o", l=32)
                srcv = src.rearrange("(l d) o -> d l o", d=64)
                for half in range(2):
                    for lh in range(2):
                        sv, sk, e_ = load_cast(None, srcv[:, lh * 16:(lh + 1) * 16, :], 64, [16, 64], eng="dve", p0=half * 64)
                        kk_ = "W1%s%d" % (nm, half)
                        cp(e_, W1[nm][half * 64:(half + 1) * 64, lh * 16:(lh + 1) * 16, :], sv, [sk] + ([kk_] if lh else []), [kk_])
            W2 = {}
            for nm, src in (("k", w2k), ("v", w2v)):
                W2[nm] = al(64)
                sv, sk, e_ = load_cast(None, src, 64, [64], eng="dve")
                cp(e_, W2[nm][0:64, 0:64], sv, [sk], ["W2" + nm])
            PEb = {}
            for nm, src in (("k", peT_k), ("v", peT_v)):
                PEb[nm] = al(32)
                sv, sk, e_ = load_cast(None, src, 64, [32], eng="dve")
                cp(e_, PEb[nm][0:64, :], sv, [sk], ["PE" + nm])
            cb = al(2, F32)
            hidT = al(256)
            memset("pool", hidT, 0.0, ["hidT"])
            for ki, nm in enumerate(("k", "v")):
                for l in range(32):
                    mm(banks[6][0:64, ki:ki + 1], W1[nm][0:64, l, :], PEb[nm][0:64, l:l + 1], l == 0, l == 31, ["W1%s0" % nm, "PE" + nm], [bk[6]])
                cp("dve", cb[0:64, ki:ki + 1], banks[6][0:64, ki:ki + 1], [bk[6], "cb"], ["cb"])
                srcT = kcT if nm == "k" else vcT
                skey = "kcT" if nm == "k" else "vcT"
                for g in range(2):
                    rows = slice(g * 64, (g + 1) * 64)
                    sview = srcT.rearrange("p (n s) -> p n s", s=16)
                    for l in range(32):
                        rhs = sview[rows, (l // 16):(l // 16) + NCMP, l % 16]
                        mm(banks[7][0:64, 0:NCMP], W1[nm][rows, l, :], rhs, l == 0, l == 31, ["W1%s%d" % (nm, g), skey], [bk[7]])
                    act(hidT[0:64, 0:NCMP], banks[7][0:64, 0:NCMP], AF.Gelu, [bk[7], "cb", "hidT"], ["hidT"], bias=cb[0:64, ki:ki + 1], scale=1.0)
                    if nm == "k":
                        mm(banks[6][0:64, 0:256], W2["k"][0:64, 0:64], hidT[0:64, :], True, True, ["W2k", "hidT"], [bk[6]])
                        cp("dve", KCMP[g][0:64, :], banks[6][0:64, 0:256], [bk[6], "KCMP%d" % g], ["KCMP%d" % g])
                    else:
                        for nt in range(2):
                            mm(banks[6][:, 0:64], hidT[0:64, nt * 128:(nt + 1) * 128], W2["v"][0:64, 0:64], True, True, ["W2v", "hidT"], [bk[6]])
                            cp("dve", VCO[:, g, nt, 0:64], banks[6][:, 0:64], [bk[6], "VCO"], ["VCO"])
            P.barrier()
            if LIM == 4:
                raise _Stop()
            off[0] = mark_B

            PDA = al(4 * 1024).rearrange("p (k n) -> p k n", k=4)
            PNS = al(4 * 1024).rearrange("p (k n) -> p k n", k=4)
            WO = al(8 * 1024).rearrange("p (k n) -> p k n", k=8)
            load_w(PDA, "PDA", p_da, 1024, kch=4)
            load_w(PNS, "PNS", p_nsa, 1024, kch=4)
            load_w(WO, "WO", w_o, 1024, kch=8)
            gF = al(8, F32)
            dma(gF, ffn_norm.rearrange("(k p) -> p k", p=128), w=["gF"], slow=True)
            QA = [al(512) for _ in range(8)]
            for h in range(8):
                memset("pool", QA[h], 0.0, ["QA%d" % h])
            CM = [al(512) for _ in range(2)]
            gn_t = [al(24, F32) for _ in range(4)]
            cs_t = [al(64, F32) for _ in range(4)]
            ycomb = [al(512, F32) for _ in range(4)]
            imp = [al(64, F32) for _ in range(4)]
            m8 = al(16, F32)
            wk64 = al(64, F32)
            mb = al(128, F32)
            memset("pool", mb, 0.0, ["mb"])
            mbT = [al(128) for _ in range(2)]
            mbi = [0]
            ydt = al(512, F32)
            ybf = al(1024)
            yT = al(1024).rearrange("p (k n) -> p k n", k=8)
            gmt = al(1024, F32)
            m1 = al(512, F32)
            m2 = al(512, F32)
            mgb = al(1024)
            mT = al(1024).rearrange("p (k n) -> p k n", k=8)
            hs = al(1024, F32)
            cTo = al(1024).rearrange("p (k n) -> p k n", k=8)
            junk = al(1024)
            xb = [al(1024)]
            ssq = [al(1, F32)]

            if LIM == 40:
                raise _Stop()
            for c in range(NQC):
                q0 = c * 512
                kt_hi = q0 // 128 + 3
                for h in range(8):
                    dma(QA[h][0:64, :], FM[8 + h // 2, (h % 2) * 64:(h % 2) * 64 + 64, q0:q0 + 512], r=["FM"], w=["QA%d" % h])
                for nt in range(2):
                    dma(CM[nt], cmaskT[nt * 128:(nt + 1) * 128, q0:q0 + 512], w=["CM%d" % nt])
                for qt in range(4):
                    t0 = q0 + qt * 128
                    dma(gn_t[qt], GN[t0:t0 + 128, :], r=["GN"], w=["gn%d" % qt])
                    dma(cs_t[qt], csel[t0:t0 + 128, :], w=["cs%d" % qt])

                for g in range(2):
                    jobs = []
                    for hg in range(4):
                        h = g * 4 + hg
                        Ehold = {}

                        def sf0(g=g, h=h):
                            return score_tile(KCMP[g][:, 0:128], QA[h], ["KCMP%d" % g, "QA%d" % h], CM[0])

                        def sf1(g=g, h=h):
                            return score_tile(KCMP[g][:, 128:256], QA[h], ["KCMP%d" % g, "QA%d" % h], CM[1])

                        def pf0(Ee, Ehold=Ehold):
                            Ehold[0] = Ee

                        def pf1(Ee, g=g, h=h, hg=hg, Ehold=Ehold):
                            Es = [Ehold[0], Ee]
                            for qt in range(4):
                                a = ACC[qt]
                                for nt in range(2):
                                    mm(banks[a][:, 0:129], Es[nt][0][:, qt * 128:(qt + 1) * 128], VCO[:, g, nt, 0:129], nt == 0, nt == 1,
                                       [Es[nt][1], "VCO"], [bk[a]])
                                rz, rzk = smalloc()
                                ts("dve", rz, banks[a][:, 64:65], 1e-30, None, ALU.add, None, [bk[a]], [rzk])
                                recip(rz, rz, [rzk], [rzk])
                                ycs = ycomb[qt][:, h * 64:(h + 1) * 64]
                                yk = "yc%d_%d" % (qt, h)
                                ts("dve", ycs, banks[a][:, 0:64], rz, gn_t[qt][:, h * 3:h * 3 + 1], ALU.mult, ALU.mult, [bk[a], rzk, "gn%d" % qt], [yk])
                                if hg == 0:
                                    ts("dve", imp[qt], banks[a][:, 65:129], rz, None, ALU.mult, None, [bk[a], rzk], ["imp%d" % qt])
                                else:
                                    stt("dve", imp[qt], banks[a][:, 65:129], rz, imp[qt], ALU.mult, ALU.add, [bk[a], rzk, "imp%d" % qt], ["imp%d" % qt])

                        jobs.append((sf0, pf0, None))
                        jobs.append((sf1, pf1, None))
                    run_jobs(jobs)
                    if LIM == 405:
                        raise _Stop()
                    for qt in range(4):
                        tt("dve", imp[qt], imp[qt], cs_t[qt], ALU.add, ["imp%d" % qt, "cs%d" % qt], ["imp%d" % qt])
                        P.op("dve", lambda e, qt=qt: e.max(out=m8[:, 0:8], in_=imp[qt]), reads=["imp%d" % qt], writes=["m8"])
                        P.op("dve", lambda e, qt=qt: e.match_replace(out=wk64, in_to_replace=m8[:, 0:8], in_values=imp[qt], imm_value=-3e38),
                             reads=["imp%d" % qt, "m8"], writes=["wk64"])
                        P.op("dve", lambda e: e.max(out=m8[:, 8:16], in_=wk64), reads=["wk64", "m8"], writes=["m8"])
                        ts("dve", mb[:, 64:128], imp[qt], m8[:, 15:16], None, ALU.is_ge, None, ["imp%d" % qt, "m8", "mb"], ["mb"])
                        ts("dve", mb[:, 64:128], mb[:, 64:128], -1.0, -NEG, ALU.add, ALU.mult, ["mb"], ["mb"])
                        if LIM == 406:
                            raise _Stop()
                        tr(banks[6][:, 0:128], mb, identf, ["mb", "identf"], [bk[6]])
                        if LIM == 407:
                            raise _Stop()
                        mbi[0] += 1
                        cp("act", mbT[mbi[0] % 2], banks[6][:, 0:128], [bk[6]], ["mbT%d" % (mbi[0] % 2)])
                        for hg in range(4):
                            h = g * 4 + hg
                            dma(QA[h][64:128, qt * 128:(qt + 1) * 128], mbT[mbi[0] % 2][64:128, :], r=["mbT%d" % (mbi[0] % 2), "QA%d" % h], w=["QA%d" % h])
                        if LIM == 409:
                            raise _Stop()
                    if LIM == 408:
                        raise _Stop()

                if LIM == 41:
                    raise _Stop()
                jobs = []
                for h in range(8):
                    g = h // 4
                    for br in (1, 2):
                        if br == 1:
                            kts = list(range(0, kt_hi + 1))
                        else:
                            kts = [kt for kt in range(q0 // 128 - 4, kt_hi + 1) if kt >= 0]
                        for kt in kts:
                            d8 = kt - (q0 // 128 - 4)

                            def sf(h=h, g=g, br=br, kt=kt, d8=d8):
                                if br == 1:
                                    mk = MSK[:, d8, :] if d8 >= 4 else None
                                    return score_tile(KSA[g][:, kt * 128:(kt + 1) * 128], QA[h], ["KSA%d" % g, "QA%d" % h], mk)
                                return score_tile(KW[g][:, kt * 128:(kt + 1) * 128], QA[h], ["KW%d" % g, "QA%d" % h], MSK[:, d8, :])

                            def pf(Ee, h=h, g=g, br=br, kt=kt):
                                Ea, Ek = Ee
                                Vt, vk = (VS, "VS") if br == 1 else (VW, "VW")
                                for qt in range(4):
                                    qti = q0 // 128 + qt
                                    if kt > qti or (br == 2 and kt < qti - 4):
                                        continue
                                    first = 0 if br == 1 else max(0, qti - 4)
                                    a = ACC[qt]
                                    mm(banks[a][:, 0:65], Ea[:, qt * 128:(qt + 1) * 128], Vt[:, kt, g, 0:65], kt == first, kt == qti, [Ek, vk], [bk[a]])

                            def ef(h=h, br=br):
                                for qt in range(4):
                                    a = ACC[qt]
                                    rz, rzk = smalloc()
                                    recip(rz, banks[a][:, 64:65], [bk[a]], [rzk])
                                    tt("dve", rz, rz, gn_t[qt][:, h * 3 + br:h * 3 + br + 1], ALU.mult, [rzk, "gn%d" % qt], [rzk])
                                    ycs = ycomb[qt][:, h * 64:(h + 1) * 64]
                                    yk = "yc%d_%d" % (qt, h)
                                    stt("dve", ycs, banks[a][:, 0:64], rz, ycs, ALU.mult, ALU.add, [bk[a], rzk, yk], [yk])

                            jobs.append((sf, pf, ef if kt == kts[-1] else None))
                run_jobs(jobs)

                if LIM == 42:
                    raise _Stop()
                for qt in range(4):
                    t0 = q0 + qt * 128
                    ykeys = ["yc%d_%d" % (qt, h) for h in range(8)]
                    dma(ydt, YDA[t0:t0 + 128, :], r=["YDA"], w=["ydt"])
                    if dbg:
                        dma(dbg_out["d_yy"][t0:t0 + 128, 0:512], ydt, r=["ydt"], w=["d_yy"])
                        dma(dbg_out["d_yy"][t0:t0 + 128, 512:1024], ycomb[qt], r=ykeys, w=["d_yy"])
                    cp("dve", ybf[:, 0:512], ydt, ["ydt", "ybf"], ["ybf"])
                    cp("act", ybf[:, 512:1024], ycomb[qt], ykeys + ["ybf"], ["ybf"])
                    pv = banks[6][:, :].bitcast(BF16)
                    for k in range(8):
                        tr(pv[:, k * 128:(k + 1) * 128], ybf[:, k * 128:(k + 1) * 128], ident, ["ybf", "ident"], [bk[6]])
                    cp("act", yT, pv.rearrange("p (k n) -> p k n", k=8), [bk[6]], ["yT"])
                    dma(hs, x[t0:t0 + 128, :], w=["hs"])
                    for half in range(2):
                        cs = slice(half * 512, (half + 1) * 512)
                        dma(gmt[:, 0:512], GM[t0:t0 + 128, half * 512:(half + 1) * 512], r=["GM", "gmt"], w=["gmt"])
                        dma(gmt[:, 512:1024], GM[t0:t0 + 128, 1024 + half * 512:1024 + (half + 1) * 512], r=["GM", "gmt"], w=["gmt"])
                        for k in range(4):
                            mm(banks[0][:, :], yT[:, k, :], PDA[:, k, cs], k == 0, k == 3, ["yT", "PDA"], [bk[0]])
                        for k in range(4):
                            mm(banks[1][:, :], yT[:, 4 + k, :], PNS[:, k, cs], k == 0, k == 3, ["yT", "PNS"], [bk[1]])
                        tt("dve", m1, banks[0][:, :], gmt[:, 0:512], ALU.mult, [bk[0], "gmt"], ["m1"])
                        tt("dve", m2, banks[1][:, :], gmt[:, 512:1024], ALU.mult, [bk[1], "gmt"], ["m2"])
                        tt("dve", mgb[:, cs], m1, m2, ALU.add, ["m1", "m2", "mgb"], ["mgb"])
                    pv7 = banks[7][:, :].bitcast(BF16)
                    for k in range(8):
                        tr(pv7[:, k * 128:(k + 1) * 128], mgb[:, k * 128:(k + 1) * 128], ident, ["mgb", "ident"], [bk[7]])
                    cp("act", mT, pv7.rearrange("p (k n) -> p k n", k=8), [bk[7]], ["mT"])
                    for half in range(2):
                        cs = slice(half * 512, (half + 1) * 512)
                        for k in range(8):
                            mm(banks[half][:, :], mT[:, k, :], WO[:, k, cs], k == 0, k == 7, ["mT", "WO"], [bk[half]])
                        tt("dve", hs[:, cs], banks[half][:, :], hs[:, cs], ALU.add, [bk[half], "hs"], ["hs"])
                    dma(H[t0:t0 + 128, :], hs, r=["hs"], w=["H"])
                    if dbg:
                        dma(dbg_out["d_h"][t0:t0 + 128, :], hs, r=["hs"], w=["d_h"])
                    rms_to_T(hs, "hs", gF, "gF", cTo, "cTo", 0, 6)
                    dma(CT[:, :, t0:t0 + 128], cTo, r=["cTo"], w=["CT"])
            P.barrier()
            if LIM == 5:
                raise _Stop()
            off[0] = PERSIST

            WQ = al(8 * 2048).rearrange("p (k n) -> p k n", k=8)
            load_w(WQ, "WQ", wq, 2048)
            K1 = al(128)
            K2 = al(128)
            for dst, src, kk in ((K1, k1T, "K1"), (K2, k2T, "K2")):
                sv, sk, e_ = load_cast(None, src, 128, [128])
                cp(e_, dst, sv, [sk], [kk])
            KK = [K1, K2]
            V16K = ["v16a_%d" % u for u in range(16)] + ["v16b_%d" % u for u in range(16)]
            C16K = ["c16a_%d" % u for u in range(8)] + ["c16b_%d" % u for u in range(8)]
            P.shared = set(["WQ", "K1", "K2", "ident", "identf", "CT", "QTs", "SVs"])
            pp = []
            for par in range(2):
                d_ = {}
                d_["cT"] = al(8 * TB).rearrange("p (k n) -> p k n", k=8)
                d_["qT"] = al(16 * TB).rearrange("p (u n) -> p u n", u=16)
                d_["sc"] = al(2048, F32).rearrange("p (u n) -> p u n", u=16)
                d_["wk"] = [al(128, F32) for _ in range(16)]
                d_["v16"] = al(256, F32).rearrange("p (h s a) -> p h s a", h=8, s=2)
                d_["cand"] = al(2048, F32).rearrange("p (h a b) -> p h a b", h=8, a=16)
                d_["wk2"] = [al(256, F32) for _ in range(8)]
                d_["c16"] = al(128, F32).rearrange("p (h a) -> p h a", h=8)
                d_["ex16"] = al(128, F32).rearrange("p (h a) -> p h a", h=8)
                d_["z8"] = al(8, F32)
                d_["ml8"] = al(8, F32)
                d_["A1"] = al(128, F32)
                d_["A2"] = al(128, F32)
                d_["A3"] = al(128, F32)
                d_["v1T"] = al(TB, F32)
                d_["t2T"] = al(TB, F32)
                d_["bT"] = al(TB, F32)
                pp.append(d_)
            for b in range(NB):
                tb0 = b * TB
                P.suffix = "_p%d" % (b % 2)
                d_ = pp[b % 2]
                cT, qT, sc, wk, v16, cand, wk2, c16, ex16, z8, ml8, A1, A2, A3, v1T, t2T, bT = (
                    d_["cT"], d_["qT"], d_["sc"], d_["wk"], d_["v16"], d_["cand"], d_["wk2"], d_["c16"], d_["ex16"], d_["z8"], d_["ml8"],
                    d_["A1"], d_["A2"], d_["A3"], d_["v1T"], d_["t2T"], d_["bT"])
                dma(cT, CT[:, :, tb0:tb0 + TB], r=["CT"], w=["cT"])
                for u in range(16):
                    pb = 4 + (u % 4)
                    for kc in range(8):
                        mm(banks[pb][:, 0:TB], WQ[:, kc, u * 128:(u + 1) * 128], cT[:, kc, :], kc == 0, kc == 7, ["WQ", "cT"], [bk[pb]])
                    cp("act", qT[:, u, :], banks[pb][:, 0:TB], [bk[pb], "qT"], ["qT"])
                for t2 in range(TB // 128):
                    tl = slice(t2 * 128, (t2 + 1) * 128)
                    for u in range(16):
                        pb = u // 4
                        mm(banks[pb][:, (u % 4) * 128:(u % 4 + 1) * 128], qT[:, u, tl], KK[u % 2], True, True, ["qT", "K1", "K2"], [bk[pb]])
                    for pb in range(4):
                        cp("act", sc[:, pb * 4:(pb + 1) * 4, :], banks[pb][:, :].rearrange("p (u n) -> p u n", u=4), [bk[pb], "sc"], ["sc"])
                    for u0 in range(0, 16, 16):
                        for u in range(u0, u0 + 16):
                            hh, ss_ = u // 2, u % 2
                            P.op("dve", lambda e, u=u, hh=hh, ss_=ss_, v16=v16, sc=sc, wk=wk, c16=c16, wk2=wk2, z8=z8, ex16=ex16: e.max(out=v16[:, hh, ss_, 0:8], in_=sc[:, u, :]), reads=["sc"], writes=["v16a_%d" % u])
                        for u in range(u0, u0 + 16):
                            hh, ss_ = u // 2, u % 2
                            P.op("dve", lambda e, u=u, hh=hh, ss_=ss_, v16=v16, sc=sc, wk=wk, c16=c16, wk2=wk2, z8=z8, ex16=ex16: e.match_replace(out=wk[u % 16], in_to_replace=v16[:, hh, ss_, 0:8], in_values=sc[:, u, :], imm_value=-3e38),
                                 reads=["sc", "v16a_%d" % u], writes=["wk%d" % (u % 16)])
                        for u in range(u0, u0 + 16):
                            hh, ss_ = u // 2, u % 2
                            P.op("dve", lambda e, u=u, hh=hh, ss_=ss_, v16=v16, sc=sc, wk=wk, c16=c16, wk2=wk2, z8=z8, ex16=ex16: e.max(out=v16[:, hh, ss_, 8:16], in_=wk[u % 16]), reads=["wk%d" % (u % 16)], writes=["v16b_%d" % u])
                    tt("dve", cand, v16[:, :, 0, :].unsqueeze(3).to_broadcast([128, 8, 16, 16]), v16[:, :, 1, :].unsqueeze(2).to_broadcast([128, 8, 16, 16]),
                       ALU.add, V16K, ["cand"])
                    for h0 in range(0, 8, 8):
                        for hh in range(h0, h0 + 8):
                            cv = cand[:, hh, :, :].rearrange("p a b -> p (a b)")
                            P.op("dve", lambda e, hh=hh, cv=cv, v16=v16, sc=sc, wk=wk, c16=c16, wk2=wk2, z8=z8, ex16=ex16: e.max(out=c16[:, hh, 0:8], in_=cv), reads=["cand"], writes=["c16a_%d" % hh])
                        for hh in range(h0, h0 + 8):
                            cv = cand[:, hh, :, :].rearrange("p a b -> p (a b)")
                            P.op("dve", lambda e, hh=hh, cv=cv, v16=v16, sc=sc, wk=wk, c16=c16, wk2=wk2, z8=z8, ex16=ex16: e.match_replace(out=wk2[hh % 8], in_to_replace=c16[:, hh, 0:8], in_values=cv, imm_value=-3e38),
                                 reads=["cand", "c16a_%d" % hh], writes=["wk2_%d" % (hh % 8)])
                        for hh in range(h0, h0 + 8):
                            P.op("dve", lambda e, hh=hh, v16=v16, sc=sc, wk=wk, c16=c16, wk2=wk2, z8=z8, ex16=ex16: e.max(out=c16[:, hh, 8:16], in_=wk2[hh % 8]), reads=["wk2_%d" % (hh % 8)], writes=["c16b_%d" % hh])
                    tt("dve", ex16, c16, c16[:, :, 0:1].to_broadcast([128, 8, 16]), ALU.subtract, C16K, ["ex16"])
                    act(ex16, ex16, AF.Exp, ["ex16"], ["ex16"])
                    P.op("dve", lambda e, v16=v16, sc=sc, wk=wk, c16=c16, wk2=wk2, z8=z8, ex16=ex16: e.reduce_sum(out=z8, in_=ex16, axis=AX.X), reads=["ex16"], writes=["z8"])
                    act(ml8, z8, AF.Ln, ["z8"], ["ml8"])
                    tt("dve", ml8, ml8, c16[:, :, 0], ALU.add, ["ml8"] + C16K, ["ml8"])
                    cp("dve", A1.rearrange("p (h a) -> p h a", h=8), v16[:, :, 0, :], V16K, ["A1"])
                    cp("dve", A2.rearrange("p (h a) -> p h a", h=8), c16[:, :, 15:16].to_broadcast([128, 8, 16]), C16K, ["A2"])
                    tt("dve", A3.rearrange("p (h a) -> p h a", h=8), v16[:, :, 0, :], ml8.unsqueeze(2).to_broadcast([128, 8, 16]), ALU.subtract, V16K + ["ml8"], ["A3"])
                    for Ax, ak, Tx, kx, px in ((A1, "A1", v1T, "v1T", 4), (A2, "A2", t2T, "t2T", 5), (A3, "A3", bT, "bT", 6)):
                        tr(banks[px][:, 0:128], Ax, identf, [ak, "identf"], [bk[px]])
                        cp("act", Tx[:, tl], banks[px][:, 0:128], [bk[px], kx], [kx])
                dma(QTs[b], qT.rearrange("p u n -> p (u n)"), r=["qT"], w=["QTs"])
                dma(SVs[b, 0], v1T, r=["v1T"], w=["SVs"])
                dma(SVs[b, 1], t2T, r=["t2T"], w=["SVs"])
                dma(SVs[b, 2], bT, r=["bT"], w=["SVs"])
            P.suffix = None
            P.barrier()
            if LIM == 6:
                raise _Stop()
            off[0] = PERSIST
            K1 = al(128)
            K2 = al(128)
            for dst, src, kk in ((K1, k1T, "K1"), (K2, k2T, "K2")):
                sv, sk, e_ = load_cast(None, src, 128, [128])
                cp(e_, dst, sv, [sk], [kk])
            gFin = al(1024, F32)
            dma(gFin, final_norm.partition_broadcast(128), w=["gFin"])
            cT = al(8 * TB).rearrange("p (k n) -> p k n", k=8)
            qT = al(16 * TB).rearrange("p (u n) -> p u n", u=16)
            v1T = al(TB, F32)
            t2T = al(TB, F32)
            bT = al(TB, F32)
            Gb = al(TB * 128).rearrange("p (t j) -> p t j", t=TB)
            hfin = [al(1024, F32) for _ in range(1)]
            junk = al(1024)
            ssq = [al(1, F32)]
            dbgb = al(1024, F32) if dbg else None
            GRP = 8
            Qrep = [[al(GRP * 128) for _ in range(2)] for _ in range(2)]
            Ma4 = [al(512) for _ in range(4)]
            Rr4 = [al(512) for _ in range(4)]
            Dd4 = [al(512, F32) for _ in range(3)]
            Da4 = [al(512, F32) for _ in range(3)]
            Mk4 = [al(512) for _ in range(3)]
            Ex4 = [al(512) for _ in range(3)]
            UTs = [al(1024).rearrange("p (k i) -> p k i", k=8) for _ in range(4)]
            VBs = [al(1024) for _ in range(4)]
            Hg = [al(TB, F32) for _ in range(3)]
            GH = [al(TB) for _ in range(3)]

            def gloads(b_):
                dma(qT.rearrange("p u n -> p (u n)"), QTs[b_], r=["QTs"], w=["qT"])
                dma(v1T, SVs[b_, 0], r=["SVs"], w=["v1T"])
                dma(t2T, SVs[b_, 1], r=["SVs"], w=["t2T"])
                dma(bT, SVs[b_, 2], r=["SVs"], w=["bT"])

            gloads(0)
            for b in range(NB):
                tb0 = b * TB
                dma(cT, CT[:, :, tb0:tb0 + TB], r=["CT"], w=["cT"])
                if LIM == 51:
                    raise _Stop()
                R1B = [0, 1, 6]
                R2B = [2, 3, 7]
                NBT = TB // 4

                def rep(b_):
                    t0_ = b_ * 4
                    gi = t0_ // GRP
                    qb = gi % 2
                    if t0_ % GRP == 0:
                        for side in range(2):
                            src = qT[:, side:16:2, gi * GRP:(gi + 1) * GRP].rearrange("p h t -> p t h").unsqueeze(3).to_broadcast([128, GRP, 8, 16])
                            dstq = Qrep[qb][side].rearrange("p (t h a) -> p t h a", t=GRP, h=8)
                            act(dstq, src, AF.Copy, ["qT"], ["Qrep%d%d" % (qb, side)])
                    r1, r2 = R1B[b_ % 3], R2B[b_ % 3]
                    for ti in range(4):
                        tq = (t0_ % GRP) + ti
                        mm(banks[r1][:, ti * 128:(ti + 1) * 128], Qrep[qb][0][:, tq * 128:(tq + 1) * 128], K1, True, True, ["Qrep%d0" % qb, "K1"], [bk[r1]])
                        mm(banks[r2][:, ti * 128:(ti + 1) * 128], Qrep[qb][1][:, tq * 128:(tq + 1) * 128], K2, True, True, ["Qrep%d1" % qb, "K2"], [bk[r2]])

                def mid(b_):
                    t0_ = b_ * 4
                    r1, r2 = R1B[b_ % 3], R2B[b_ % 3]
                    i4, i3 = b_ % 4, b_ % 3
                    v3 = lambda a_: a_.rearrange("p (t n) -> p t n", t=4)
                    bc = lambda a_: a_[:, t0_:t0_ + 4].unsqueeze(2).to_broadcast([128, 4, 128])
                    tt("dve", v3(Da4[i3]), v3(banks[r2][:, :]), bc(bT), ALU.add, [bk[r2], "bT"], ["Da4_%d" % i3])
                    tt("dve", v3(Dd4[i3]), v3(banks[r2][:, :]), bc(v1T), ALU.add, [bk[r2], "v1T"], ["Dd4_%d" % i3])
                    act(Ex4[i3], Da4[i3], AF.Exp, ["Da4_%d" % i3], ["Ex4_%d" % i3])
                    tt("dve", v3(Ma4[i4]), v3(banks[r1][:, :]), bc(v1T), ALU.is_equal, [bk[r1], "v1T"], ["Ma4_%d" % i4])
                    tt("dve", v3(Mk4[i3]), v3(Dd4[i3]), bc(t2T), ALU.is_ge, ["Dd4_%d" % i3, "t2T"], ["Mk4_%d" % i3])
                    tt("pool", Rr4[i4], Mk4[i3], Ex4[i3], ALU.mult, ["Mk4_%d" % i3, "Ex4_%d" % i3], ["Rr4_%d" % i4])

                def gmm(b_):
                    t0_ = b_ * 4
                    i4 = b_ % 4
                    pg = 4 + b_ % 2
                    for ti in range(4):
                        mm(banks[pg][:, ti * 128:(ti + 1) * 128], Ma4[i4][:, ti * 128:(ti + 1) * 128], Rr4[i4][:, ti * 128:(ti + 1) * 128], True, True,
                           ["Ma4_%d" % i4, "Rr4_%d" % i4], [bk[pg]])
                    cp("act", Gb[:, t0_:t0_ + 4, :], banks[pg][:, :].rearrange("p (t j) -> p t j", t=4), [bk[pg], "Gb"], ["Gb"])

                rep(0)
                rep(1)
                rep(2)
                GDEL = 2
                for b_ in range(NBT):
                    mid(b_)
                    if b_ + 3 < NBT:
                        rep(b_ + 3)
                    if b_ - GDEL >= 0:
                        gmm(b_ - GDEL)
                for b_ in range(max(0, NBT - GDEL), NBT):
                    gmm(b_)
                if LIM == 52:
                    raise _Stop()
                if b + 1 < NB:
                    gloads(b + 1)

                def loads(j):
                    ub = j % 4
                    dma(UTs[ub], UT[j].rearrange("p (k i) -> p k i", k=8), r=["UT"], w=["UTs%d" % ub], q="sp")
                    dma(VBs[ub], VB[j], r=["VB"], w=["VBs%d" % ub], q="sp")

                def umm(j):
                    ub = j % 4
                    ph = 4 + (j % 3)
                    for kc in range(8):
                        mm(banks[ph][:, 0:TB], UTs[ub][:, kc, :], cT[:, kc, :], kc == 0, kc == 7, ["UTs%d" % ub, "cT"], [bk[ph]])

                def rest(j):
                    ub = j % 4
                    ph = 4 + (j % 3)
                    hb_ = j % 3
                    act(Hg[hb_], banks[ph][:, 0:TB], AF.Gelu, [bk[ph]], ["Hg%d" % hb_])
                    gb_ = j % 3
                    tt("dve", GH[gb_], Hg[hb_], Gb[:, :, j], ALU.mult, ["Hg%d" % hb_, "Gb"], ["GH%d" % gb_])
                    for t2 in range(TB // 128):
                        for half in range(2):
                            pb = t2 * 2 + half
                            mm(banks[pb][:, :], GH[gb_][:, t2 * 128:(t2 + 1) * 128], VBs[ub][:, half * 512:(half + 1) * 512], j == 0, j == 127,
                               ["GH%d" % gb_, "VBs%d" % ub], [bk[pb]])

                loads(0)
                loads(1)
                loads(2)
                umm(0)
                umm(1)
                for j in range(128):
                    if j + 3 < 128:
                        loads(j + 3)
                    if j + 2 < 128:
                        umm(j + 2)
                    rest(j)
                if LIM == 53:
                    raise _Stop()
                for t2 in range(TB // 128):
                    t0 = tb0 + t2 * 128
                    hf = hfin[0]
                    hk = "hfin0"
                    dma(hf, H[t0:t0 + 128, :], r=["H"], w=[hk])
                    if dbg:
                        for half in range(2):
                            cp("dve", dbgb[:, half * 512:(half + 1) * 512], banks[t2 * 2 + half][:, :], [bk[t2 * 2 + half], "dbgb"], ["dbgb"])
                        dma(dbg_out["d_pe"][t0:t0 + 128, :], dbgb, r=["dbgb"], w=["d_pe"])
                    for half in range(2):
                        cs = slice(half * 512, (half + 1) * 512)
                        tt("dve", hf[:, cs], banks[t2 * 2 + half][:, :], hf[:, cs], ALU.add, [bk[t2 * 2 + half], hk], [hk])
                    act(junk, hf, AF.Square, [hk], ["junk", "ssq0"], scale=1.0 / 32.0, accum=ssq[0])
                    act(ssq[0], ssq[0], AF.Sqrt, ["ssq0"], ["ssq0"], bias=1e-6, scale=1.0)
                    recip(ssq[0], ssq[0], ["ssq0"], ["ssq0"])
                    stt("dve", hf, hf, ssq[0], gFin, ALU.mult, ALU.mult, [hk, "ssq0", "gFin"], [hk])
                    fin_ops.append(dma(y[t0:t0 + 128, :], hf, r=[hk], w=["y"], q="sp"))
        except _Stop:
            pass
        P.emit(es, fin_ops)
    return nc


fin_ops = []


def host_consts(S):
    bf = ml_dtypes.bfloat16
    pos = np.arange(S, dtype=np.float32)
    inv = np.power(np.float32(500000.0), -np.arange(8, dtype=np.float32) * 2.0 / 16.0).astype(np.float32)
    ang = pos[None, :] * inv[:, None]
    cosv, sinv = np.cos(ang).astype(np.float32), np.sin(ang).astype(np.float32)
    ropec = np.ones((128, S), np.float32)
    ropes = np.zeros((128, S), np.float32)
    for hb in (0, 64):
        ropec[hb:hb + 8] = cosv
        ropec[hb + 8:hb + 16] = cosv
        ropes[hb:hb + 8] = -sinv
        ropes[hb + 8:hb + 16] = sinv
    p = np.arange(128)[:, None]
    f = np.arange(512)[None, :]
    masks = np.zeros((8, 128, 512), np.float32)
    for d in range(4):
        masks[d] = np.where(128 * d + p > f, 0.0, NEG)
        masks[4 + d] = np.where(128 * d + p <= f, 0.0, NEG)
    n = np.arange(256)[:, None]
    t = np.arange(S)[None, :]
    cmaskT = np.where((16 * n + 31 <= t) & (n < (S - 32) // 16 + 1), 0.0, NEG)
    esel = (np.arange(64)[:, None] == (np.arange(S)[None, :] // 64)).astype(np.float32)
    tq = np.arange(S)[:, None]
    j = np.arange(64)[None, :]
    cur = tq // 64
    forced = (j == 0) | (j == cur) | (j == cur - 1)
    valid = (j <= cur) & (j < S // 64)
    csel = np.where(valid, np.where(forced, 1e4, 0.0), -1e30).astype(np.float32)
    ci = np.arange(256)[:, None] * 16
    sj = np.arange(64)[None, :] * 64
    ovl = ((ci < sj + 64) & (ci + 32 > sj)).astype(np.float32)
    ovl[(S - 32) // 16 + 1:] = 0.0
    return dict(ropec=ropec, ropes=ropes, masks=masks.astype(bf), cmaskT=cmaskT.astype(bf), esel=esel.astype(bf),
                csel=csel, ovl=ovl.astype(bf))


def host_weights(inp):
    f = lambda a: np.ascontiguousarray(np.asarray(a, dtype=np.float32))
    w_in = f(inp["w_in"])[0]
    roped_cols = np.concatenate([np.arange(c0, c0 + 128) for c0 in FM_COLS[:N_ROPED]])
    perm = np.arange(128)
    for hb in (0, 64):
        perm[hb:hb + 8] = np.arange(hb + 8, hb + 16)
        perm[hb + 8:hb + 16] = np.arange(hb, hb + 8)
    rot_cols = np.concatenate([roped_cols[i * 128:(i + 1) * 128][perm] for i in range(N_ROPED)])
    w = dict(
        attn_norm=f(inp["attn_norm"])[0], w_in=w_in, w_rot=np.ascontiguousarray(w_in[:, rot_cols]),
        lam4=np.stack([f(inp["da_lambda_q1"])[0], f(inp["da_lambda_k1"])[0], f(inp["da_lambda_q2"])[0], f(inp["da_lambda_k2"])[0]]),
        subln=f(inp["da_subln"])[0],
        peT_k=np.ascontiguousarray(f(inp["cmp_pe_k"])[0].T), peT_v=np.ascontiguousarray(f(inp["cmp_pe_v"])[0].T),
        w1k=f(inp["cmp_w1_k"])[0], w1v=f(inp["cmp_w1_v"])[0], w2k=f(inp["cmp_w2_k"])[0], w2v=f(inp["cmp_w2_v"])[0],
        p_da=f(inp["p_da"])[0], p_nsa=f(inp["p_nsa"])[0], w_o=f(inp["w_o"])[0], ffn_norm=f(inp["ffn_norm"])[0],
        wq=f(inp["peer_wq"])[0], k1T=np.ascontiguousarray(f(inp["peer_k1"])[0].T), k2T=np.ascontiguousarray(f(inp["peer_k2"])[0].T),
        peer_u=f(inp["peer_u"])[0], peer_v=f(inp["peer_v"])[0], final_norm=f(inp["final_norm"]),
    )
    return w


def run(inputs, S, dbg=False, cores=None, LIM=99):
    del fin_ops[:]
    xin = np.asarray(inputs["x"], dtype=np.float32)
    B = xin.shape[0]
    nc = build(S, dbg, LIM)
    shared = host_weights(inputs)
    shared.update(host_consts(S))
    in_maps = []
    for b in range(B):
        m = dict(shared)
        m["x"] = np.ascontiguousarray(xin[b])
        in_maps.append(m)
    res = run_bass_kernel_spmd(nc, in_maps, core_ids=list(range(B)))
    return res


def kernel(**inputs):
    S = inputs["x"].shape[1]
    res = run(inputs, S)
    return np.stack([np.asarray(r["y"], dtype=np.float32) for r in res.results], axis=0)
```

```python
import contextlib
import math
import numpy as np
import ml_dtypes
import concourse.bass as bass
import concourse.mybir as mybir
from concourse.bass_utils import run_bass_kernel_spmd

F32 = mybir.dt.float32
BF16 = mybir.dt.bfloat16
ALU = mybir.AluOpType
AF = mybir.ActivationFunctionType
AX = mybir.AxisListType

D = 1024
NEG = -30000.0


class _Op:
    __slots__ = ("eng", "fn", "deps", "marked", "val", "is_dma", "sem_i")

    def __init__(self, eng, fn, is_dma):
        self.eng = eng
        self.fn = fn
        self.deps = []
        self.marked = False
        self.val = None
        self.is_dma = is_dma
        self.sem_i = None


class Prog:
    COMPUTE = ("pe", "act", "dve", "pool")
    NDMA_SEM = 8

    def __init__(self, nc):
        self.nc = nc
        self.ops = {e: [] for e in ("pe", "act", "dve", "pool", "sp")}
        self.state = {}
        self.bar = {e: [] for e in self.ops}
        self.dmas_since_bar = []

    suffix = None
    shared = ()

    def _k(self, k):
        if self.suffix is None or k in self.shared or k.startswith("bk") or k.startswith("stg"):
            return k
        return k + self.suffix

    def _add(self, eng, fn, reads, writes, is_dma):
        if self.suffix is not None:
            reads = [self._k(k) for k in reads]
            writes = [self._k(k) for k in writes]
        op = _Op(eng, fn, is_dma)
        deps = list(self.bar[eng])
        self.bar[eng] = []
        for k in reads:
            st = self.state.get(k)
            if st is not None and st[0] is not None:
                deps.append(st[0])
        for k in writes:
            st = self.state.get(k)
            if st is not None:
                if st[0] is not None:
                    deps.append(st[0])
                deps.extend(st[1].values())
                deps.extend(st[2])
        seen = set()
        for d in deps:
            if id(d) in seen:
                continue
            seen.add(id(d))
            if d.eng == "pe" and eng == "pe" and not d.is_dma and not is_dma:
                continue
            op.deps.append(d)
            d.marked = True
        for k in reads:
            st = self.state.setdefault(k, [None, {}, []])
            if is_dma:
                st[2].append(op)
            else:
                st[1][eng] = op
        for k in writes:
            self.state[k] = [op, {}, []]
        self.ops[eng].append(op)
        if is_dma:
            self.dmas_since_bar.append(op)
        return op

    def op(self, eng, fn, reads=(), writes=()):
        return self._add(eng, fn, reads, writes, False)

    def dma(self, eng, fn, reads=(), writes=()):
        op = self._add(eng, fn, reads, writes, True)
        op.marked = True
        return op

    def barrier(self):
        deps = []
        for e, lst in self.ops.items():
            for o in reversed(lst):
                if not o.is_dma:
                    deps.append(o)
                    break
        deps.extend(self.dmas_since_bar)
        self.dmas_since_bar = []
        for e in self.bar:
            self.bar[e] = self.bar[e] + deps
        self.state = {}

    def emit(self, es, final_ops):
        nc = self.nc
        csem = {e: es.enter_context(nc.semaphore("s_" + e)) for e in self.COMPUTE}
        dsem = {e: [es.enter_context(nc.semaphore("d_%s%d" % (e, i))) for i in range(self.NDMA_SEM)]
                for e in ("sp", "pool")}
        for e, lst in self.ops.items():
            cnt = 0
            dcnt = 0
            for o in lst:
                if o.is_dma:
                    o.sem_i = dcnt % self.NDMA_SEM
                    o.val = 16 * (dcnt // self.NDMA_SEM + 1)
                    dcnt += 1
                elif o.marked:
                    cnt += 1
                    o.val = cnt
        block = es.enter_context(nc.Block())

        def run(ename, e):
            waited = {}
            for o in self.ops[ename]:
                waits = {}
                for d in o.deps:
                    if d.is_dma:
                        key = (d.eng, d.sem_i)
                        sem = dsem[d.eng][d.sem_i]
                    else:
                        key = (d.eng, None)
                        sem = csem[d.eng]
                    if waited.get(key, 0) >= d.val:
                        continue
                    if key not in waits or waits[key][1] < d.val:
                        waits[key] = (sem, d.val)
                if o.is_dma and o.val > 16:
                    key = (ename, o.sem_i)
                    if waited.get(key, 0) < o.val - 16:
                        if key not in waits or waits[key][1] < o.val - 16:
                            waits[key] = (dsem[ename][o.sem_i], o.val - 16)
                for key, (sem, v) in waits.items():
                    e.wait_ge(sem, v)
                    waited[key] = v
                ins = o.fn(e)
                if o.is_dma:
                    ins.then_inc(dsem[ename][o.sem_i], 16)
                elif o.marked:
                    ins.then_inc(csem[ename], 1)
            if ename == "sp":
                for o in final_ops:
                    e.wait_ge(dsem[o.eng][o.sem_i], o.val)

        @block.sync
        def _(e):
            run("sp", e)

        @block.tensor
        def _(e):
            run("pe", e)

        @block.scalar
        def _(e):
            run("act", e)

        @block.vector
        def _(e):
            run("dve", e)

        @block.gpsimd
        def _(e):
            run("pool", e)


C_QDA, C_KDA, C_VDA, C_QN, C_KC, C_VC, C_KS, C_VS, C_KW, C_VW, C_GN, C_GM = (
    0, 512, 1024, 1536, 2048, 2176, 2304, 2432, 2560, 2688, 2816, 2840)
FM_COLS = [C_QDA + 128 * i for i in range(4)] + [C_KDA + 128 * i for i in range(4)] + \
          [C_QN + 128 * i for i in range(4)] + [C_KC, C_KS, C_KW, C_VC]
N_ROPED = 15
LAMBDA_INIT = 0.8 - 0.6 * math.exp(0.0)


class _Stop(Exception):
    pass


def build(S, dbg=False, LIM=99):
    NT = S // 128
    NQC = S // 512
    NCMP = (S - 32) // 16 + 1
    TB = 256
    NB = S // TB
    nc = bass.Bass("TRN2", target_bir_lowering=False)

    def din(name, shape, dt=F32):
        return nc.dram_tensor(name, list(shape), dt, kind="ExternalInput").ap()

    def dscr(name, shape, dt):
        return nc.dram_tensor(name, list(shape), dt, kind="Internal").ap()

    x = din("x", [S, D])
    attn_norm = din("attn_norm", [D])
    w_in = din("w_in", [D, 4888])
    w_rot = din("w_rot", [D, 1920])
    lam4 = din("lam4", [4, 64])
    subln = din("subln", [128])
    peT_k = din("peT_k", [64, 32])
    peT_v = din("peT_v", [64, 32])
    w1k = din("w1k", [2048, 64])
    w1v = din("w1v", [2048, 64])
    w2k = din("w2k", [64, 64])
    w2v = din("w2v", [64, 64])
    p_da = din("p_da", [512, D])
    p_nsa = din("p_nsa", [512, D])
    w_o = din("w_o", [D, D])
    ffn_norm = din("ffn_norm", [D])
    wq = din("wq", [D, 2048])
    k1T = din("k1T", [128, 128])
    k2T = din("k2T", [128, 128])
    peer_u = din("peer_u", [16384, D])
    peer_v = din("peer_v", [16384, D])
    final_norm = din("final_norm", [D])
    ropec = din("ropec", [128, S])
    ropes = din("ropes", [128, S])
    masks = din("masks", [8, 128, 512], BF16)
    cmaskT = din("cmaskT", [256, S], BF16)
    esel = din("esel", [64, S], BF16)
    csel = din("csel", [S, 64])
    ovl = din("ovl", [256, 64], BF16)
    y = nc.dram_tensor("y", [S, D], F32, kind="ExternalOutput").ap()

    FM = dscr("FM", [16, 128, S], BF16)
    VT = dscr("VT", [S, 768], BF16)
    GN = dscr("GN", [S, 24], F32)
    GM = dscr("GM", [S, 2048], F32)
    H = dscr("H", [S, D], F32)
    CT = dscr("CT", [128, 8, S], BF16)
    YDA = dscr("YDA", [S, 512], F32)
    QTs = dscr("QTs", [S // 256, 128, 16 * 256], BF16)
    SVs = dscr("SVs", [S // 256, 3, 128, 256], F32)
    UT = dscr("UT", [128, 128, 1024], BF16)
    VB = dscr("VB", [128, 128, 1024], BF16)
    dbg_out = {}
    if dbg:
        dbg_out["d_fm"] = nc.dram_tensor("d_fm", [16, 128, S], BF16, kind="ExternalOutput").ap()
        dbg_out["d_h"] = nc.dram_tensor("d_h", [S, D], F32, kind="ExternalOutput").ap()
        dbg_out["d_yy"] = nc.dram_tensor("d_yy", [S, 1024], F32, kind="ExternalOutput").ap()
        dbg_out["d_pe"] = nc.dram_tensor("d_pe", [S, D], F32, kind="ExternalOutput").ap()

    es = contextlib.ExitStack()
    with es:
        ARENA_N = 104000
        arena = es.enter_context(nc.sbuf_tensor("arena", [128, ARENA_N], BF16))
        banks = [es.enter_context(nc.psum_tensor("bk%d" % i, [128, 512], F32)) for i in range(8)]
        bk = ["bk%d" % i for i in range(8)]
        off = [0]

        def al(n, dt=BF16):
            k = n * (2 if dt == F32 else 1)
            assert off[0] + k <= ARENA_N, ("arena overflow", off[0], k)
            a = arena[:, off[0]:off[0] + k]
            off[0] += (k + 15) // 16 * 16
            return a.bitcast(dt) if dt != BF16 else a

        P = Prog(nc)
        dq = ["sp", "pool"]
        dqi = [0]

        def dma(out, in_, r=(), w=(), q=None, slow=False):
            if q is None:
                q = dq[dqi[0] % 2]
                dqi[0] += 1
            if slow:
                return P.dma(q, lambda e: e.dma_start(out=out, in_=in_, allow_slow_non_contiguous=True), reads=r, writes=w)
            return P.dma(q, lambda e: e.dma_start(out=out, in_=in_), reads=r, writes=w)

        def mm(out, lhsT, rhs, st, sp_, r, w):
            return P.op("pe", lambda e: e.matmul(out, lhsT=lhsT, rhs=rhs, start=st, stop=sp_), reads=r, writes=w)

        def tr(out, in_, ident_, r, w):
            return P.op("pe", lambda e: e.transpose(out=out, in_=in_, identity=ident_), reads=r, writes=w)

        def act(out, in_, func, r, w, bias=None, scale=None, accum=None):
            kw = {}
            if bias is not None:
                kw["bias"] = bias
            if scale is not None:
                kw["scale"] = scale
            if accum is not None:
                kw["accum_out"] = accum
            return P.op("act", lambda e: e.activation(out=out, in_=in_, func=func, **kw), reads=r, writes=w)

        def ts(eng, out, in0, s1, s2, op0, op1, r, w):
            if op1 is None:
                return P.op(eng, lambda e: e.tensor_scalar(out=out, in0=in0, scalar1=s1, scalar2=None, op0=op0), reads=r, writes=w)
            return P.op(eng, lambda e: e.tensor_scalar(out=out, in0=in0, scalar1=s1, scalar2=s2, op0=op0, op1=op1), reads=r, writes=w)

        def tt(eng, out, in0, in1, op, r, w):
            return P.op(eng, lambda e: e.tensor_tensor(out=out, in0=in0, in1=in1, op=op), reads=r, writes=w)

        def stt(eng, out, in0, scalar, in1, op0, op1, r, w):
            return P.op(eng, lambda e: e.scalar_tensor_tensor(out=out, in0=in0, scalar=scalar, in1=in1, op0=op0, op1=op1), reads=r, writes=w)

        def cp(eng, out, in_, r, w):
            if eng == "act":
                return act(out, in_, AF.Copy, r, w)
            return P.op(eng, lambda e: e.tensor_copy(out=out, in_=in_), reads=r, writes=w)

        def recip(out, in_, r, w):
            return P.op("dve", lambda e: e.reciprocal(out=out, in_=in_), reads=r, writes=w)

        def memset(eng, out, v, w):
            return P.op(eng, lambda e: e.memset(out, v), writes=w)

        try:
            identf = al(128, F32)
            ident = al(128)
            memset("pool", identf, 0.0, ["identf"])
            P.op("pool", lambda e: e.affine_select(out=identf, in_=identf, pattern=[[-1, 128]], compare_op=ALU.not_equal,
                                                   fill=1.0, base=0, channel_multiplier=1), reads=["identf"], writes=["identf"])
            cp("dve", ident, identf, ["identf"], ["ident"])
            stg = [al(1024, F32) for _ in range(2)]
            stgk = ["stg0", "stg1"]
            stgi = [0]
            PERSIST = off[0]

            def load_cast(dst, src, n_p, shape_free, eng=None, p0=0):
                i = stgi[0] % 2
                stgi[0] += 1
                n = int(np.prod(shape_free))
                assert n <= 1024
                sv = stg[i][p0:p0 + n_p, 0:n]
                if len(shape_free) == 2:
                    sv = sv.rearrange("p (a b) -> p a b", a=shape_free[0])
                dma(sv, src, w=[stgk[i]])
                e_ = eng or ("dve" if i == 0 else "pool")
                return sv, stgk[i], e_

            def load_w(dst, dkey, src, ncols, kch=8):
                srcv = src.rearrange("(kc p) n -> p kc n", p=128)
                step = 1024 // kch
                first = True
                for c0 in range(0, ncols, step):
                    c1 = min(ncols, c0 + step)
                    sv, sk, e_ = load_cast(None, srcv[:, :, c0:c1], 128, [kch, c1 - c0])
                    cp(e_, dst[:, :, c0:c1], sv, [sk] + ([] if first else [dkey]), [dkey])
                    first = False

            u_v = peer_u.rearrange("(i j) d -> j i d", j=128)
            v_v = peer_v.rearrange("(i j) d -> j i d", j=128)
            t_state = {"j": 0, "s1": 0}

            def t_stage1(j):
                if j >= 128 or t_state["s1"] > j:
                    return
                t_state["s1"] = j + 1
                a = (2 * j) % 4
                b = (2 * j + 1) % 4
                dma(t_f[a], u_v[j], w=["t_f%d" % a], q="sp")
                dma(t_f[b], v_v[j], w=["t_f%d" % b], q="sp")
                cp("act" if j % 2 else "dve", t_b[a], t_f[a], ["t_f%d" % a], ["t_b%d" % a])
                cp("dve" if j % 2 else "act", t_b[b], t_f[b], ["t_f%d" % b], ["t_b%d" % b])
                dma(VB[j], t_b[b], r=["t_b%d" % b], w=["VB"], q="sp")

            def t_step():
                j = t_state["j"]
                if j >= 128:
                    return
                t_state["j"] = j + 1
                t_stage1(j)
                t_stage1(j + 1)
                a = (2 * j) % 4
                pb = 6 + (j % 2)
                pbv = banks[pb][:, :].bitcast(BF16)
                for kc in range(8):
                    tr(pbv[:, kc * 128:(kc + 1) * 128], t_b[a][:, kc * 128:(kc + 1) * 128], ident, ["t_b%d" % a, "ident"], [bk[pb]])
                o = j % 2
                cp("act" if j % 2 else "dve", t_o[o], pbv, [bk[pb]], ["t_o%d" % o])
                dma(UT[j], t_o[o], r=["t_o%d" % o], w=["UT"], q="sp")

            Wfm = al(8 * 2048).rearrange("p (k n) -> p k n", k=8)
            Wrot = al(8 * 1920).rearrange("p (k n) -> p k n", k=8)
            Wv = al(8 * 768).rearrange("p (k n) -> p k n", k=8)
            Wgn = al(8 * 24).rearrange("p (k n) -> p k n", k=8)
            Wgm = al(8 * 2048).rearrange("p (k n) -> p k n", k=8)
            for i, c0 in enumerate(FM_COLS):
                load_w(Wfm[:, :, i * 128:(i + 1) * 128], "Wfm%d" % i, w_in[:, c0:c0 + 128], 128)
            load_w(Wrot, "Wrot", w_rot, 1920)
            load_w(Wv[:, :, 0:512], "Wv0", w_in[:, C_VDA:C_VDA + 512], 512)
            load_w(Wv[:, :, 512:640], "Wv1", w_in[:, C_VS:C_VS + 128], 128)
            load_w(Wv[:, :, 640:768], "Wv2", w_in[:, C_VW:C_VW + 128], 128)
            load_w(Wgn, "Wgn", w_in[:, C_GN:C_GN + 24], 24)
            load_w(Wgm, "Wgm", w_in[:, C_GM:C_GM + 2048], 2048)
            WFMK = ["Wfm%d" % i for i in range(16)]
            gA = al(8, F32)
            dma(gA, attn_norm.rearrange("(k p) -> p k", p=128), w=["gA"], slow=True)
            xs = [al(1024, F32) for _ in range(2)]
            xb = [al(1024) for _ in range(2)]
            junk = al(1024, F32)
            ssq = [al(1, F32) for _ in range(2)]
            aT = al(8 * 512).rearrange("p (k n) -> p k n", k=8)
            rc = al(512, F32)
            rs_ = al(512, F32)
            tA = [al(512, F32) for _ in range(2)]
            tB = [al(512, F32) for _ in range(2)]
            fmo = [al(512) for _ in range(3)]
            vo = [al(768) for _ in range(2)]
            gno = [al(24, F32) for _ in range(2)]
            gmo = [al(2048, F32) for _ in range(2)]
            t_f = [al(1024, F32) for _ in range(4)]
            t_b = [al(1024) for _ in range(4)]
            t_o = [al(1024) for _ in range(2)]
            T_PER_TILE = -(-128 // (NQC * 16))

            def rms_to_T(xt_ap, xkey, gain, gkey, dstT, dkey, i2, pbank):
                act(junk, xt_ap, AF.Square, [xkey], ["junk", "ssq%d" % i2], scale=1.0 / 32.0, accum=ssq[i2])
                act(ssq[i2], ssq[i2], AF.Sqrt, ["ssq%d" % i2], ["ssq%d" % i2], bias=1e-6, scale=1.0)
                recip(ssq[i2], ssq[i2], ["ssq%d" % i2], ["ssq%d" % i2])
                act(xb[i2], xt_ap, AF.Copy, [xkey, "ssq%d" % i2], ["xb%d" % i2], scale=ssq[i2])
                pv = banks[pbank][:, :].bitcast(BF16)
                for kc in range(8):
                    tr(pv[:, kc * 128:(kc + 1) * 128], xb[i2][:, kc * 128:(kc + 1) * 128], ident, ["xb%d" % i2, "ident"], [bk[pbank]])
                tt("dve", dstT, pv.rearrange("p (k n) -> p k n", k=8), gain.unsqueeze(2).to_broadcast([128, 8, 128]),
                   ALU.mult, [bk[pbank], gkey, dkey], [dkey])

            fm_cnt = 0
            for c in range(NQC):
                q0 = c * 512
                dma(rc, ropec[:, q0:q0 + 512], w=["rc"])
                dma(rs_, ropes[:, q0:q0 + 512], w=["rs"])
                for t4 in range(4):
                    t0 = q0 + t4 * 128
                    i2 = t4 % 2
                    dma(xs[i2], x[t0:t0 + 128, :], w=["xs%d" % i2])
                    rms_to_T(xs[i2], "xs%d" % i2, gA, "gA", aT[:, :, t4 * 128:(t4 + 1) * 128], "aT", i2, 6 + i2)
                for i in range(16):
                    pz = (2 * i) % 4
                    pr = (2 * i + 1) % 4
                    for kc in range(8):
                        mm(banks[pz][:, :], Wfm[:, kc, i * 128:(i + 1) * 128], aT[:, kc, :], kc == 0, kc == 7, ["aT", WFMK[i]], [bk[pz]])
                    fo = fm_cnt % 3
                    fm_cnt += 1
                    if i < N_ROPED:
                        for kc in range(8):
                            mm(banks[pr][:, :], Wrot[:, kc, i * 128:(i + 1) * 128], aT[:, kc, :], kc == 0, kc == 7, ["aT", "Wrot"], [bk[pr]])
                        a2 = i % 2
                        tt("dve", tA[a2], banks[pz][:, :], rc, ALU.mult, [bk[pz], "rc"], ["tA%d" % a2])
                        tt("dve", tB[a2], banks[pr][:, :], rs_, ALU.mult, [bk[pr], "rs"], ["tB%d" % a2])
                        tt("pool", fmo[fo], tA[a2], tB[a2], ALU.add, ["tA%d" % a2, "tB%d" % a2], ["fmo%d" % fo])
                    else:
                        cp("act", fmo[fo], banks[pz][:, :], [bk[pz]], ["fmo%d" % fo])
                    dma(FM[i, :, q0:q0 + 512], fmo[fo], r=["fmo%d" % fo], w=["FM"])
                    for _ in range(T_PER_TILE):
                        t_step()
                    if dbg:
                        dma(dbg_out["d_fm"][i, :, q0:q0 + 512], fmo[fo], r=["fmo%d" % fo], w=["d_fm"])
                for t4 in range(4):
                    t0 = q0 + t4 * 128
                    i2 = t4 % 2
                    lcol = slice(t4 * 128, (t4 + 1) * 128)
                    for (c0, c1, pb) in ((0, 512, 4), (512, 768, 5)):
                        for kc in range(8):
                            mm(banks[pb][:, 0:c1 - c0], aT[:, kc, lcol], Wv[:, kc, c0:c1], kc == 0, kc == 7, ["aT", "Wv0", "Wv1", "Wv2"], [bk[pb]])
                        cp("act", vo[i2][:, c0:c1], banks[pb][:, 0:c1 - c0], [bk[pb], "vo%d" % i2], ["vo%d" % i2])
                    dma(VT[t0:t0 + 128, :], vo[i2], r=["vo%d" % i2], w=["VT"])
                    for kc in range(8):
                        mm(banks[5][:, 0:24], aT[:, kc, lcol], Wgn[:, kc, :], kc == 0, kc == 7, ["aT", "Wgn"], [bk[5]])
                    act(gno[i2], banks[5][:, 0:24], AF.Sigmoid, [bk[5]], ["gno%d" % i2])
                    dma(GN[t0:t0 + 128, :], gno[i2], r=["gno%d" % i2], w=["GN"])
                    for g4 in range(4):
                        pb = g4 % 4
                        for kc in range(8):
                            mm(banks[pb][:, :], aT[:, kc, lcol], Wgm[:, kc, g4 * 512:(g4 + 1) * 512], kc == 0, kc == 7, ["aT", "Wgm"], [bk[pb]])
                        act(gmo[i2][:, g4 * 512:(g4 + 1) * 512], banks[pb][:, :], AF.Sigmoid, [bk[pb], "gmo%d" % i2], ["gmo%d" % i2])
                    dma(GM[t0:t0 + 128, :], gmo[i2], r=["gmo%d" % i2], w=["GM"])
            while t_state["j"] < 128:
                t_step()
            P.barrier()
            if LIM == 2:
                raise _Stop()
            off[0] = PERSIST

            SC = [0, 1, 6]
            ACC = [2, 3, 4, 5]
            sc_i = [0]
            ecnt = [0]
            smc = [0]
            MSK = al(8 * 512).rearrange("p (m n) -> p m n", m=8)
            dma(MSK, masks.rearrange("m p n -> p m n"), w=["MSK"])
            E = [al(512) for _ in range(4)]
            sm = al(64, F32)
            PERSIST2 = off[0]

            def smalloc():
                i = smc[0] % 64
                smc[0] += 1
                return sm[:, i:i + 1], "sm%d" % i

            def score_tile(lhsT, rhs, kkeys, mask_ap):
                b = SC[sc_i[0] % 3]
                sc_i[0] += 1
                mm(banks[b][:, :], lhsT, rhs, True, mask_ap is None, kkeys, [bk[b]])
                if mask_ap is not None:
                    mm(banks[b][:, :], ident, mask_ap, False, True, ["ident", "MSK", "CM0", "CM1"], [bk[b]])
                ei = ecnt[0] % 4
                ecnt[0] += 1
                act(E[ei], banks[b][:, :], AF.Exp, [bk[b]], ["E%d" % ei], scale=0.125)
                return E[ei], "E%d" % ei

            LOOK = 2

            def run_jobs(jobs):
                q_ = []
                for (sf, pf, ef) in jobs:
                    q_.append((sf(), pf, ef))
                    if len(q_) > LOOK:
                        Ee, pf0, ef0 = q_.pop(0)
                        if pf0 is not None:
                            pf0(Ee)
                        if ef0 is not None:
                            ef0()
                for (Ee, pf0, ef0) in q_:
                    if pf0 is not None:
                        pf0(Ee)
                    if ef0 is not None:
                        ef0()

            KDA = [al(S) for _ in range(4)]
            for h in range(4):
                dma(KDA[h], FM[4 + h], r=["FM"], w=["KDA%d" % h])
            VDA = al(NT * 4 * 130).rearrange("p (t h d) -> p t h d", t=NT, h=4)
            memset("pool", VDA[:, :, :, 128:130], 1.0, ["VDA"])
            VTv = VT.rearrange("(t p) c -> p t c", p=128)
            for h in range(4):
                dma(VDA[:, :, h, 0:128], VTv[:, :, h * 128:(h + 1) * 128], r=["VT", "VDA"], w=["VDA"])
            lamt = al(256, F32)
            dma(lamt, lam4.rearrange("a d -> (a d)").partition_broadcast(128), w=["lamt"])
            lamp = al(128, F32)
            lam2 = al(4, F32)
            l4 = lamt.rearrange("p (a d) -> p a d", a=4)
            tt("dve", lamp.rearrange("p (a d) -> p a d", a=2), l4[:, 0:4:2, :], l4[:, 1:4:2, :], ALU.mult, ["lamt"], ["lamp"])
            P.op("dve", lambda e: e.reduce_sum(out=lam2[:, 0:2], in_=lamp.rearrange("p (a d) -> p a d", a=2), axis=AX.X), reads=["lamp"], writes=["lam2"])
            act(lam2[:, 0:2], lam2[:, 0:2], AF.Exp, ["lam2"], ["lam2"])
            tt("dve", lam2[:, 2:3], lam2[:, 1:2], lam2[:, 0:1], ALU.subtract, ["lam2"], ["lam2"])
            ts("dve", lam2[:, 3:4], lam2[:, 2:3], -LAMBDA_INIT, None, ALU.add, None, ["lam2"], ["lam2"])
            neglam = lam2[:, 3:4]
            sublnB = al(128, F32)
            dma(sublnB, subln.partition_broadcast(128), w=["sublnB"])
            ts("dve", sublnB, sublnB, 1.0 - LAMBDA_INIT, None, ALU.mult, None, ["sublnB"], ["sublnB"])
            QDA = [[al(512) for _ in range(2)] for _ in range(4)]
            for h in range(4):
                memset("pool", QDA[h][0][64:128, :], 0.0, ["QDA%d_0" % h])
                memset("pool", QDA[h][1][0:64, :], 0.0, ["QDA%d_1" % h])
            o1 = [al(128, F32) for _ in range(4)]
            yda = [al(512, F32) for _ in range(4)]
            junkb = al(128)
            for c in range(NQC):
                q0 = c * 512
                kt_hi = q0 // 128 + 3
                for h in range(4):
                    dma(QDA[h][0][0:64, :], FM[h, 0:64, q0:q0 + 512], r=["FM", "QDA%d_0" % h], w=["QDA%d_0" % h])
                    dma(QDA[h][1][64:128, :], FM[h, 64:128, q0:q0 + 512], r=["FM", "QDA%d_1" % h], w=["QDA%d_1" % h])
                jobs = []
                for h in range(4):
                    for cc in range(2):
                        rows = slice(cc * 64, (cc + 1) * 64)
                        for kt in range(0, kt_hi + 1):
                            d8 = kt - (q0 // 128 - 4)
                            mk = MSK[:, d8, :] if d8 >= 4 else None

                            def sf(h=h, cc=cc, kt=kt, mk=mk):
                                return score_tile(KDA[h][:, kt * 128:(kt + 1) * 128], QDA[h][cc], ["KDA%d" % h, "QDA%d_%d" % (h, cc)], mk)

                            def pf(Ee, h=h, kt=kt):
                                Ea, Ek = Ee
                                for qt in range(4):
                                    qti = q0 // 128 + qt
                                    if kt > qti:
                                        continue
                                    a = ACC[qt]
                                    mm(banks[a][:, 0:129], Ea[:, qt * 128:(qt + 1) * 128], VDA[:, kt, h, 0:129], kt == 0, kt == qti, [Ek, "VDA"], [bk[a]])

                            def ef(h=h, cc=cc):
                                for qt in range(4):
                                    a = ACC[qt]
                                    rz, rzk = smalloc()
                                    recip(rz, banks[a][:, 128:129], [bk[a]], [rzk])
                                    if cc == 0:
                                        ts("dve", o1[qt], banks[a][:, 0:128], rz, None, ALU.mult, None, [bk[a], rzk], ["o1_%d" % qt])
                                    else:
                                        tt("dve", rz, rz, neglam, ALU.mult, [rzk, "lam2"], [rzk])
                                        stt("dve", o1[qt], banks[a][:, 0:128], rz, o1[qt], ALU.mult, ALU.add, [bk[a], rzk, "o1_%d" % qt], ["o1_%d" % qt])
                                        s2, s2k = smalloc()
                                        act(junkb, o1[qt], AF.Square, ["o1_%d" % qt], ["junkb", s2k], scale=1.0 / math.sqrt(128.0), accum=s2)
                                        act(s2, s2, AF.Sqrt, [s2k], [s2k], bias=1e-6, scale=1.0)
                                        recip(s2, s2, [s2k], [s2k])
                                        stt("dve", yda[qt][:, h * 128:(h + 1) * 128], o1[qt], s2, sublnB, ALU.mult, ALU.mult,
                                            ["o1_%d" % qt, s2k, "sublnB", "yda%d" % qt], ["yda%d" % qt])

                            jobs.append((sf, pf, ef if kt == kt_hi else None))
                run_jobs(jobs)
                for qt in range(4):
                    t0 = q0 + qt * 128
                    dma(YDA[t0:t0 + 128, :], yda[qt], r=["yda%d" % qt], w=["YDA"])
            P.barrier()
            if LIM == 3:
                raise _Stop()
            off[0] = PERSIST2

            KSA = [al(S) for _ in range(2)]
            KW = [al(S) for _ in range(2)]
            for g in range(2):
                dma(KSA[g][0:64, :], FM[13, g * 64:(g + 1) * 64, :], r=["FM"], w=["KSA%d" % g])
                dma(KSA[g][64:128, :], esel, r=["KSA%d" % g], w=["KSA%d" % g])
                memset("pool", KW[g][64:128, :], 0.0, ["KW%d" % g])
                dma(KW[g][0:64, :], FM[14, g * 64:(g + 1) * 64, :], r=["FM", "KW%d" % g], w=["KW%d" % g])
            VS = al(NT * 2 * 66).rearrange("p (t g d) -> p t g d", t=NT, g=2)
            VW = al(NT * 2 * 66).rearrange("p (t g d) -> p t g d", t=NT, g=2)
            memset("pool", VS[:, :, :, 64:66], 1.0, ["VS"])
            memset("pool", VW[:, :, :, 64:66], 1.0, ["VW"])
            for g in range(2):
                dma(VS[:, :, g, 0:64], VTv[:, :, 512 + g * 64:512 + (g + 1) * 64], r=["VT", "VS"], w=["VS"])
                dma(VW[:, :, g, 0:64], VTv[:, :, 640 + g * 64:640 + (g + 1) * 64], r=["VT", "VW"], w=["VW"])
            KCMP = [al(256) for _ in range(2)]
            for g in range(2):
                memset("pool", KCMP[g], 0.0, ["KCMP%d" % g])
            VCO = al(2 * 2 * 130).rearrange("p (g t d) -> p g t d", g=2, t=2)
            memset("pool", VCO, 0.0, ["VCO"])
            memset("pool", VCO[:, :, :, 64:65], 1.0, ["VCO"])
            for g in range(2):
                for nt in range(2):
                    dma(VCO[:, g, nt, 65:129], ovl[nt * 128:(nt + 1) * 128, :], r=["VCO"], w=["VCO"])

            mark_B = off[0]
            kcT = al(S)
            vcT = al(S)
            dma(kcT, FM[12], r=["FM"], w=["kcT"])
            dma(vcT, FM[15], r=["FM"], w=["vcT"])
            W1 = {}
            for nm, src in (("k", w1k), ("v", w1v)):
                W1[nm] = al(2048).rearrange("p (l o) -> p l o", l=32)
                srcv = src.rearrange("(l d) o -> d l o", d=64)
                for half in range(2):
                    for lh in range(2):
                        sv, sk, e_ = load_cast(None, srcv[:, lh * 16:(lh + 1) * 16, :], 64, [16, 64], eng="dve", p0=half * 64)
                        kk_ = "W1%s%d" % (nm, half)
                        cp(e_, W1[nm][half * 64:(half + 1) * 64, lh * 16:(lh + 1) * 16, :], sv, [sk] + ([kk_] if lh else []), [kk_])
            W2 = {}
            for nm, src in (("k", w2k), ("v", w2v)):
                W2[nm] = al(64)
                sv, sk, e_ = load_cast(None, src, 64, [64], eng="dve")
                cp(e_, W2[nm][0:64, 0:64], sv, [sk], ["W2" + nm])
            PEb = {}
            for nm, src in (("k", peT_k), ("v", peT_v)):
                PEb[nm] = al(32)
                sv, sk, e_ = load_cast(None, src, 64, [32], eng="dve")
                cp(e_, PEb[nm][0:64, :], sv, [sk], ["PE" + nm])
            cb = al(2, F32)
            hidT = al(256)
            memset("pool", hidT, 0.0, ["hidT"])
            for ki, nm in enumerate(("k", "v")):
                for l in range(32):
                    mm(banks[6][0:64, ki:ki + 1], W1[nm][0:64, l, :], PEb[nm][0:64, l:l + 1], l == 0, l == 31, ["W1%s0" % nm, "PE" + nm], [bk[6]])
                cp("dve", cb[0:64, ki:ki + 1], banks[6][0:64, ki:ki + 1], [bk[6], "cb"], ["cb"])
                srcT = kcT if nm == "k" else vcT
                skey = "kcT" if nm == "k" else "vcT"
                for g in range(2):
                    rows = slice(g * 64, (g + 1) * 64)
                    sview = srcT.rearrange("p (n s) -> p n s", s=16)
                    for l in range(32):
                        rhs = sview[rows, (l // 16):(l // 16) + NCMP, l % 16]
                        mm(banks[7][0:64, 0:NCMP], W1[nm][rows, l, :], rhs, l == 0, l == 31, ["W1%s%d" % (nm, g), skey], [bk[7]])
                    act(hidT[0:64, 0:NCMP], banks[7][0:64, 0:NCMP], AF.Gelu, [bk[7], "cb", "hidT"], ["hidT"], bias=cb[0:64, ki:ki + 1], scale=1.0)
                    if nm == "k":
                        mm(banks[6][0:64, 0:256], W2["k"][0:64, 0:64], hidT[0:64, :], True, True, ["W2k", "hidT"], [bk[6]])
                        cp("dve", KCMP[g][0:64, :], banks[6][0:64, 0:256], [bk[6], "KCMP%d" % g], ["KCMP%d" % g])
                    else:
                        for nt in range(2):
                            mm(banks[6][:, 0:64], hidT[0:64, nt * 128:(nt + 1) * 128], W2["v"][0:64, 0:64], True, True, ["W2v", "hidT"], [bk[6]])
                            cp("dve", VCO[:, g, nt, 0:64], banks[6][:, 0:64], [bk[6], "VCO"], ["VCO"])
            P.barrier()
            if LIM == 4:
                raise _Stop()
            off[0] = mark_B

            PDA = al(4 * 1024).rearrange("p (k n) -> p k n", k=4)
            PNS = al(4 * 1024).rearrange("p (k n) -> p k n", k=4)
            WO = al(8 * 1024).rearrange("p (k n) -> p k n", k=8)
            load_w(PDA, "PDA", p_da, 1024, kch=4)
            load_w(PNS, "PNS", p_nsa, 1024, kch=4)
            load_w(WO, "WO", w_o, 1024, kch=8)
            gF = al(8, F32)
            dma(gF, ffn_norm.rearrange("(k p) -> p k", p=128), w=["gF"], slow=True)
            QA = [al(512) for _ in range(8)]
            for h in range(8):
                memset("pool", QA[h], 0.0, ["QA%d" % h])
            CM = [al(512) for _ in range(2)]
            gn_t = [al(24, F32) for _ in range(4)]
            cs_t = [al(64, F32) for _ in range(4)]
            ycomb = [al(512, F32) for _ in range(4)]
            imp = [al(64, F32) for _ in range(4)]
            m8 = al(16, F32)
            wk64 = al(64, F32)
            mb = al(128, F32)
            memset("pool", mb, 0.0, ["mb"])
            mbT = [al(128) for _ in range(2)]
            mbi = [0]
            ydt = al(512, F32)
            ybf = al(1024)
            yT = al(1024).rearrange("p (k n) -> p k n", k=8)
            gmt = al(1024, F32)
            m1 = al(512, F32)
            m2 = al(512, F32)
            mgb = al(1024)
            mT = al(1024).rearrange("p (k n) -> p k n", k=8)
            hs = al(1024, F32)
            cTo = al(1024).rearrange("p (k n) -> p k n", k=8)
            junk = al(1024)
            xb = [al(1024)]
            ssq = [al(1, F32)]

            if LIM == 40:
                raise _Stop()
            for c in range(NQC):
                q0 = c * 512
                kt_hi = q0 // 128 + 3
                for h in range(8):
                    dma(QA[h][0:64, :], FM[8 + h // 2, (h % 2) * 64:(h % 2) * 64 + 64, q0:q0 + 512], r=["FM"], w=["QA%d" % h])
                for nt in range(2):
                    dma(CM[nt], cmaskT[nt * 128:(nt + 1) * 128, q0:q0 + 512], w=["CM%d" % nt])
                for qt in range(4):
                    t0 = q0 + qt * 128
                    dma(gn_t[qt], GN[t0:t0 + 128, :], r=["GN"], w=["gn%d" % qt])
                    dma(cs_t[qt], csel[t0:t0 + 128, :], w=["cs%d" % qt])

                for g in range(2):
                    jobs = []
                    for hg in range(4):
                        h = g * 4 + hg
                        Ehold = {}

                        def sf0(g=g, h=h):
                            return score_tile(KCMP[g][:, 0:128], QA[h], ["KCMP%d" % g, "QA%d" % h], CM[0])

                        def sf1(g=g, h=h):
                            return score_tile(KCMP[g][:, 128:256], QA[h], ["KCMP%d" % g, "QA%d" % h], CM[1])

                        def pf0(Ee, Ehold=Ehold):
                            Ehold[0] = Ee

                        def pf1(Ee, g=g, h=h, hg=hg, Ehold=Ehold):
                            Es = [Ehold[0], Ee]
                            for qt in range(4):
                                a = ACC[qt]
                                for nt in range(2):
                                    mm(banks[a][:, 0:129], Es[nt][0][:, qt * 128:(qt + 1) * 128], VCO[:, g, nt, 0:129], nt == 0, nt == 1,
                                       [Es[nt][1], "VCO"], [bk[a]])
                                rz, rzk = smalloc()
                                ts("dve", rz, banks[a][:, 64:65], 1e-30, None, ALU.add, None, [bk[a]], [rzk])
                                recip(rz, rz, [rzk], [rzk])
                                ycs = ycomb[qt][:, h * 64:(h + 1) * 64]
                                yk = "yc%d_%d" % (qt, h)
                                ts("dve", ycs, banks[a][:, 0:64], rz, gn_t[qt][:, h * 3:h * 3 + 1], ALU.mult, ALU.mult, [bk[a], rzk, "gn%d" % qt], [yk])
                                if hg == 0:
                                    ts("dve", imp[qt], banks[a][:, 65:129], rz, None, ALU.mult, None, [bk[a], rzk], ["imp%d" % qt])
                                else:
                                    stt("dve", imp[qt], banks[a][:, 65:129], rz, imp[qt], ALU.mult, ALU.add, [bk[a], rzk, "imp%d" % qt], ["imp%d" % qt])

                        jobs.append((sf0, pf0, None))
                        jobs.append((sf1, pf1, None))
                    run_jobs(jobs)
                    if LIM == 405:
                        raise _Stop()
                    for qt in range(4):
                        tt("dve", imp[qt], imp[qt], cs_t[qt], ALU.add, ["imp%d" % qt, "cs%d" % qt], ["imp%d" % qt])
                        P.op("dve", lambda e, qt=qt: e.max(out=m8[:, 0:8], in_=imp[qt]), reads=["imp%d" % qt], writes=["m8"])
                        P.op("dve", lambda e, qt=qt: e.match_replace(out=wk64, in_to_replace=m8[:, 0:8], in_values=imp[qt], imm_value=-3e38),
                             reads=["imp%d" % qt, "m8"], writes=["wk64"])
                        P.op("dve", lambda e: e.max(out=m8[:, 8:16], in_=wk64), reads=["wk64", "m8"], writes=["m8"])
                        ts("dve", mb[:, 64:128], imp[qt], m8[:, 15:16], None, ALU.is_ge, None, ["imp%d" % qt, "m8", "mb"], ["mb"])
                        ts("dve", mb[:, 64:128], mb[:, 64:128], -1.0, -NEG, ALU.add, ALU.mult, ["mb"], ["mb"])
                        if LIM == 406:
                            raise _Stop()
                        tr(banks[6][:, 0:128], mb, identf, ["mb", "identf"], [bk[6]])
                        if LIM == 407:
                            raise _Stop()
                        mbi[0] += 1
                        cp("act", mbT[mbi[0] % 2], banks[6][:, 0:128], [bk[6]], ["mbT%d" % (mbi[0] % 2)])
                        for hg in range(4):
                            h = g * 4 + hg
                            dma(QA[h][64:128, qt * 128:(qt + 1) * 128], mbT[mbi[0] % 2][64:128, :], r=["mbT%d" % (mbi[0] % 2), "QA%d" % h], w=["QA%d" % h])
                        if LIM == 409:
                            raise _Stop()
                    if LIM == 408:
                        raise _Stop()

                if LIM == 41:
                    raise _Stop()
                jobs = []
                for h in range(8):
                    g = h // 4
                    for br in (1, 2):
                        if br == 1:
                            kts = list(range(0, kt_hi + 1))
                        else:
                            kts = [kt for kt in range(q0 // 128 - 4, kt_hi + 1) if kt >= 0]
                        for kt in kts:
                            d8 = kt - (q0 // 128 - 4)

                            def sf(h=h, g=g, br=br, kt=kt, d8=d8):
                                if br == 1:
                                    mk = MSK[:, d8, :] if d8 >= 4 else None
                                    return score_tile(KSA[g][:, kt * 128:(kt + 1) * 128], QA[h], ["KSA%d" % g, "QA%d" % h], mk)
                                return score_tile(KW[g][:, kt * 128:(kt + 1) * 128], QA[h], ["KW%d" % g, "QA%d" % h], MSK[:, d8, :])

                            def pf(Ee, h=h, g=g, br=br, kt=kt):
                                Ea, Ek = Ee
                                Vt, vk = (VS, "VS") if br == 1 else (VW, "VW")
                                for qt in range(4):
                                    qti = q0 // 128 + qt
                                    if kt > qti or (br == 2 and kt < qti - 4):
                                        continue
                                    first = 0 if br == 1 else max(0, qti - 4)
                                    a = ACC[qt]
                                    mm(banks[a][:, 0:65], Ea[:, qt * 128:(qt + 1) * 128], Vt[:, kt, g, 0:65], kt == first, kt == qti, [Ek, vk], [bk[a]])

                            def ef(h=h, br=br):
                                for qt in range(4):
                                    a = ACC[qt]
                                    rz, rzk = smalloc()
                                    recip(rz, banks[a][:, 64:65], [bk[a]], [rzk])
                                    tt("dve", rz, rz, gn_t[qt][:, h * 3 + br:h * 3 + br + 1], ALU.mult, [rzk, "gn%d" % qt], [rzk])
                                    ycs = ycomb[qt][:, h * 64:(h + 1) * 64]
                                    yk = "yc%d_%d" % (qt, h)
                                    stt("dve", ycs, banks[a][:, 0:64], rz, ycs, ALU.mult, ALU.add, [bk[a], rzk, yk], [yk])

                            jobs.append((sf, pf, ef if kt == kts[-1] else None))
                run_jobs(jobs)

                if LIM == 42:
                    raise _Stop()
                for qt in range(4):
                    t0 = q0 + qt * 128
                    ykeys = ["yc%d_%d" % (qt, h) for h in range(8)]
                    dma(ydt, YDA[t0:t0 + 128, :], r=["YDA"], w=["ydt"])
                    if dbg:
                        dma(dbg_out["d_yy"][t0:t0 + 128, 0:512], ydt, r=["ydt"], w=["d_yy"])
                        dma(dbg_out["d_yy"][t0:t0 + 128, 512:1024], ycomb[qt], r=ykeys, w=["d_yy"])
                    cp("dve", ybf[:, 0:512], ydt, ["ydt", "ybf"], ["ybf"])
                    cp("act", ybf[:, 512:1024], ycomb[qt], ykeys + ["ybf"], ["ybf"])
                    pv = banks[6][:, :].bitcast(BF16)
                    for k in range(8):
                        tr(pv[:, k * 128:(k + 1) * 128], ybf[:, k * 128:(k + 1) * 128], ident, ["ybf", "ident"], [bk[6]])
                    cp("act", yT, pv.rearrange("p (k n) -> p k n", k=8), [bk[6]], ["yT"])
                    dma(hs, x[t0:t0 + 128, :], w=["hs"])
                    for half in range(2):
                        cs = slice(half * 512, (half + 1) * 512)
                        dma(gmt[:, 0:512], GM[t0:t0 + 128, half * 512:(half + 1) * 512], r=["GM", "gmt"], w=["gmt"])
                        dma(gmt[:, 512:1024], GM[t0:t0 + 128, 1024 + half * 512:1024 + (half + 1) * 512], r=["GM", "gmt"], w=["gmt"])
                        for k in range(4):
                            mm(banks[0][:, :], yT[:, k, :], PDA[:, k, cs], k == 0, k == 3, ["yT", "PDA"], [bk[0]])
                        for k in range(4):
                            mm(banks[1][:, :], yT[:, 4 + k, :], PNS[:, k, cs], k == 0, k == 3, ["yT", "PNS"], [bk[1]])
                        tt("dve", m1, banks[0][:, :], gmt[:, 0:512], ALU.mult, [bk[0], "gmt"], ["m1"])
                        tt("dve", m2, banks[1][:, :], gmt[:, 512:1024], ALU.mult, [bk[1], "gmt"], ["m2"])
                        tt("dve", mgb[:, cs], m1, m2, ALU.add, ["m1", "m2", "mgb"], ["mgb"])
                    pv7 = banks[7][:, :].bitcast(BF16)
                    for k in range(8):
                        tr(pv7[:, k * 128:(k + 1) * 128], mgb[:, k * 128:(k + 1) * 128], ident, ["mgb", "ident"], [bk[7]])
                    cp("act", mT, pv7.rearrange("p (k n) -> p k n", k=8), [bk[7]], ["mT"])
                    for half in range(2):
                        cs = slice(half * 512, (half + 1) * 512)
                        for k in range(8):
                            mm(banks[half][:, :], mT[:, k, :], WO[:, k, cs], k == 0, k == 7, ["mT", "WO"], [bk[half]])
                        tt("dve", hs[:, cs], banks[half][:, :], hs[:, cs], ALU.add, [bk[half], "hs"], ["hs"])
                    dma(H[t0:t0 + 128, :], hs, r=["hs"], w=["H"])
                    if dbg:
                        dma(dbg_out["d_h"][t0:t0 + 128, :], hs, r=["hs"], w=["d_h"])
                    rms_to_T(hs, "hs", gF, "gF", cTo, "cTo", 0, 6)
                    dma(CT[:, :, t0:t0 + 128], cTo, r=["cTo"], w=["CT"])
            P.barrier()
            if LIM == 5:
                raise _Stop()
            off[0] = PERSIST

            WQ = al(8 * 2048).rearrange("p (k n) -> p k n", k=8)
            load_w(WQ, "WQ", wq, 2048)
            K1 = al(128)
            K2 = al(128)
            for dst, src, kk in ((K1, k1T, "K1"), (K2, k2T, "K2")):
                sv, sk, e_ = load_cast(None, src, 128, [128])
                cp(e_, dst, sv, [sk], [kk])
            KK = [K1, K2]
            V16K = ["v16a_%d" % u for u in range(16)] + ["v16b_%d" % u for u in range(16)]
            C16K = ["c16a_%d" % u for u in range(8)] + ["c16b_%d" % u for u in range(8)]
            P.shared = set(["WQ", "K1", "K2", "ident", "identf", "CT", "QTs", "SVs"])
            pp = []
            for par in range(2):
                d_ = {}
                d_["cT"] = al(8 * TB).rearrange("p (k n) -> p k n", k=8)
                d_["qT"] = al(16 * TB).rearrange("p (u n) -> p u n", u=16)
                d_["sc"] = [al(2048, F32).rearrange("p (u n) -> p u n", u=16) for _ in range(2)]
                d_["wk"] = [al(128, F32) for _ in range(16)]
                d_["v16"] = al(256, F32).rearrange("p (h s a) -> p h s a", h=8, s=2)
                d_["cand"] = al(2048, F32).rearrange("p (h a b) -> p h a b", h=8, a=16)
                d_["wk2"] = [al(256, F32) for _ in range(8)]
                d_["c16"] = al(128, F32).rearrange("p (h a) -> p h a", h=8)
                d_["ex16"] = al(128, F32).rearrange("p (h a) -> p h a", h=8)
                d_["z8"] = al(8, F32)
                d_["ml8"] = al(8, F32)
                d_["A1"] = al(128, F32)
                d_["A2"] = al(128, F32)
                d_["A3"] = al(128, F32)
                d_["v1T"] = al(TB, F32)
                d_["t2T"] = al(TB, F32)
                d_["bT"] = al(TB, F32)
                pp.append(d_)
            def pp_p1(b):
                tb0 = b * TB
                P.suffix = "_p%d" % (b % 2)
                d_ = pp[b % 2]
                cT, qT = d_["cT"], d_["qT"]
                dma(cT, CT[:, :, tb0:tb0 + TB], r=["CT"], w=["cT"])
                for u in range(16):
                    pb = 4 + (u % 4)
                    for kc in range(8):
                        mm(banks[pb][:, 0:TB], WQ[:, kc, u * 128:(u + 1) * 128], cT[:, kc, :], kc == 0, kc == 7, ["WQ", "cT"], [bk[pb]])
                    cp("act", qT[:, u, :], banks[pb][:, 0:TB], [bk[pb], "qT"], ["qT"])

            def pp_p2(b):
                P.suffix = "_p%d" % (b % 2)
                d_ = pp[b % 2]
                cT, qT, wk, v16, cand, wk2, c16, ex16, z8, ml8, A1, A2, A3, v1T, t2T, bT = (
                    d_["cT"], d_["qT"], d_["wk"], d_["v16"], d_["cand"], d_["wk2"], d_["c16"], d_["ex16"], d_["z8"], d_["ml8"],
                    d_["A1"], d_["A2"], d_["A3"], d_["v1T"], d_["t2T"], d_["bT"])
                for t2 in range(TB // 128):
                    tl = slice(t2 * 128, (t2 + 1) * 128)
                    sc = d_["sc"][t2]
                    sck = "sc%d" % t2
                    for u in range(16):
                        pb = u // 4
                        mm(banks[pb][:, (u % 4) * 128:(u % 4 + 1) * 128], qT[:, u, tl], KK[u % 2], True, True, ["qT", "K1", "K2"], [bk[pb]])
                    for pb in range(4):
                        cp("act", sc[:, pb * 4:(pb + 1) * 4, :], banks[pb][:, :].rearrange("p (u n) -> p u n", u=4), [bk[pb], sck], [sck])
                for t2 in range(TB // 128):
                    tl = slice(t2 * 128, (t2 + 1) * 128)
                    sc = d_["sc"][t2]
                    sck = "sc%d" % t2
                    for u0 in range(0, 16, 16):
                        for u in range(u0, u0 + 16):
                            hh, ss_ = u // 2, u % 2
                            P.op("dve", lambda e, u=u, hh=hh, ss_=ss_, v16=v16, sc=sc, wk=wk, c16=c16, wk2=wk2, z8=z8, ex16=ex16: e.max(out=v16[:, hh, ss_, 0:8], in_=sc[:, u, :]), reads=[sck], writes=["v16a_%d" % u])
                        for u in range(u0, u0 + 16):
                            hh, ss_ = u // 2, u % 2
                            P.op("dve", lambda e, u=u, hh=hh, ss_=ss_, v16=v16, sc=sc, wk=wk, c16=c16, wk2=wk2, z8=z8, ex16=ex16: e.match_replace(out=wk[u % 16], in_to_replace=v16[:, hh, ss_, 0:8], in_values=sc[:, u, :], imm_value=-3e38),
                                 reads=[sck, "v16a_%d" % u], writes=["wk%d" % (u % 16)])
                        for u in range(u0, u0 + 16):
                            hh, ss_ = u // 2, u % 2
                            P.op("dve", lambda e, u=u, hh=hh, ss_=ss_, v16=v16, sc=sc, wk=wk, c16=c16, wk2=wk2, z8=z8, ex16=ex16: e.max(out=v16[:, hh, ss_, 8:16], in_=wk[u % 16]), reads=["wk%d" % (u % 16)], writes=["v16b_%d" % u])
                    tt("dve", cand, v16[:, :, 0, :].unsqueeze(3).to_broadcast([128, 8, 16, 16]), v16[:, :, 1, :].unsqueeze(2).to_broadcast([128, 8, 16, 16]),
                       ALU.add, V16K, ["cand"])
                    for h0 in range(0, 8, 8):
                        for hh in range(h0, h0 + 8):
                            cv = cand[:, hh, :, :].rearrange("p a b -> p (a b)")
                            P.op("dve", lambda e, hh=hh, cv=cv, v16=v16, sc=sc, wk=wk, c16=c16, wk2=wk2, z8=z8, ex16=ex16: e.max(out=c16[:, hh, 0:8], in_=cv), reads=["cand"], writes=["c16a_%d" % hh])
                        for hh in range(h0, h0 + 8):
                            cv = cand[:, hh, :, :].rearrange("p a b -> p (a b)")
                            P.op("dve", lambda e, hh=hh, cv=cv, v16=v16, sc=sc, wk=wk, c16=c16, wk2=wk2, z8=z8, ex16=ex16: e.match_replace(out=wk2[hh % 8], in_to_replace=c16[:, hh, 0:8], in_values=cv, imm_value=-3e38),
                                 reads=["cand", "c16a_%d" % hh], writes=["wk2_%d" % (hh % 8)])
                        for hh in range(h0, h0 + 8):
                            P.op("dve", lambda e, hh=hh, v16=v16, sc=sc, wk=wk, c16=c16, wk2=wk2, z8=z8, ex16=ex16: e.max(out=c16[:, hh, 8:16], in_=wk2[hh % 8]), reads=["wk2_%d" % (hh % 8)], writes=["c16b_%d" % hh])
                    tt("dve", ex16, c16, c16[:, :, 0:1].to_broadcast([128, 8, 16]), ALU.subtract, C16K, ["ex16"])
                    act(ex16, ex16, AF.Exp, ["ex16"], ["ex16"])
                    P.op("dve", lambda e, v16=v16, sc=sc, wk=wk, c16=c16, wk2=wk2, z8=z8, ex16=ex16: e.reduce_sum(out=z8, in_=ex16, axis=AX.X), reads=["ex16"], writes=["z8"])
                    act(ml8, z8, AF.Ln, ["z8"], ["ml8"])
                    tt("dve", ml8, ml8, c16[:, :, 0], ALU.add, ["ml8"] + C16K, ["ml8"])
                    cp("dve", A1.rearrange("p (h a) -> p h a", h=8), v16[:, :, 0, :], V16K, ["A1"])
                    cp("dve", A2.rearrange("p (h a) -> p h a", h=8), c16[:, :, 15:16].to_broadcast([128, 8, 16]), C16K, ["A2"])
                    tt("dve", A3.rearrange("p (h a) -> p h a", h=8), v16[:, :, 0, :], ml8.unsqueeze(2).to_broadcast([128, 8, 16]), ALU.subtract, V16K + ["ml8"], ["A3"])
                    for Ax, ak, Tx, kx, px in ((A1, "A1", v1T, "v1T", 4), (A2, "A2", t2T, "t2T", 5), (A3, "A3", bT, "bT", 6)):
                        tr(banks[px][:, 0:128], Ax, identf, [ak, "identf"], [bk[px]])
                        cp("act", Tx[:, tl], banks[px][:, 0:128], [bk[px], kx], [kx])
                dma(QTs[b], qT.rearrange("p u n -> p (u n)"), r=["qT"], w=["QTs"])
                dma(SVs[b, 0], v1T, r=["v1T"], w=["SVs"])
                dma(SVs[b, 1], t2T, r=["t2T"], w=["SVs"])
                dma(SVs[b, 2], bT, r=["bT"], w=["SVs"])

            pp_p1(0)
            for b in range(NB):
                if b + 1 < NB:
                    pp_p1(b + 1)
                pp_p2(b)
            P.suffix = None
            P.barrier()
            if LIM == 6:
                raise _Stop()
            off[0] = PERSIST
            K1 = al(128)
            K2 = al(128)
            for dst, src, kk in ((K1, k1T, "K1"), (K2, k2T, "K2")):
                sv, sk, e_ = load_cast(None, src, 128, [128])
                cp(e_, dst, sv, [sk], [kk])
            gFin = al(1024, F32)
            dma(gFin, final_norm.partition_broadcast(128), w=["gFin"])
            cT = al(8 * TB).rearrange("p (k n) -> p k n", k=8)
            qT = al(16 * TB).rearrange("p (u n) -> p u n", u=16)
            v1T = al(TB, F32)
            t2T = al(TB, F32)
            bT = al(TB, F32)
            Gb = al(TB * 128).rearrange("p (t j) -> p t j", t=TB)
            hfin = [al(1024, F32) for _ in range(1)]
            junk = al(1024)
            ssq = [al(1, F32)]
            dbgb = al(1024, F32) if dbg else None
            GRP = 8
            Qrep = [[al(GRP * 128) for _ in range(2)] for _ in range(2)]
            Ma4 = [al(512) for _ in range(4)]
            Rr4 = [al(512) for _ in range(4)]
            Dd4 = [al(512, F32) for _ in range(3)]
            Da4 = [al(512, F32) for _ in range(3)]
            Mk4 = [al(512) for _ in range(3)]
            Ex4 = [al(512) for _ in range(3)]
            UTs = [al(1024).rearrange("p (k i) -> p k i", k=8) for _ in range(4)]
            VBs = [al(1024) for _ in range(4)]
            Hg = [al(TB, F32) for _ in range(3)]
            GH = [al(TB) for _ in range(3)]

            for b in range(NB):
                tb0 = b * TB
                dma(cT, CT[:, :, tb0:tb0 + TB], r=["CT"], w=["cT"])
                dma(qT.rearrange("p u n -> p (u n)"), QTs[b], r=["QTs"], w=["qT"])
                dma(v1T, SVs[b, 0], r=["SVs"], w=["v1T"])
                dma(t2T, SVs[b, 1], r=["SVs"], w=["t2T"])
                dma(bT, SVs[b, 2], r=["SVs"], w=["bT"])
                if LIM == 51:
                    raise _Stop()
                R1B = [0, 1, 6]
                R2B = [2, 3, 7]
                NBT = TB // 4

                def rep(b_):
                    t0_ = b_ * 4
                    gi = t0_ // GRP
                    qb = gi % 2
                    if t0_ % GRP == 0:
                        for side in range(2):
                            src = qT[:, side:16:2, gi * GRP:(gi + 1) * GRP].rearrange("p h t -> p t h").unsqueeze(3).to_broadcast([128, GRP, 8, 16])
                            dstq = Qrep[qb][side].rearrange("p (t h a) -> p t h a", t=GRP, h=8)
                            act(dstq, src, AF.Copy, ["qT"], ["Qrep%d%d" % (qb, side)])
                    r1, r2 = R1B[b_ % 3], R2B[b_ % 3]
                    for ti in range(4):
                        tq = (t0_ % GRP) + ti
                        mm(banks[r1][:, ti * 128:(ti + 1) * 128], Qrep[qb][0][:, tq * 128:(tq + 1) * 128], K1, True, True, ["Qrep%d0" % qb, "K1"], [bk[r1]])
                        mm(banks[r2][:, ti * 128:(ti + 1) * 128], Qrep[qb][1][:, tq * 128:(tq + 1) * 128], K2, True, True, ["Qrep%d1" % qb, "K2"], [bk[r2]])

                def mid(b_):
                    t0_ = b_ * 4
                    r1, r2 = R1B[b_ % 3], R2B[b_ % 3]
                    i4, i3 = b_ % 4, b_ % 3
                    v3 = lambda a_: a_.rearrange("p (t n) -> p t n", t=4)
                    bc = lambda a_: a_[:, t0_:t0_ + 4].unsqueeze(2).to_broadcast([128, 4, 128])
                    tt("dve", v3(Da4[i3]), v3(banks[r2][:, :]), bc(bT), ALU.add, [bk[r2], "bT"], ["Da4_%d" % i3])
                    tt("dve", v3(Dd4[i3]), v3(banks[r2][:, :]), bc(v1T), ALU.add, [bk[r2], "v1T"], ["Dd4_%d" % i3])
                    act(Ex4[i3], Da4[i3], AF.Exp, ["Da4_%d" % i3], ["Ex4_%d" % i3])
                    tt("dve", v3(Ma4[i4]), v3(banks[r1][:, :]), bc(v1T), ALU.is_equal, [bk[r1], "v1T"], ["Ma4_%d" % i4])
                    tt("dve", v3(Mk4[i3]), v3(Dd4[i3]), bc(t2T), ALU.is_ge, ["Dd4_%d" % i3, "t2T"], ["Mk4_%d" % i3])
                    tt("pool", Rr4[i4], Mk4[i3], Ex4[i3], ALU.mult, ["Mk4_%d" % i3, "Ex4_%d" % i3], ["Rr4_%d" % i4])

                def gmm(b_):
                    t0_ = b_ * 4
                    i4 = b_ % 4
                    pg = 4 + b_ % 2
                    for ti in range(4):
                        mm(banks[pg][:, ti * 128:(ti + 1) * 128], Ma4[i4][:, ti * 128:(ti + 1) * 128], Rr4[i4][:, ti * 128:(ti + 1) * 128], True, True,
                           ["Ma4_%d" % i4, "Rr4_%d" % i4], [bk[pg]])
                    cp("act", Gb[:, t0_:t0_ + 4, :], banks[pg][:, :].rearrange("p (t j) -> p t j", t=4), [bk[pg], "Gb"], ["Gb"])

                rep(0)
                rep(1)
                rep(2)
                GDEL = 2
                for b_ in range(NBT):
                    mid(b_)
                    if b_ + 3 < NBT:
                        rep(b_ + 3)
                    if b_ - GDEL >= 0:
                        gmm(b_ - GDEL)
                for b_ in range(max(0, NBT - GDEL), NBT):
                    gmm(b_)
                if LIM == 52:
                    raise _Stop()

                def loads(j):
                    ub = j % 4
                    dma(UTs[ub], UT[j].rearrange("p (k i) -> p k i", k=8), r=["UT"], w=["UTs%d" % ub], q="sp")
                    dma(VBs[ub], VB[j], r=["VB"], w=["VBs%d" % ub], q="sp")

                def umm(j):
                    ub = j % 4
                    ph = 4 + (j % 3)
                    for kc in range(8):
                        mm(banks[ph][:, 0:TB], UTs[ub][:, kc, :], cT[:, kc, :], kc == 0, kc == 7, ["UTs%d" % ub, "cT"], [bk[ph]])

                def rest(j):
                    ub = j % 4
                    ph = 4 + (j % 3)
                    hb_ = j % 3
                    act(Hg[hb_], banks[ph][:, 0:TB], AF.Gelu, [bk[ph]], ["Hg%d" % hb_])
                    gb_ = j % 3
                    tt("dve", GH[gb_], Hg[hb_], Gb[:, :, j], ALU.mult, ["Hg%d" % hb_, "Gb"], ["GH%d" % gb_])
                    for t2 in range(TB // 128):
                        for half in range(2):
                            pb = t2 * 2 + half
                            mm(banks[pb][:, :], GH[gb_][:, t2 * 128:(t2 + 1) * 128], VBs[ub][:, half * 512:(half + 1) * 512], j == 0, j == 127,
                               ["GH%d" % gb_, "VBs%d" % ub], [bk[pb]])

                loads(0)
                loads(1)
                loads(2)
                umm(0)
                umm(1)
                for j in range(128):
                    if j + 3 < 128:
                        loads(j + 3)
                    if j + 2 < 128:
                        umm(j + 2)
                    rest(j)
                if LIM == 53:
                    raise _Stop()
                for t2 in range(TB // 128):
                    t0 = tb0 + t2 * 128
                    hf = hfin[0]
                    hk = "hfin0"
                    dma(hf, H[t0:t0 + 128, :], r=["H"], w=[hk])
                    if dbg:
                        for half in range(2):
                            cp("dve", dbgb[:, half * 512:(half + 1) * 512], banks[t2 * 2 + half][:, :], [bk[t2 * 2 + half], "dbgb"], ["dbgb"])
                        dma(dbg_out["d_pe"][t0:t0 + 128, :], dbgb, r=["dbgb"], w=["d_pe"])
                    for half in range(2):
                        cs = slice(half * 512, (half + 1) * 512)
                        tt("dve", hf[:, cs], banks[t2 * 2 + half][:, :], hf[:, cs], ALU.add, [bk[t2 * 2 + half], hk], [hk])
                    act(junk, hf, AF.Square, [hk], ["junk", "ssq0"], scale=1.0 / 32.0, accum=ssq[0])
                    act(ssq[0], ssq[0], AF.Sqrt, ["ssq0"], ["ssq0"], bias=1e-6, scale=1.0)
                    recip(ssq[0], ssq[0], ["ssq0"], ["ssq0"])
                    stt("dve", hf, hf, ssq[0], gFin, ALU.mult, ALU.mult, [hk, "ssq0", "gFin"], [hk])
                    fin_ops.append(dma(y[t0:t0 + 128, :], hf, r=[hk], w=["y"], q="sp"))
        except _Stop:
            pass
        P.emit(es, fin_ops)
    return nc


fin_ops = []


def host_consts(S):
    bf = ml_dtypes.bfloat16
    pos = np.arange(S, dtype=np.float32)
    inv = np.power(np.float32(500000.0), -np.arange(8, dtype=np.float32) * 2.0 / 16.0).astype(np.float32)
    ang = pos[None, :] * inv[:, None]
    cosv, sinv = np.cos(ang).astype(np.float32), np.sin(ang).astype(np.float32)
    ropec = np.ones((128, S), np.float32)
    ropes = np.zeros((128, S), np.float32)
    for hb in (0, 64):
        ropec[hb:hb + 8] = cosv
        ropec[hb + 8:hb + 16] = cosv
        ropes[hb:hb + 8] = -sinv
        ropes[hb + 8:hb + 16] = sinv
    p = np.arange(128)[:, None]
    f = np.arange(512)[None, :]
    masks = np.zeros((8, 128, 512), np.float32)
    for d in range(4):
        masks[d] = np.where(128 * d + p > f, 0.0, NEG)
        masks[4 + d] = np.where(128 * d + p <= f, 0.0, NEG)
    n = np.arange(256)[:, None]
    t = np.arange(S)[None, :]
    cmaskT = np.where((16 * n + 31 <= t) & (n < (S - 32) // 16 + 1), 0.0, NEG)
    esel = (np.arange(64)[:, None] == (np.arange(S)[None, :] // 64)).astype(np.float32)
    tq = np.arange(S)[:, None]
    j = np.arange(64)[None, :]
    cur = tq // 64
    forced = (j == 0) | (j == cur) | (j == cur - 1)
    valid = (j <= cur) & (j < S // 64)
    csel = np.where(valid, np.where(forced, 1e4, 0.0), -1e30).astype(np.float32)
    ci = np.arange(256)[:, None] * 16
    sj = np.arange(64)[None, :] * 64
    ovl = ((ci < sj + 64) & (ci + 32 > sj)).astype(np.float32)
    ovl[(S - 32) // 16 + 1:] = 0.0
    return dict(ropec=ropec, ropes=ropes, masks=masks.astype(bf), cmaskT=cmaskT.astype(bf), esel=esel.astype(bf),
                csel=csel, ovl=ovl.astype(bf))


def host_weights(inp):
    f = lambda a: np.ascontiguousarray(np.asarray(a, dtype=np.float32))
    w_in = f(inp["w_in"])[0]
    roped_cols = np.concatenate([np.arange(c0, c0 + 128) for c0 in FM_COLS[:N_ROPED]])
    perm = np.arange(128)
    for hb in (0, 64):
        perm[hb:hb + 8] = np.arange(hb + 8, hb + 16)
        perm[hb + 8:hb + 16] = np.arange(hb, hb + 8)
    rot_cols = np.concatenate([roped_cols[i * 128:(i + 1) * 128][perm] for i in range(N_ROPED)])
    w = dict(
        attn_norm=f(inp["attn_norm"])[0], w_in=w_in, w_rot=np.ascontiguousarray(w_in[:, rot_cols]),
        lam4=np.stack([f(inp["da_lambda_q1"])[0], f(inp["da_lambda_k1"])[0], f(inp["da_lambda_q2"])[0], f(inp["da_lambda_k2"])[0]]),
        subln=f(inp["da_subln"])[0],
        peT_k=np.ascontiguousarray(f(inp["cmp_pe_k"])[0].T), peT_v=np.ascontiguousarray(f(inp["cmp_pe_v"])[0].T),
        w1k=f(inp["cmp_w1_k"])[0], w1v=f(inp["cmp_w1_v"])[0], w2k=f(inp["cmp_w2_k"])[0], w2v=f(inp["cmp_w2_v"])[0],
        p_da=f(inp["p_da"])[0], p_nsa=f(inp["p_nsa"])[0], w_o=f(inp["w_o"])[0], ffn_norm=f(inp["ffn_norm"])[0],
        wq=f(inp["peer_wq"])[0], k1T=np.ascontiguousarray(f(inp["peer_k1"])[0].T), k2T=np.ascontiguousarray(f(inp["peer_k2"])[0].T),
        peer_u=f(inp["peer_u"])[0], peer_v=f(inp["peer_v"])[0], final_norm=f(inp["final_norm"]),
    )
    return w


def run(inputs, S, dbg=False, cores=None, LIM=99):
    del fin_ops[:]
    xin = np.asarray(inputs["x"], dtype=np.float32)
    B = xin.shape[0]
    nc = build(S, dbg, LIM)
    shared = host_weights(inputs)
    shared.update(host_consts(S))
    in_maps = []
    for b in range(B):
        m = dict(shared)
        m["x"] = np.ascontiguousarray(xin[b])
        in_maps.append(m)
    res = run_bass_kernel_spmd(nc, in_maps, core_ids=list(range(B)))
    return res


def kernel(**inputs):
    S = inputs["x"].shape[1]
    res = run(inputs, S)
    return np.stack([np.asarray(r["y"], dtype=np.float32) for r in res.results], axis=0)
```

```python
import contextlib
import math
import numpy as np
import ml_dtypes
import concourse.bass as bass
import concourse.mybir as mybir
from concourse.bass_utils import run_bass_kernel_spmd

F32 = mybir.dt.float32
BF16 = mybir.dt.bfloat16
ALU = mybir.AluOpType
AF = mybir.ActivationFunctionType
AX = mybir.AxisListType

D = 1024
NEG = -30000.0


class _Op:
    __slots__ = ("eng", "fn", "deps", "marked", "val", "is_dma", "sem_i")

    def __init__(self, eng, fn, is_dma):
        self.eng = eng
        self.fn = fn
        self.deps = []
        self.marked = False
        self.val = None
        self.is_dma = is_dma
        self.sem_i = None


class Prog:
    COMPUTE = ("pe", "act", "dve", "pool")
    NDMA_SEM = 8

    def __init__(self, nc):
        self.nc = nc
        self.ops = {e: [] for e in ("pe", "act", "dve", "pool", "sp")}
        self.state = {}
        self.bar = {e: [] for e in self.ops}
        self.dmas_since_bar = []

    suffix = None
    shared = ()

    def _k(self, k):
        if self.suffix is None or k in self.shared or k.startswith("bk") or k.startswith("stg"):
            return k
        return k + self.suffix

    def _add(self, eng, fn, reads, writes, is_dma):
        if self.suffix is not None:
            reads = [self._k(k) for k in reads]
            writes = [self._k(k) for k in writes]
        op = _Op(eng, fn, is_dma)
        deps = list(self.bar[eng])
        self.bar[eng] = []
        for k in reads:
            st = self.state.get(k)
            if st is not None and st[0] is not None:
                deps.append(st[0])
        for k in writes:
            st = self.state.get(k)
            if st is not None:
                if st[0] is not None:
                    deps.append(st[0])
                deps.extend(st[1].values())
                deps.extend(st[2])
        seen = set()
        for d in deps:
            if id(d) in seen:
                continue
            seen.add(id(d))
            if d.eng == "pe" and eng == "pe" and not d.is_dma and not is_dma:
                continue
            op.deps.append(d)
            d.marked = True
        for k in reads:
            st = self.state.setdefault(k, [None, {}, []])
            if is_dma:
                st[2].append(op)
            else:
                st[1][eng] = op
        for k in writes:
            self.state[k] = [op, {}, []]
        self.ops[eng].append(op)
        if is_dma:
            self.dmas_since_bar.append(op)
        return op

    def op(self, eng, fn, reads=(), writes=()):
        return self._add(eng, fn, reads, writes, False)

    def dma(self, eng, fn, reads=(), writes=()):
        op = self._add(eng, fn, reads, writes, True)
        op.marked = True
        return op

    def barrier(self):
        deps = []
        for e, lst in self.ops.items():
            for o in reversed(lst):
                if not o.is_dma:
                    deps.append(o)
                    break
        deps.extend(self.dmas_since_bar)
        self.dmas_since_bar = []
        for e in self.bar:
            self.bar[e] = self.bar[e] + deps
        self.state = {}

    def emit(self, es, final_ops):
        nc = self.nc
        csem = {e: es.enter_context(nc.semaphore("s_" + e)) for e in self.COMPUTE}
        dsem = {e: [es.enter_context(nc.semaphore("d_%s%d" % (e, i))) for i in range(self.NDMA_SEM)]
                for e in ("sp", "pool")}
        for e, lst in self.ops.items():
            cnt = 0
            dcnt = 0
            for o in lst:
                if o.is_dma:
                    o.sem_i = dcnt % self.NDMA_SEM
                    o.val = 16 * (dcnt // self.NDMA_SEM + 1)
                    dcnt += 1
                elif o.marked:
                    cnt += 1
                    o.val = cnt
        block = es.enter_context(nc.Block())

        def run(ename, e):
            waited = {}
            for o in self.ops[ename]:
                waits = {}
                for d in o.deps:
                    if d.is_dma:
                        key = (d.eng, d.sem_i)
                        sem = dsem[d.eng][d.sem_i]
                    else:
                        key = (d.eng, None)
                        sem = csem[d.eng]
                    if waited.get(key, 0) >= d.val:
                        continue
                    if key not in waits or waits[key][1] < d.val:
                        waits[key] = (sem, d.val)
                if o.is_dma and o.val > 16:
                    key = (ename, o.sem_i)
                    if waited.get(key, 0) < o.val - 16:
                        if key not in waits or waits[key][1] < o.val - 16:
                            waits[key] = (dsem[ename][o.sem_i], o.val - 16)
                for key, (sem, v) in waits.items():
                    e.wait_ge(sem, v)
                    waited[key] = v
                ins = o.fn(e)
                if o.is_dma:
                    ins.then_inc(dsem[ename][o.sem_i], 16)
                elif o.marked:
                    ins.then_inc(csem[ename], 1)
            if ename == "sp":
                for o in final_ops:
                    e.wait_ge(dsem[o.eng][o.sem_i], o.val)

        @block.sync
        def _(e):
            run("sp", e)

        @block.tensor
        def _(e):
            run("pe", e)

        @block.scalar
        def _(e):
            run("act", e)

        @block.vector
        def _(e):
            run("dve", e)

        @block.gpsimd
        def _(e):
            run("pool", e)


C_QDA, C_KDA, C_VDA, C_QN, C_KC, C_VC, C_KS, C_VS, C_KW, C_VW, C_GN, C_GM = (
    0, 512, 1024, 1536, 2048, 2176, 2304, 2432, 2560, 2688, 2816, 2840)
FM_COLS = [C_QDA + 128 * i for i in range(4)] + [C_KDA + 128 * i for i in range(4)] + \
          [C_QN + 128 * i for i in range(4)] + [C_KC, C_KS, C_KW, C_VC]
N_ROPED = 15
LAMBDA_INIT = 0.8 - 0.6 * math.exp(0.0)


class _Stop(Exception):
    pass


def build(S, dbg=False, LIM=99):
    NT = S // 128
    NQC = S // 512
    NCMP = (S - 32) // 16 + 1
    TB = 256
    NB = S // TB
    nc = bass.Bass("TRN2", target_bir_lowering=False)

    def din(name, shape, dt=F32):
        return nc.dram_tensor(name, list(shape), dt, kind="ExternalInput").ap()

    def dscr(name, shape, dt):
        return nc.dram_tensor(name, list(shape), dt, kind="Internal").ap()

    x = din("x", [S, D])
    attn_norm = din("attn_norm", [D])
    w_in = din("w_in", [D, 4888])
    w_rot = din("w_rot", [D, 1920])
    lam4 = din("lam4", [4, 64])
    subln = din("subln", [128])
    peT_k = din("peT_k", [64, 32])
    peT_v = din("peT_v", [64, 32])
    w1k = din("w1k", [2048, 64])
    w1v = din("w1v", [2048, 64])
    w2k = din("w2k", [64, 64])
    w2v = din("w2v", [64, 64])
    p_da = din("p_da", [512, D])
    p_nsa = din("p_nsa", [512, D])
    w_o = din("w_o", [D, D])
    ffn_norm = din("ffn_norm", [D])
    wq = din("wq", [D, 2048])
    k1T = din("k1T", [128, 128])
    k2T = din("k2T", [128, 128])
    peer_u = din("peer_u", [16384, D])
    peer_v = din("peer_v", [16384, D])
    final_norm = din("final_norm", [D])
    ropec = din("ropec", [128, S])
    ropes = din("ropes", [128, S])
    masks = din("masks", [8, 128, 512], BF16)
    cmaskT = din("cmaskT", [256, S], BF16)
    esel = din("esel", [64, S], BF16)
    csel = din("csel", [S, 64])
    ovl = din("ovl", [256, 64], BF16)
    y = nc.dram_tensor("y", [S, D], F32, kind="ExternalOutput").ap()

    FM = dscr("FM", [16, 128, S], BF16)
    VT = dscr("VT", [S, 768], BF16)
    GN = dscr("GN", [S, 24], F32)
    GM = dscr("GM", [S, 2048], F32)
    H = dscr("H", [S, D], F32)
    CT = dscr("CT", [128, 8, S], BF16)
    YDA = dscr("YDA", [S, 512], F32)
    QTs = dscr("QTs", [S // 256, 128, 16 * 256], BF16)
    SVs = dscr("SVs", [S // 256, 3, 128, 256], F32)
    UT = dscr("UT", [128, 128, 1024], BF16)
    VB = dscr("VB", [128, 128, 1024], BF16)
    dbg_out = {}
    if dbg:
        dbg_out["d_fm"] = nc.dram_tensor("d_fm", [16, 128, S], BF16, kind="ExternalOutput").ap()
        dbg_out["d_h"] = nc.dram_tensor("d_h", [S, D], F32, kind="ExternalOutput").ap()
        dbg_out["d_yy"] = nc.dram_tensor("d_yy", [S, 1024], F32, kind="ExternalOutput").ap()
        dbg_out["d_pe"] = nc.dram_tensor("d_pe", [S, D], F32, kind="ExternalOutput").ap()

    es = contextlib.ExitStack()
    with es:
        ARENA_N = 104000
        arena = es.enter_context(nc.sbuf_tensor("arena", [128, ARENA_N], BF16))
        banks = [es.enter_context(nc.psum_tensor("bk%d" % i, [128, 512], F32)) for i in range(8)]
        bk = ["bk%d" % i for i in range(8)]
        off = [0]

        def al(n, dt=BF16):
            k = n * (2 if dt == F32 else 1)
            assert off[0] + k <= ARENA_N, ("arena overflow", off[0], k)
            a = arena[:, off[0]:off[0] + k]
            off[0] += (k + 15) // 16 * 16
            return a.bitcast(dt) if dt != BF16 else a

        P = Prog(nc)
        dq = ["sp", "pool"]
        dqi = [0]

        def dma(out, in_, r=(), w=(), q=None, slow=False):
            if q is None:
                q = dq[dqi[0] % 2]
                dqi[0] += 1
            if slow:
                return P.dma(q, lambda e: e.dma_start(out=out, in_=in_, allow_slow_non_contiguous=True), reads=r, writes=w)
            return P.dma(q, lambda e: e.dma_start(out=out, in_=in_), reads=r, writes=w)

        def mm(out, lhsT, rhs, st, sp_, r, w):
            return P.op("pe", lambda e: e.matmul(out, lhsT=lhsT, rhs=rhs, start=st, stop=sp_), reads=r, writes=w)

        def tr(out, in_, ident_, r, w):
            return P.op("pe", lambda e: e.transpose(out=out, in_=in_, identity=ident_), reads=r, writes=w)

        def act(out, in_, func, r, w, bias=None, scale=None, accum=None):
            kw = {}
            if bias is not None:
                kw["bias"] = bias
            if scale is not None:
                kw["scale"] = scale
            if accum is not None:
                kw["accum_out"] = accum
            return P.op("act", lambda e: e.activation(out=out, in_=in_, func=func, **kw), reads=r, writes=w)

        def ts(eng, out, in0, s1, s2, op0, op1, r, w):
            if op1 is None:
                return P.op(eng, lambda e: e.tensor_scalar(out=out, in0=in0, scalar1=s1, scalar2=None, op0=op0), reads=r, writes=w)
            return P.op(eng, lambda e: e.tensor_scalar(out=out, in0=in0, scalar1=s1, scalar2=s2, op0=op0, op1=op1), reads=r, writes=w)

        def tt(eng, out, in0, in1, op, r, w):
            return P.op(eng, lambda e: e.tensor_tensor(out=out, in0=in0, in1=in1, op=op), reads=r, writes=w)

        def stt(eng, out, in0, scalar, in1, op0, op1, r, w):
            return P.op(eng, lambda e: e.scalar_tensor_tensor(out=out, in0=in0, scalar=scalar, in1=in1, op0=op0, op1=op1), reads=r, writes=w)

        def cp(eng, out, in_, r, w):
            if eng == "act":
                return act(out, in_, AF.Copy, r, w)
            return P.op(eng, lambda e: e.tensor_copy(out=out, in_=in_), reads=r, writes=w)

        def recip(out, in_, r, w):
            return P.op("dve", lambda e: e.reciprocal(out=out, in_=in_), reads=r, writes=w)

        def memset(eng, out, v, w):
            return P.op(eng, lambda e: e.memset(out, v), writes=w)

        try:
            identf = al(128, F32)
            ident = al(128)
            memset("pool", identf, 0.0, ["identf"])
            P.op("pool", lambda e: e.affine_select(out=identf, in_=identf, pattern=[[-1, 128]], compare_op=ALU.not_equal,
                                                   fill=1.0, base=0, channel_multiplier=1), reads=["identf"], writes=["identf"])
            cp("dve", ident, identf, ["identf"], ["ident"])
            stg = [al(1024, F32) for _ in range(2)]
            stgk = ["stg0", "stg1"]
            stgi = [0]
            PERSIST = off[0]

            def load_cast(dst, src, n_p, shape_free, eng=None, p0=0):
                i = stgi[0] % 2
                stgi[0] += 1
                n = int(np.prod(shape_free))
                assert n <= 1024
                sv = stg[i][p0:p0 + n_p, 0:n]
                if len(shape_free) == 2:
                    sv = sv.rearrange("p (a b) -> p a b", a=shape_free[0])
                dma(sv, src, w=[stgk[i]])
                e_ = eng or ("dve" if i == 0 else "pool")
                return sv, stgk[i], e_

            def load_w(dst, dkey, src, ncols, kch=8):
                srcv = src.rearrange("(kc p) n -> p kc n", p=128)
                step = 1024 // kch
                first = True
                for c0 in range(0, ncols, step):
                    c1 = min(ncols, c0 + step)
                    sv, sk, e_ = load_cast(None, srcv[:, :, c0:c1], 128, [kch, c1 - c0])
                    cp(e_, dst[:, :, c0:c1], sv, [sk] + ([] if first else [dkey]), [dkey])
                    first = False

            u_v = peer_u.rearrange("(i j) d -> j i d", j=128)
            v_v = peer_v.rearrange("(i j) d -> j i d", j=128)
            t_state = {"j": 0, "s1": 0}

            def t_stage1(j):
                if j >= 128 or t_state["s1"] > j:
                    return
                t_state["s1"] = j + 1
                a = (2 * j) % 4
                b = (2 * j + 1) % 4
                dma(t_f[a], u_v[j], w=["t_f%d" % a], q="sp")
                dma(t_f[b], v_v[j], w=["t_f%d" % b], q="sp")
                cp("act" if j % 2 else "dve", t_b[a], t_f[a], ["t_f%d" % a], ["t_b%d" % a])
                cp("dve" if j % 2 else "act", t_b[b], t_f[b], ["t_f%d" % b], ["t_b%d" % b])
                dma(VB[j], t_b[b], r=["t_b%d" % b], w=["VB"], q="sp")

            def t_step():
                j = t_state["j"]
                if j >= 128:
                    return
                t_state["j"] = j + 1
                t_stage1(j)
                t_stage1(j + 1)
                a = (2 * j) % 4
                pb = 6 + (j % 2)
                pbv = banks[pb][:, :].bitcast(BF16)
                for kc in range(8):
                    tr(pbv[:, kc * 128:(kc + 1) * 128], t_b[a][:, kc * 128:(kc + 1) * 128], ident, ["t_b%d" % a, "ident"], [bk[pb]])
                o = j % 2
                cp("act" if j % 2 else "dve", t_o[o], pbv, [bk[pb]], ["t_o%d" % o])
                dma(UT[j], t_o[o], r=["t_o%d" % o], w=["UT"], q="sp")

            Wfm = al(8 * 2048).rearrange("p (k n) -> p k n", k=8)
            Wrot = al(8 * 1920).rearrange("p (k n) -> p k n", k=8)
            Wv = al(8 * 768).rearrange("p (k n) -> p k n", k=8)
            Wgn = al(8 * 24).rearrange("p (k n) -> p k n", k=8)
            Wgm = al(8 * 2048).rearrange("p (k n) -> p k n", k=8)
            for i, c0 in enumerate(FM_COLS):
                load_w(Wfm[:, :, i * 128:(i + 1) * 128], "Wfm%d" % i, w_in[:, c0:c0 + 128], 128)
            load_w(Wrot, "Wrot", w_rot, 1920)
            load_w(Wv[:, :, 0:512], "Wv0", w_in[:, C_VDA:C_VDA + 512], 512)
            load_w(Wv[:, :, 512:640], "Wv1", w_in[:, C_VS:C_VS + 128], 128)
            load_w(Wv[:, :, 640:768], "Wv2", w_in[:, C_VW:C_VW + 128], 128)
            load_w(Wgn, "Wgn", w_in[:, C_GN:C_GN + 24], 24)
            load_w(Wgm, "Wgm", w_in[:, C_GM:C_GM + 2048], 2048)
            WFMK = ["Wfm%d" % i for i in range(16)]
            gA = al(8, F32)
            dma(gA, attn_norm.rearrange("(k p) -> p k", p=128), w=["gA"], slow=True)
            xs = [al(1024, F32) for _ in range(2)]
            xb = [al(1024) for _ in range(2)]
            junk = al(1024, F32)
            ssq = [al(1, F32) for _ in range(2)]
            aT = al(8 * 512).rearrange("p (k n) -> p k n", k=8)
            rc = al(512, F32)
            rs_ = al(512, F32)
            tA = [al(512, F32) for _ in range(2)]
            tB = [al(512, F32) for _ in range(2)]
            fmo = [al(512) for _ in range(3)]
            vo = [al(768) for _ in range(2)]
            gno = [al(24, F32) for _ in range(2)]
            gmo = [al(2048, F32) for _ in range(2)]
            t_f = [al(1024, F32) for _ in range(4)]
            t_b = [al(1024) for _ in range(4)]
            t_o = [al(1024) for _ in range(2)]
            T_PER_TILE = -(-128 // (NQC * 16))

            def rms_to_T(xt_ap, xkey, gain, gkey, dstT, dkey, i2, pbank):
                act(junk, xt_ap, AF.Square, [xkey], ["junk", "ssq%d" % i2], scale=1.0 / 32.0, accum=ssq[i2])
                act(ssq[i2], ssq[i2], AF.Sqrt, ["ssq%d" % i2], ["ssq%d" % i2], bias=1e-6, scale=1.0)
                recip(ssq[i2], ssq[i2], ["ssq%d" % i2], ["ssq%d" % i2])
                act(xb[i2], xt_ap, AF.Copy, [xkey, "ssq%d" % i2], ["xb%d" % i2], scale=ssq[i2])
                pv = banks[pbank][:, :].bitcast(BF16)
                for kc in range(8):
                    tr(pv[:, kc * 128:(kc + 1) * 128], xb[i2][:, kc * 128:(kc + 1) * 128], ident, ["xb%d" % i2, "ident"], [bk[pbank]])
                tt("dve", dstT, pv.rearrange("p (k n) -> p k n", k=8), gain.unsqueeze(2).to_broadcast([128, 8, 128]),
                   ALU.mult, [bk[pbank], gkey, dkey], [dkey])

            fm_cnt = 0
            for c in range(NQC):
                q0 = c * 512
                dma(rc, ropec[:, q0:q0 + 512], w=["rc"])
                dma(rs_, ropes[:, q0:q0 + 512], w=["rs"])
                for t4 in range(4):
                    t0 = q0 + t4 * 128
                    i2 = t4 % 2
                    dma(xs[i2], x[t0:t0 + 128, :], w=["xs%d" % i2])
                    rms_to_T(xs[i2], "xs%d" % i2, gA, "gA", aT[:, :, t4 * 128:(t4 + 1) * 128], "aT", i2, 6 + i2)
                for i in range(16):
                    pz = (2 * i) % 4
                    pr = (2 * i + 1) % 4
                    for kc in range(8):
                        mm(banks[pz][:, :], Wfm[:, kc, i * 128:(i + 1) * 128], aT[:, kc, :], kc == 0, kc == 7, ["aT", WFMK[i]], [bk[pz]])
                    fo = fm_cnt % 3
                    fm_cnt += 1
                    if i < N_ROPED:
                        for kc in range(8):
                            mm(banks[pr][:, :], Wrot[:, kc, i * 128:(i + 1) * 128], aT[:, kc, :], kc == 0, kc == 7, ["aT", "Wrot"], [bk[pr]])
                        a2 = i % 2
                        tt("dve", tA[a2], banks[pz][:, :], rc, ALU.mult, [bk[pz], "rc"], ["tA%d" % a2])
                        tt("dve", tB[a2], banks[pr][:, :], rs_, ALU.mult, [bk[pr], "rs"], ["tB%d" % a2])
                        tt("pool", fmo[fo], tA[a2], tB[a2], ALU.add, ["tA%d" % a2, "tB%d" % a2], ["fmo%d" % fo])
                    else:
                        cp("act", fmo[fo], banks[pz][:, :], [bk[pz]], ["fmo%d" % fo])
                    dma(FM[i, :, q0:q0 + 512], fmo[fo], r=["fmo%d" % fo], w=["FM"])
                    for _ in range(T_PER_TILE):
                        t_step()
                    if dbg:
                        dma(dbg_out["d_fm"][i, :, q0:q0 + 512], fmo[fo], r=["fmo%d" % fo], w=["d_fm"])
                for t4 in range(4):
                    t0 = q0 + t4 * 128
                    i2 = t4 % 2
                    lcol = slice(t4 * 128, (t4 + 1) * 128)
                    for (c0, c1, pb) in ((0, 512, 4), (512, 768, 5)):
                        for kc in range(8):
                            mm(banks[pb][:, 0:c1 - c0], aT[:, kc, lcol], Wv[:, kc, c0:c1], kc == 0, kc == 7, ["aT", "Wv0", "Wv1", "Wv2"], [bk[pb]])
                        cp("act", vo[i2][:, c0:c1], banks[pb][:, 0:c1 - c0], [bk[pb], "vo%d" % i2], ["vo%d" % i2])
                    dma(VT[t0:t0 + 128, :], vo[i2], r=["vo%d" % i2], w=["VT"])
                    for kc in range(8):
                        mm(banks[5][:, 0:24], aT[:, kc, lcol], Wgn[:, kc, :], kc == 0, kc == 7, ["aT", "Wgn"], [bk[5]])
                    act(gno[i2], banks[5][:, 0:24], AF.Sigmoid, [bk[5]], ["gno%d" % i2])
                    dma(GN[t0:t0 + 128, :], gno[i2], r=["gno%d" % i2], w=["GN"])
                    for g4 in range(4):
                        pb = g4 % 4
                        for kc in range(8):
                            mm(banks[pb][:, :], aT[:, kc, lcol], Wgm[:, kc, g4 * 512:(g4 + 1) * 512], kc == 0, kc == 7, ["aT", "Wgm"], [bk[pb]])
                        act(gmo[i2][:, g4 * 512:(g4 + 1) * 512], banks[pb][:, :], AF.Sigmoid, [bk[pb], "gmo%d" % i2], ["gmo%d" % i2])
                    dma(GM[t0:t0 + 128, :], gmo[i2], r=["gmo%d" % i2], w=["GM"])
            while t_state["j"] < 128:
                t_step()
            P.barrier()
            if LIM == 2:
                raise _Stop()
            off[0] = PERSIST

            SC = [0, 1, 6]
            ACC = [2, 3, 4, 5]
            sc_i = [0]
            ecnt = [0]
            smc = [0]
            MSK = al(8 * 512).rearrange("p (m n) -> p m n", m=8)
            dma(MSK, masks.rearrange("m p n -> p m n"), w=["MSK"])
            E = [al(512) for _ in range(4)]
            sm = al(64, F32)
            PERSIST2 = off[0]

            def smalloc():
                i = smc[0] % 64
                smc[0] += 1
                return sm[:, i:i + 1], "sm%d" % i

            def score_tile(lhsT, rhs, kkeys, mask_ap):
                b = SC[sc_i[0] % 3]
                sc_i[0] += 1
                mm(banks[b][:, :], lhsT, rhs, True, mask_ap is None, kkeys, [bk[b]])
                if mask_ap is not None:
                    mm(banks[b][:, :], ident, mask_ap, False, True, ["ident", "MSK", "CM0", "CM1"], [bk[b]])
                ei = ecnt[0] % 4
                ecnt[0] += 1
                act(E[ei], banks[b][:, :], AF.Exp, [bk[b]], ["E%d" % ei], scale=0.125)
                return E[ei], "E%d" % ei

            LOOK = 2

            def run_jobs(jobs):
                q_ = []
                for (sf, pf, ef) in jobs:
                    q_.append((sf(), pf, ef))
                    if len(q_) > LOOK:
                        Ee, pf0, ef0 = q_.pop(0)
                        if pf0 is not None:
                            pf0(Ee)
                        if ef0 is not None:
                            ef0()
                for (Ee, pf0, ef0) in q_:
                    if pf0 is not None:
                        pf0(Ee)
                    if ef0 is not None:
                        ef0()

            KDA = [al(S) for _ in range(4)]
            for h in range(4):
                dma(KDA[h], FM[4 + h], r=["FM"], w=["KDA%d" % h])
            VDA = al(NT * 4 * 130).rearrange("p (t h d) -> p t h d", t=NT, h=4)
            memset("pool", VDA[:, :, :, 128:130], 1.0, ["VDA"])
            VTv = VT.rearrange("(t p) c -> p t c", p=128)
            for h in range(4):
                dma(VDA[:, :, h, 0:128], VTv[:, :, h * 128:(h + 1) * 128], r=["VT", "VDA"], w=["VDA"])
            lamt = al(256, F32)
            dma(lamt, lam4.rearrange("a d -> (a d)").partition_broadcast(128), w=["lamt"])
            lamp = al(128, F32)
            lam2 = al(4, F32)
            l4 = lamt.rearrange("p (a d) -> p a d", a=4)
            tt("dve", lamp.rearrange("p (a d) -> p a d", a=2), l4[:, 0:4:2, :], l4[:, 1:4:2, :], ALU.mult, ["lamt"], ["lamp"])
            P.op("dve", lambda e: e.reduce_sum(out=lam2[:, 0:2], in_=lamp.rearrange("p (a d) -> p a d", a=2), axis=AX.X), reads=["lamp"], writes=["lam2"])
            act(lam2[:, 0:2], lam2[:, 0:2], AF.Exp, ["lam2"], ["lam2"])
            tt("dve", lam2[:, 2:3], lam2[:, 1:2], lam2[:, 0:1], ALU.subtract, ["lam2"], ["lam2"])
            ts("dve", lam2[:, 3:4], lam2[:, 2:3], -LAMBDA_INIT, None, ALU.add, None, ["lam2"], ["lam2"])
            neglam = lam2[:, 3:4]
            sublnB = al(128, F32)
            dma(sublnB, subln.partition_broadcast(128), w=["sublnB"])
            ts("dve", sublnB, sublnB, 1.0 - LAMBDA_INIT, None, ALU.mult, None, ["sublnB"], ["sublnB"])
            QDA = [[al(512) for _ in range(2)] for _ in range(4)]
            for h in range(4):
                memset("pool", QDA[h][0][64:128, :], 0.0, ["QDA%d_0" % h])
                memset("pool", QDA[h][1][0:64, :], 0.0, ["QDA%d_1" % h])
            o1 = [al(128, F32) for _ in range(4)]
            yda = [al(512, F32) for _ in range(4)]
            junkb = al(128)
            for c in range(NQC):
                q0 = c * 512
                kt_hi = q0 // 128 + 3
                for h in range(4):
                    dma(QDA[h][0][0:64, :], FM[h, 0:64, q0:q0 + 512], r=["FM", "QDA%d_0" % h], w=["QDA%d_0" % h])
                    dma(QDA[h][1][64:128, :], FM[h, 64:128, q0:q0 + 512], r=["FM", "QDA%d_1" % h], w=["QDA%d_1" % h])
                jobs = []
                for h in range(4):
                    for cc in range(2):
                        rows = slice(cc * 64, (cc + 1) * 64)
                        for kt in range(0, kt_hi + 1):
                            d8 = kt - (q0 // 128 - 4)
                            mk = MSK[:, d8, :] if d8 >= 4 else None

                            def sf(h=h, cc=cc, kt=kt, mk=mk):
                                return score_tile(KDA[h][:, kt * 128:(kt + 1) * 128], QDA[h][cc], ["KDA%d" % h, "QDA%d_%d" % (h, cc)], mk)

                            def pf(Ee, h=h, kt=kt):
                                Ea, Ek = Ee
                                for qt in range(4):
                                    qti = q0 // 128 + qt
                                    if kt > qti:
                                        continue
                                    a = ACC[qt]
                                    mm(banks[a][:, 0:129], Ea[:, qt * 128:(qt + 1) * 128], VDA[:, kt, h, 0:129], kt == 0, kt == qti, [Ek, "VDA"], [bk[a]])

                            def ef(h=h, cc=cc):
                                for qt in range(4):
                                    a = ACC[qt]
                                    rz, rzk = smalloc()
                                    recip(rz, banks[a][:, 128:129], [bk[a]], [rzk])
                                    if cc == 0:
                                        ts("dve", o1[qt], banks[a][:, 0:128], rz, None, ALU.mult, None, [bk[a], rzk], ["o1_%d" % qt])
                                    else:
                                        tt("dve", rz, rz, neglam, ALU.mult, [rzk, "lam2"], [rzk])
                                        stt("dve", o1[qt], banks[a][:, 0:128], rz, o1[qt], ALU.mult, ALU.add, [bk[a], rzk, "o1_%d" % qt], ["o1_%d" % qt])
                                        s2, s2k = smalloc()
                                        act(junkb, o1[qt], AF.Square, ["o1_%d" % qt], ["junkb", s2k], scale=1.0 / math.sqrt(128.0), accum=s2)
                                        act(s2, s2, AF.Sqrt, [s2k], [s2k], bias=1e-6, scale=1.0)
                                        recip(s2, s2, [s2k], [s2k])
                                        stt("dve", yda[qt][:, h * 128:(h + 1) * 128], o1[qt], s2, sublnB, ALU.mult, ALU.mult,
                                            ["o1_%d" % qt, s2k, "sublnB", "yda%d" % qt], ["yda%d" % qt])

                            jobs.append((sf, pf, ef if kt == kt_hi else None))
                run_jobs(jobs)
                for qt in range(4):
                    t0 = q0 + qt * 128
                    dma(YDA[t0:t0 + 128, :], yda[qt], r=["yda%d" % qt], w=["YDA"])
            P.barrier()
            if LIM == 3:
                raise _Stop()
            off[0] = PERSIST2

            KSA = [al(S) for _ in range(2)]
            KW = [al(S) for _ in range(2)]
            for g in range(2):
                dma(KSA[g][0:64, :], FM[13, g * 64:(g + 1) * 64, :], r=["FM"], w=["KSA%d" % g])
                dma(KSA[g][64:128, :], esel, r=["KSA%d" % g], w=["KSA%d" % g])
                memset("pool", KW[g][64:128, :], 0.0, ["KW%d" % g])
                dma(KW[g][0:64, :], FM[14, g * 64:(g + 1) * 64, :], r=["FM", "KW%d" % g], w=["KW%d" % g])
            VS = al(NT * 2 * 66).rearrange("p (t g d) -> p t g d", t=NT, g=2)
            VW = al(NT * 2 * 66).rearrange("p (t g d) -> p t g d", t=NT, g=2)
            memset("pool", VS[:, :, :, 64:66], 1.0, ["VS"])
            memset("pool", VW[:, :, :, 64:66], 1.0, ["VW"])
            for g in range(2):
                dma(VS[:, :, g, 0:64], VTv[:, :, 512 + g * 64:512 + (g + 1) * 64], r=["VT", "VS"], w=["VS"])
                dma(VW[:, :, g, 0:64], VTv[:, :, 640 + g * 64:640 + (g + 1) * 64], r=["VT", "VW"], w=["VW"])
            KCMP = [al(256) for _ in range(2)]
            for g in range(2):
                memset("pool", KCMP[g], 0.0, ["KCMP%d" % g])
            VCO = al(2 * 2 * 130).rearrange("p (g t d) -> p g t d", g=2, t=2)
            memset("pool", VCO, 0.0, ["VCO"])
            memset("pool", VCO[:, :, :, 64:65], 1.0, ["VCO"])
            for g in range(2):
                for nt in range(2):
                    dma(VCO[:, g, nt, 65:129], ovl[nt * 128:(nt + 1) * 128, :], r=["VCO"], w=["VCO"])

            mark_B = off[0]
            kcT = al(S)
            vcT = al(S)
            dma(kcT, FM[12], r=["FM"], w=["kcT"])
            dma(vcT, FM[15], r=["FM"], w=["vcT"])
            W1 = {}
            for nm, src in (("k", w1k), ("v", w1v)):
                W1[nm] = al(2048).rearrange("p (l o) -> p l o", l=32)
                srcv = src.rearrange("(l d) o -> d l o", d=64)
                for half in range(2):
                    for lh in range(2):
                        sv, sk, e_ = load_cast(None, srcv[:, lh * 16:(lh + 1) * 16, :], 64, [16, 64], eng="dve", p0=half * 64)
                        kk_ = "W1%s%d" % (nm, half)
                        cp(e_, W1[nm][half * 64:(half + 1) * 64, lh * 16:(lh + 1) * 16, :], sv, [sk] + ([kk_] if lh else []), [kk_])
            W2 = {}
            for nm, src in (("k", w2k), ("v", w2v)):
                W2[nm] = al(64)
                sv, sk, e_ = load_cast(None, src, 64, [64], eng="dve")
                cp(e_, W2[nm][0:64, 0:64], sv, [sk], ["W2" + nm])
            PEb = {}
            for nm, src in (("k", peT_k), ("v", peT_v)):
                PEb[nm] = al(32)
                sv, sk, e_ = load_cast(None, src, 64, [32], eng="dve")
                cp(e_, PEb[nm][0:64, :], sv, [sk], ["PE" + nm])
            cb = al(2, F32)
            hidT = al(256)
            memset("pool", hidT, 0.0, ["hidT"])
            for ki, nm in enumerate(("k", "v")):
                for l in range(32):
                    mm(banks[6][0:64, ki:ki + 1], W1[nm][0:64, l, :], PEb[nm][0:64, l:l + 1], l == 0, l == 31, ["W1%s0" % nm, "PE" + nm], [bk[6]])
                cp("dve", cb[0:64, ki:ki + 1], banks[6][0:64, ki:ki + 1], [bk[6], "cb"], ["cb"])
                srcT = kcT if nm == "k" else vcT
                skey = "kcT" if nm == "k" else "vcT"
                for g in range(2):
                    rows = slice(g * 64, (g + 1) * 64)
                    sview = srcT.rearrange("p (n s) -> p n s", s=16)
                    for l in range(32):
                        rhs = sview[rows, (l // 16):(l // 16) + NCMP, l % 16]
                        mm(banks[7][0:64, 0:NCMP], W1[nm][rows, l, :], rhs, l == 0, l == 31, ["W1%s%d" % (nm, g), skey], [bk[7]])
                    act(hidT[0:64, 0:NCMP], banks[7][0:64, 0:NCMP], AF.Gelu, [bk[7], "cb", "hidT"], ["hidT"], bias=cb[0:64, ki:ki + 1], scale=1.0)
                    if nm == "k":
                        mm(banks[6][0:64, 0:256], W2["k"][0:64, 0:64], hidT[0:64, :], True, True, ["W2k", "hidT"], [bk[6]])
                        cp("dve", KCMP[g][0:64, :], banks[6][0:64, 0:256], [bk[6], "KCMP%d" % g], ["KCMP%d" % g])
                    else:
                        for nt in range(2):
                            mm(banks[6][:, 0:64], hidT[0:64, nt * 128:(nt + 1) * 128], W2["v"][0:64, 0:64], True, True, ["W2v", "hidT"], [bk[6]])
                            cp("dve", VCO[:, g, nt, 0:64], banks[6][:, 0:64], [bk[6], "VCO"], ["VCO"])
            P.barrier()
            if LIM == 4:
                raise _Stop()
            off[0] = mark_B

            PDA = al(4 * 1024).rearrange("p (k n) -> p k n", k=4)
            PNS = al(4 * 1024).rearrange("p (k n) -> p k n", k=4)
            WO = al(8 * 1024).rearrange("p (k n) -> p k n", k=8)
            load_w(PDA, "PDA", p_da, 1024, kch=4)
            load_w(PNS, "PNS", p_nsa, 1024, kch=4)
            load_w(WO, "WO", w_o, 1024, kch=8)
            gF = al(8, F32)
            dma(gF, ffn_norm.rearrange("(k p) -> p k", p=128), w=["gF"], slow=True)
            QA = [al(512) for _ in range(8)]
            for h in range(8):
                memset("pool", QA[h], 0.0, ["QA%d" % h])
            CM = [al(512) for _ in range(2)]
            gn_t = [al(24, F32) for _ in range(4)]
            cs_t = [al(64, F32) for _ in range(4)]
            ycomb = [al(512, F32) for _ in range(4)]
            imp = [al(64, F32) for _ in range(4)]
            m8 = al(16, F32)
            wk64 = al(64, F32)
            mb = al(128, F32)
            memset("pool", mb, 0.0, ["mb"])
            mbT = [al(128) for _ in range(2)]
            mbi = [0]
            ydt = al(512, F32)
            ybf = al(1024)
            yT = al(1024).rearrange("p (k n) -> p k n", k=8)
            gmt = al(1024, F32)
            m1 = al(512, F32)
            m2 = al(512, F32)
            mgb = al(1024)
            mT = al(1024).rearrange("p (k n) -> p k n", k=8)
            hs = al(1024, F32)
            cTo = al(1024).rearrange("p (k n) -> p k n", k=8)
            junk = al(1024)
            xb = [al(1024)]
            ssq = [al(1, F32)]

            if LIM == 40:
                raise _Stop()
            for c in range(NQC):
                q0 = c * 512
                kt_hi = q0 // 128 + 3
                for h in range(8):
                    dma(QA[h][0:64, :], FM[8 + h // 2, (h % 2) * 64:(h % 2) * 64 + 64, q0:q0 + 512], r=["FM"], w=["QA%d" % h])
                for nt in range(2):
                    dma(CM[nt], cmaskT[nt * 128:(nt + 1) * 128, q0:q0 + 512], w=["CM%d" % nt])
                for qt in range(4):
                    t0 = q0 + qt * 128
                    dma(gn_t[qt], GN[t0:t0 + 128, :], r=["GN"], w=["gn%d" % qt])
                    dma(cs_t[qt], csel[t0:t0 + 128, :], w=["cs%d" % qt])

                for g in range(2):
                    jobs = []
                    for hg in range(4):
                        h = g * 4 + hg
                        Ehold = {}

                        def sf0(g=g, h=h):
                            return score_tile(KCMP[g][:, 0:128], QA[h], ["KCMP%d" % g, "QA%d" % h], CM[0])

                        def sf1(g=g, h=h):
                            return score_tile(KCMP[g][:, 128:256], QA[h], ["KCMP%d" % g, "QA%d" % h], CM[1])

                        def pf0(Ee, Ehold=Ehold):
                            Ehold[0] = Ee

                        def pf1(Ee, g=g, h=h, hg=hg, Ehold=Ehold):
                            Es = [Ehold[0], Ee]
                            for qt in range(4):
                                a = ACC[qt]
                                for nt in range(2):
                                    mm(banks[a][:, 0:129], Es[nt][0][:, qt * 128:(qt + 1) * 128], VCO[:, g, nt, 0:129], nt == 0, nt == 1,
                                       [Es[nt][1], "VCO"], [bk[a]])
                                rz, rzk = smalloc()
                                ts("dve", rz, banks[a][:, 64:65], 1e-30, None, ALU.add, None, [bk[a]], [rzk])
                                recip(rz, rz, [rzk], [rzk])
                                ycs = ycomb[qt][:, h * 64:(h + 1) * 64]
                                yk = "yc%d_%d" % (qt, h)
                                ts("dve", ycs, banks[a][:, 0:64], rz, gn_t[qt][:, h * 3:h * 3 + 1], ALU.mult, ALU.mult, [bk[a], rzk, "gn%d" % qt], [yk])
                                if hg == 0:
                                    ts("dve", imp[qt], banks[a][:, 65:129], rz, None, ALU.mult, None, [bk[a], rzk], ["imp%d" % qt])
                                else:
                                    stt("dve", imp[qt], banks[a][:, 65:129], rz, imp[qt], ALU.mult, ALU.add, [bk[a], rzk, "imp%d" % qt], ["imp%d" % qt])

                        jobs.append((sf0, pf0, None))
                        jobs.append((sf1, pf1, None))
                    run_jobs(jobs)
                    if LIM == 405:
                        raise _Stop()
                    for qt in range(4):
                        tt("dve", imp[qt], imp[qt], cs_t[qt], ALU.add, ["imp%d" % qt, "cs%d" % qt], ["imp%d" % qt])
                        P.op("dve", lambda e, qt=qt: e.max(out=m8[:, 0:8], in_=imp[qt]), reads=["imp%d" % qt], writes=["m8"])
                        P.op("dve", lambda e, qt=qt: e.match_replace(out=wk64, in_to_replace=m8[:, 0:8], in_values=imp[qt], imm_value=-3e38),
                             reads=["imp%d" % qt, "m8"], writes=["wk64"])
                        P.op("dve", lambda e: e.max(out=m8[:, 8:16], in_=wk64), reads=["wk64", "m8"], writes=["m8"])
                        ts("dve", mb[:, 64:128], imp[qt], m8[:, 15:16], None, ALU.is_ge, None, ["imp%d" % qt, "m8", "mb"], ["mb"])
                        ts("dve", mb[:, 64:128], mb[:, 64:128], -1.0, -NEG, ALU.add, ALU.mult, ["mb"], ["mb"])
                        if LIM == 406:
                            raise _Stop()
                        tr(banks[6][:, 0:128], mb, identf, ["mb", "identf"], [bk[6]])
                        if LIM == 407:
                            raise _Stop()
                        mbi[0] += 1
                        cp("act", mbT[mbi[0] % 2], banks[6][:, 0:128], [bk[6]], ["mbT%d" % (mbi[0] % 2)])
                        for hg in range(4):
                            h = g * 4 + hg
                            dma(QA[h][64:128, qt * 128:(qt + 1) * 128], mbT[mbi[0] % 2][64:128, :], r=["mbT%d" % (mbi[0] % 2), "QA%d" % h], w=["QA%d" % h])
                        if LIM == 409:
                            raise _Stop()
                    if LIM == 408:
                        raise _Stop()

                if LIM == 41:
                    raise _Stop()
                jobs = []
                for h in range(8):
                    g = h // 4
                    for br in (1, 2):
                        if br == 1:
                            kts = list(range(0, kt_hi + 1))
                        else:
                            kts = [kt for kt in range(q0 // 128 - 4, kt_hi + 1) if kt >= 0]
                        for kt in kts:
                            d8 = kt - (q0 // 128 - 4)

                            def sf(h=h, g=g, br=br, kt=kt, d8=d8):
                                if br == 1:
                                    mk = MSK[:, d8, :] if d8 >= 4 else None
                                    return score_tile(KSA[g][:, kt * 128:(kt + 1) * 128], QA[h], ["KSA%d" % g, "QA%d" % h], mk)
                                return score_tile(KW[g][:, kt * 128:(kt + 1) * 128], QA[h], ["KW%d" % g, "QA%d" % h], MSK[:, d8, :])

                            def pf(Ee, h=h, g=g, br=br, kt=kt):
                                Ea, Ek = Ee
                                Vt, vk = (VS, "VS") if br == 1 else (VW, "VW")
                                for qt in range(4):
                                    qti = q0 // 128 + qt
                                    if kt > qti or (br == 2 and kt < qti - 4):
                                        continue
                                    first = 0 if br == 1 else max(0, qti - 4)
                                    a = ACC[qt]
                                    mm(banks[a][:, 0:65], Ea[:, qt * 128:(qt + 1) * 128], Vt[:, kt, g, 0:65], kt == first, kt == qti, [Ek, vk], [bk[a]])

                            def ef(h=h, br=br):
                                for qt in range(4):
                                    a = ACC[qt]
                                    rz, rzk = smalloc()
                                    recip(rz, banks[a][:, 64:65], [bk[a]], [rzk])
                                    tt("dve", rz, rz, gn_t[qt][:, h * 3 + br:h * 3 + br + 1], ALU.mult, [rzk, "gn%d" % qt], [rzk])
                                    ycs = ycomb[qt][:, h * 64:(h + 1) * 64]
                                    yk = "yc%d_%d" % (qt, h)
                                    stt("dve", ycs, banks[a][:, 0:64], rz, ycs, ALU.mult, ALU.add, [bk[a], rzk, yk], [yk])

                            jobs.append((sf, pf, ef if kt == kts[-1] else None))
                run_jobs(jobs)

                if LIM == 42:
                    raise _Stop()
                for qt in range(4):
                    t0 = q0 + qt * 128
                    ykeys = ["yc%d_%d" % (qt, h) for h in range(8)]
                    dma(ydt, YDA[t0:t0 + 128, :], r=["YDA"], w=["ydt"])
                    if dbg:
                        dma(dbg_out["d_yy"][t0:t0 + 128, 0:512], ydt, r=["ydt"], w=["d_yy"])
                        dma(dbg_out["d_yy"][t0:t0 + 128, 512:1024], ycomb[qt], r=ykeys, w=["d_yy"])
                    cp("dve", ybf[:, 0:512], ydt, ["ydt", "ybf"], ["ybf"])
                    cp("act", ybf[:, 512:1024], ycomb[qt], ykeys + ["ybf"], ["ybf"])
                    pv = banks[6][:, :].bitcast(BF16)
                    for k in range(8):
                        tr(pv[:, k * 128:(k + 1) * 128], ybf[:, k * 128:(k + 1) * 128], ident, ["ybf", "ident"], [bk[6]])
                    cp("act", yT, pv.rearrange("p (k n) -> p k n", k=8), [bk[6]], ["yT"])
                    dma(hs, x[t0:t0 + 128, :], w=["hs"])
                    for half in range(2):
                        cs = slice(half * 512, (half + 1) * 512)
                        dma(gmt[:, 0:512], GM[t0:t0 + 128, half * 512:(half + 1) * 512], r=["GM", "gmt"], w=["gmt"])
                        dma(gmt[:, 512:1024], GM[t0:t0 + 128, 1024 + half * 512:1024 + (half + 1) * 512], r=["GM", "gmt"], w=["gmt"])
                        for k in range(4):
                            mm(banks[0][:, :], yT[:, k, :], PDA[:, k, cs], k == 0, k == 3, ["yT", "PDA"], [bk[0]])
                        for k in range(4):
                            mm(banks[1][:, :], yT[:, 4 + k, :], PNS[:, k, cs], k == 0, k == 3, ["yT", "PNS"], [bk[1]])
                        tt("dve", m1, banks[0][:, :], gmt[:, 0:512], ALU.mult, [bk[0], "gmt"], ["m1"])
                        tt("dve", m2, banks[1][:, :], gmt[:, 512:1024], ALU.mult, [bk[1], "gmt"], ["m2"])
                        tt("dve", mgb[:, cs], m1, m2, ALU.add, ["m1", "m2", "mgb"], ["mgb"])
                    pv7 = banks[7][:, :].bitcast(BF16)
                    for k in range(8):
                        tr(pv7[:, k * 128:(k + 1) * 128], mgb[:, k * 128:(k + 1) * 128], ident, ["mgb", "ident"], [bk[7]])
                    cp("act", mT, pv7.rearrange("p (k n) -> p k n", k=8), [bk[7]], ["mT"])
                    for half in range(2):
                        cs = slice(half * 512, (half + 1) * 512)
                        for k in range(8):
                            mm(banks[half][:, :], mT[:, k, :], WO[:, k, cs], k == 0, k == 7, ["mT", "WO"], [bk[half]])
                        tt("dve", hs[:, cs], banks[half][:, :], hs[:, cs], ALU.add, [bk[half], "hs"], ["hs"])
                    dma(H[t0:t0 + 128, :], hs, r=["hs"], w=["H"])
                    if dbg:
                        dma(dbg_out["d_h"][t0:t0 + 128, :], hs, r=["hs"], w=["d_h"])
                    rms_to_T(hs, "hs", gF, "gF", cTo, "cTo", 0, 6)
                    dma(CT[:, :, t0:t0 + 128], cTo, r=["cTo"], w=["CT"])
            P.barrier()
            if LIM == 5:
                raise _Stop()
            off[0] = PERSIST

            WQ = al(8 * 2048).rearrange("p (k n) -> p k n", k=8)
            load_w(WQ, "WQ", wq, 2048)
            K1 = al(128)
            K2 = al(128)
            for dst, src, kk in ((K1, k1T, "K1"), (K2, k2T, "K2")):
                sv, sk, e_ = load_cast(None, src, 128, [128])
                cp(e_, dst, sv, [sk], [kk])
            KK = [K1, K2]
            V16K = ["v16a_%d" % u for u in range(16)] + ["v16b_%d" % u for u in range(16)]
            C16K = ["c16a_%d" % u for u in range(8)] + ["c16b_%d" % u for u in range(8)]
            P.shared = set(["WQ", "K1", "K2", "ident", "identf", "CT", "QTs", "SVs"])
            pp = []
            for par in range(2):
                d_ = {}
                d_["cT"] = al(8 * TB).rearrange("p (k n) -> p k n", k=8)
                d_["qT"] = al(16 * TB).rearrange("p (u n) -> p u n", u=16)
                d_["sc"] = [al(2048, F32).rearrange("p (u n) -> p u n", u=16) for _ in range(2)]
                d_["wk"] = [al(128, F32) for _ in range(16)]
                d_["v16"] = al(256, F32).rearrange("p (h s a) -> p h s a", h=8, s=2)
                d_["cand"] = al(2048, F32).rearrange("p (h a b) -> p h a b", h=8, a=16)
                d_["wk2"] = [al(256, F32) for _ in range(8)]
                d_["c16"] = al(128, F32).rearrange("p (h a) -> p h a", h=8)
                d_["ex16"] = al(128, F32).rearrange("p (h a) -> p h a", h=8)
                d_["z8"] = al(8, F32)
                d_["ml8"] = al(8, F32)
                d_["A1"] = al(128, F32)
                d_["A2"] = al(128, F32)
                d_["A3"] = al(128, F32)
                d_["v1T"] = al(TB, F32)
                d_["t2T"] = al(TB, F32)
                d_["bT"] = al(TB, F32)
                pp.append(d_)
            def pp_p1(b):
                tb0 = b * TB
                P.suffix = "_p%d" % (b % 2)
                d_ = pp[b % 2]
                cT, qT = d_["cT"], d_["qT"]
                dma(cT, CT[:, :, tb0:tb0 + TB], r=["CT"], w=["cT"])
                for u in range(16):
                    pb = 4 + (u % 4)
                    for kc in range(8):
                        mm(banks[pb][:, 0:TB], WQ[:, kc, u * 128:(u + 1) * 128], cT[:, kc, :], kc == 0, kc == 7, ["WQ", "cT"], [bk[pb]])
                    cp("act", qT[:, u, :], banks[pb][:, 0:TB], [bk[pb], "qT"], ["qT"])
                for t2 in range(TB // 128):
                    tl = slice(t2 * 128, (t2 + 1) * 128)
                    sc = d_["sc"][t2]
                    sck = "sc%d" % t2
                    for u in range(16):
                        pb = u // 4
                        mm(banks[pb][:, (u % 4) * 128:(u % 4 + 1) * 128], qT[:, u, tl], KK[u % 2], True, True, ["qT", "K1", "K2"], [bk[pb]])
                    for pb in range(4):
                        cp("act", sc[:, pb * 4:(pb + 1) * 4, :], banks[pb][:, :].rearrange("p (u n) -> p u n", u=4), [bk[pb], sck], [sck])

            def pp_p2(b):
                P.suffix = "_p%d" % (b % 2)
                d_ = pp[b % 2]
                cT, qT, wk, v16, cand, wk2, c16, ex16, z8, ml8, A1, A2, A3, v1T, t2T, bT = (
                    d_["cT"], d_["qT"], d_["wk"], d_["v16"], d_["cand"], d_["wk2"], d_["c16"], d_["ex16"], d_["z8"], d_["ml8"],
                    d_["A1"], d_["A2"], d_["A3"], d_["v1T"], d_["t2T"], d_["bT"])
                for t2 in range(TB // 128):
                    tl = slice(t2 * 128, (t2 + 1) * 128)
                    sc = d_["sc"][t2]
                    sck = "sc%d" % t2
                    for u0 in range(0, 16, 16):
                        for u in range(u0, u0 + 16):
                            hh, ss_ = u // 2, u % 2
                            P.op("dve", lambda e, u=u, hh=hh, ss_=ss_, v16=v16, sc=sc, wk=wk, c16=c16, wk2=wk2, z8=z8, ex16=ex16: e.max(out=v16[:, hh, ss_, 0:8], in_=sc[:, u, :]), reads=[sck], writes=["v16a_%d" % u])
                        for u in range(u0, u0 + 16):
                            hh, ss_ = u // 2, u % 2
                            P.op("dve", lambda e, u=u, hh=hh, ss_=ss_, v16=v16, sc=sc, wk=wk, c16=c16, wk2=wk2, z8=z8, ex16=ex16: e.match_replace(out=wk[u % 16], in_to_replace=v16[:, hh, ss_, 0:8], in_values=sc[:, u, :], imm_value=-3e38),
                                 reads=[sck, "v16a_%d" % u], writes=["wk%d" % (u % 16)])
                        for u in range(u0, u0 + 16):
                            hh, ss_ = u // 2, u % 2
                            P.op("dve", lambda e, u=u, hh=hh, ss_=ss_, v16=v16, sc=sc, wk=wk, c16=c16, wk2=wk2, z8=z8, ex16=ex16: e.max(out=v16[:, hh, ss_, 8:16], in_=wk[u % 16]), reads=["wk%d" % (u % 16)], writes=["v16b_%d" % u])
                    tt("dve", cand, v16[:, :, 0, :].unsqueeze(3).to_broadcast([128, 8, 16, 16]), v16[:, :, 1, :].unsqueeze(2).to_broadcast([128, 8, 16, 16]),
                       ALU.add, V16K, ["cand"])
                    for h0 in range(0, 8, 8):
                        for hh in range(h0, h0 + 8):
                            cv = cand[:, hh, :, :].rearrange("p a b -> p (a b)")
                            P.op("dve", lambda e, hh=hh, cv=cv, v16=v16, sc=sc, wk=wk, c16=c16, wk2=wk2, z8=z8, ex16=ex16: e.max(out=c16[:, hh, 0:8], in_=cv), reads=["cand"], writes=["c16a_%d" % hh])
                        for hh in range(h0, h0 + 8):
                            cv = cand[:, hh, :, :].rearrange("p a b -> p (a b)")
                            P.op("dve", lambda e, hh=hh, cv=cv, v16=v16, sc=sc, wk=wk, c16=c16, wk2=wk2, z8=z8, ex16=ex16: e.match_replace(out=wk2[hh % 8], in_to_replace=c16[:, hh, 0:8], in_values=cv, imm_value=-3e38),
                                 reads=["cand", "c16a_%d" % hh], writes=["wk2_%d" % (hh % 8)])
                        for hh in range(h0, h0 + 8):
                            P.op("dve", lambda e, hh=hh, v16=v16, sc=sc, wk=wk, c16=c16, wk2=wk2, z8=z8, ex16=ex16: e.max(out=c16[:, hh, 8:16], in_=wk2[hh % 8]), reads=["wk2_%d" % (hh % 8)], writes=["c16b_%d" % hh])
                    tt("dve", ex16, c16, c16[:, :, 0:1].to_broadcast([128, 8, 16]), ALU.subtract, C16K, ["ex16"])
                    act(ex16, ex16, AF.Exp, ["ex16"], ["ex16"])
                    P.op("dve", lambda e, v16=v16, sc=sc, wk=wk, c16=c16, wk2=wk2, z8=z8, ex16=ex16: e.reduce_sum(out=z8, in_=ex16, axis=AX.X), reads=["ex16"], writes=["z8"])
                    act(ml8, z8, AF.Ln, ["z8"], ["ml8"])
                    tt("dve", ml8, ml8, c16[:, :, 0], ALU.add, ["ml8"] + C16K, ["ml8"])
                    cp("dve", A1.rearrange("p (h a) -> p h a", h=8), v16[:, :, 0, :], V16K, ["A1"])
                    cp("dve", A2.rearrange("p (h a) -> p h a", h=8), c16[:, :, 15:16].to_broadcast([128, 8, 16]), C16K, ["A2"])
                    tt("dve", A3.rearrange("p (h a) -> p h a", h=8), v16[:, :, 0, :], ml8.unsqueeze(2).to_broadcast([128, 8, 16]), ALU.subtract, V16K + ["ml8"], ["A3"])
                    for Ax, ak, Tx, kx, px in ((A1, "A1", v1T, "v1T", 4), (A2, "A2", t2T, "t2T", 5), (A3, "A3", bT, "bT", 6)):
                        tr(banks[px][:, 0:128], Ax, identf, [ak, "identf"], [bk[px]])
                        cp("act", Tx[:, tl], banks[px][:, 0:128], [bk[px], kx], [kx])
                dma(QTs[b], qT.rearrange("p u n -> p (u n)"), r=["qT"], w=["QTs"])
                dma(SVs[b, 0], v1T, r=["v1T"], w=["SVs"])
                dma(SVs[b, 1], t2T, r=["t2T"], w=["SVs"])
                dma(SVs[b, 2], bT, r=["bT"], w=["SVs"])

            pp_p1(0)
            for b in range(NB):
                if b + 1 < NB:
                    pp_p1(b + 1)
                pp_p2(b)
            P.suffix = None
            P.barrier()
            if LIM == 6:
                raise _Stop()
            off[0] = PERSIST
            K1 = al(128)
            K2 = al(128)
            for dst, src, kk in ((K1, k1T, "K1"), (K2, k2T, "K2")):
                sv, sk, e_ = load_cast(None, src, 128, [128])
                cp(e_, dst, sv, [sk], [kk])
            gFin = al(1024, F32)
            dma(gFin, final_norm.partition_broadcast(128), w=["gFin"])
            cT = al(8 * TB).rearrange("p (k n) -> p k n", k=8)
            qT = al(16 * TB).rearrange("p (u n) -> p u n", u=16)
            v1T = al(TB, F32)
            t2T = al(TB, F32)
            bT = al(TB, F32)
            Gb = al(TB * 128).rearrange("p (t j) -> p t j", t=TB)
            hfin = [al(1024, F32) for _ in range(1)]
            junk = al(1024)
            ssq = [al(1, F32)]
            dbgb = al(1024, F32) if dbg else None
            GRP = 8
            Qrep = [[al(GRP * 128) for _ in range(2)] for _ in range(2)]
            Ma4 = [al(512) for _ in range(4)]
            Rr4 = [al(512) for _ in range(4)]
            Dd4 = [al(512, F32) for _ in range(3)]
            Da4 = [al(512, F32) for _ in range(3)]
            Mk4 = [al(512) for _ in range(3)]
            Ex4 = [al(512) for _ in range(3)]
            UTs = [al(1024).rearrange("p (k i) -> p k i", k=8) for _ in range(4)]
            VBs = [al(1024) for _ in range(4)]
            Hg = [al(TB, F32) for _ in range(3)]
            GH = [al(TB) for _ in range(3)]

            for b in range(NB):
                tb0 = b * TB
                dma(cT, CT[:, :, tb0:tb0 + TB], r=["CT"], w=["cT"])
                dma(qT.rearrange("p u n -> p (u n)"), QTs[b], r=["QTs"], w=["qT"])
                dma(v1T, SVs[b, 0], r=["SVs"], w=["v1T"])
                dma(t2T, SVs[b, 1], r=["SVs"], w=["t2T"])
                dma(bT, SVs[b, 2], r=["SVs"], w=["bT"])
                if LIM == 51:
                    raise _Stop()
                R1B = [0, 1, 6]
                R2B = [2, 3, 7]
                NBT = TB // 4

                def rep(b_):
                    t0_ = b_ * 4
                    gi = t0_ // GRP
                    qb = gi % 2
                    if t0_ % GRP == 0:
                        for side in range(2):
                            src = qT[:, side:16:2, gi * GRP:(gi + 1) * GRP].rearrange("p h t -> p t h").unsqueeze(3).to_broadcast([128, GRP, 8, 16])
                            dstq = Qrep[qb][side].rearrange("p (t h a) -> p t h a", t=GRP, h=8)
                            act(dstq, src, AF.Copy, ["qT"], ["Qrep%d%d" % (qb, side)])
                    r1, r2 = R1B[b_ % 3], R2B[b_ % 3]
                    for ti in range(4):
                        tq = (t0_ % GRP) + ti
                        mm(banks[r1][:, ti * 128:(ti + 1) * 128], Qrep[qb][0][:, tq * 128:(tq + 1) * 128], K1, True, True, ["Qrep%d0" % qb, "K1"], [bk[r1]])
                        mm(banks[r2][:, ti * 128:(ti + 1) * 128], Qrep[qb][1][:, tq * 128:(tq + 1) * 128], K2, True, True, ["Qrep%d1" % qb, "K2"], [bk[r2]])

                def mid(b_):
                    t0_ = b_ * 4
                    r1, r2 = R1B[b_ % 3], R2B[b_ % 3]
                    i4, i3 = b_ % 4, b_ % 3
                    v3 = lambda a_: a_.rearrange("p (t n) -> p t n", t=4)
                    bc = lambda a_: a_[:, t0_:t0_ + 4].unsqueeze(2).to_broadcast([128, 4, 128])
                    tt("dve", v3(Da4[i3]), v3(banks[r2][:, :]), bc(bT), ALU.add, [bk[r2], "bT"], ["Da4_%d" % i3])
                    tt("dve", v3(Dd4[i3]), v3(banks[r2][:, :]), bc(v1T), ALU.add, [bk[r2], "v1T"], ["Dd4_%d" % i3])
                    act(Ex4[i3], Da4[i3], AF.Exp, ["Da4_%d" % i3], ["Ex4_%d" % i3])
                    tt("dve", v3(Ma4[i4]), v3(banks[r1][:, :]), bc(v1T), ALU.is_equal, [bk[r1], "v1T"], ["Ma4_%d" % i4])
                    tt("dve", v3(Mk4[i3]), v3(Dd4[i3]), bc(t2T), ALU.is_ge, ["Dd4_%d" % i3, "t2T"], ["Mk4_%d" % i3])
                    tt("pool", Rr4[i4], Mk4[i3], Ex4[i3], ALU.mult, ["Mk4_%d" % i3, "Ex4_%d" % i3], ["Rr4_%d" % i4])

                def gmm(b_):
                    t0_ = b_ * 4
                    i4 = b_ % 4
                    pg = 4 + b_ % 2
                    for ti in range(4):
                        mm(banks[pg][:, ti * 128:(ti + 1) * 128], Ma4[i4][:, ti * 128:(ti + 1) * 128], Rr4[i4][:, ti * 128:(ti + 1) * 128], True, True,
                           ["Ma4_%d" % i4, "Rr4_%d" % i4], [bk[pg]])
                    cp("act", Gb[:, t0_:t0_ + 4, :], banks[pg][:, :].rearrange("p (t j) -> p t j", t=4), [bk[pg], "Gb"], ["Gb"])

                rep(0)
                rep(1)
                rep(2)
                GDEL = 2
                for b_ in range(NBT):
                    mid(b_)
                    if b_ + 3 < NBT:
                        rep(b_ + 3)
                    if b_ - GDEL >= 0:
                        gmm(b_ - GDEL)
                for b_ in range(max(0, NBT - GDEL), NBT):
                    gmm(b_)
                if LIM == 52:
                    raise _Stop()

                def loads(j):
                    ub = j % 4
                    dma(UTs[ub], UT[j].rearrange("p (k i) -> p k i", k=8), r=["UT"], w=["UTs%d" % ub], q="sp")
                    dma(VBs[ub], VB[j], r=["VB"], w=["VBs%d" % ub], q="sp")

                def umm(j):
                    ub = j % 4
                    ph = 4 + (j % 3)
                    for kc in range(8):
                        mm(banks[ph][:, 0:TB], UTs[ub][:, kc, :], cT[:, kc, :], kc == 0, kc == 7, ["UTs%d" % ub, "cT"], [bk[ph]])

                def rest(j):
                    ub = j % 4
                    ph = 4 + (j % 3)
                    hb_ = j % 3
                    act(Hg[hb_], banks[ph][:, 0:TB], AF.Gelu, [bk[ph]], ["Hg%d" % hb_])
                    gb_ = j % 3
                    tt("dve", GH[gb_], Hg[hb_], Gb[:, :, j], ALU.mult, ["Hg%d" % hb_, "Gb"], ["GH%d" % gb_])
                    for t2 in range(TB // 128):
                        for half in range(2):
                            pb = t2 * 2 + half
                            mm(banks[pb][:, :], GH[gb_][:, t2 * 128:(t2 + 1) * 128], VBs[ub][:, half * 512:(half + 1) * 512], j == 0, j == 127,
                               ["GH%d" % gb_, "VBs%d" % ub], [bk[pb]])

                loads(0)
                loads(1)
                loads(2)
                umm(0)
                umm(1)
                for j in range(128):
                    if j + 3 < 128:
                        loads(j + 3)
                    if j + 2 < 128:
                        umm(j + 2)
                    rest(j)
                if LIM == 53:
                    raise _Stop()
                for t2 in range(TB // 128):
                    t0 = tb0 + t2 * 128
                    hf = hfin[0]
                    hk = "hfin0"
                    dma(hf, H[t0:t0 + 128, :], r=["H"], w=[hk])
                    if dbg:
                        for half in range(2):
                            cp("dve", dbgb[:, half * 512:(half + 1) * 512], banks[t2 * 2 + half][:, :], [bk[t2 * 2 + half], "dbgb"], ["dbgb"])
                        dma(dbg_out["d_pe"][t0:t0 + 128, :], dbgb, r=["dbgb"], w=["d_pe"])
                    for half in range(2):
                        cs = slice(half * 512, (half + 1) * 512)
                        tt("dve", hf[:, cs], banks[t2 * 2 + half][:, :], hf[:, cs], ALU.add, [bk[t2 * 2 + half], hk], [hk])
                    act(junk, hf, AF.Square, [hk], ["junk", "ssq0"], scale=1.0 / 32.0, accum=ssq[0])
                    act(ssq[0], ssq[0], AF.Sqrt, ["ssq0"], ["ssq0"], bias=1e-6, scale=1.0)
                    recip(ssq[0], ssq[0], ["ssq0"], ["ssq0"])
                    stt("dve", hf, hf, ssq[0], gFin, ALU.mult, ALU.mult, [hk, "ssq0", "gFin"], [hk])
                    fin_ops.append(dma(y[t0:t0 + 128, :], hf, r=[hk], w=["y"], q="sp"))
        except _Stop:
            pass
        P.emit(es, fin_ops)
    return nc


fin_ops = []


def host_consts(S):
    bf = ml_dtypes.bfloat16
    pos = np.arange(S, dtype=np.float32)
    inv = np.power(np.float32(500000.0), -np.arange(8, dtype=np.float32) * 2.0 / 16.0).astype(np.float32)
    ang = pos[None, :] * inv[:, None]
    cosv, sinv = np.cos(ang).astype(np.float32), np.sin(ang).astype(np.float32)
    ropec = np.ones((128, S), np.float32)
    ropes = np.zeros((128, S), np.float32)
    for hb in (0, 64):
        ropec[hb:hb + 8] = cosv
        ropec[hb + 8:hb + 16] = cosv
        ropes[hb:hb + 8] = -sinv
        ropes[hb + 8:hb + 16] = sinv
    p = np.arange(128)[:, None]
    f = np.arange(512)[None, :]
    masks = np.zeros((8, 128, 512), np.float32)
    for d in range(4):
        masks[d] = np.where(128 * d + p > f, 0.0, NEG)
        masks[4 + d] = np.where(128 * d + p <= f, 0.0, NEG)
    n = np.arange(256)[:, None]
    t = np.arange(S)[None, :]
    cmaskT = np.where((16 * n + 31 <= t) & (n < (S - 32) // 16 + 1), 0.0, NEG)
    esel = (np.arange(64)[:, None] == (np.arange(S)[None, :] // 64)).astype(np.float32)
    tq = np.arange(S)[:, None]
    j = np.arange(64)[None, :]
    cur = tq // 64
    forced = (j == 0) | (j == cur) | (j == cur - 1)
    valid = (j <= cur) & (j < S // 64)
    csel = np.where(valid, np.where(forced, 1e4, 0.0), -1e30).astype(np.float32)
    ci = np.arange(256)[:, None] * 16
    sj = np.arange(64)[None, :] * 64
    ovl = ((ci < sj + 64) & (ci + 32 > sj)).astype(np.float32)
    ovl[(S - 32) // 16 + 1:] = 0.0
    return dict(ropec=ropec, ropes=ropes, masks=masks.astype(bf), cmaskT=cmaskT.astype(bf), esel=esel.astype(bf),
                csel=csel, ovl=ovl.astype(bf))


def host_weights(inp):
    f = lambda a: np.ascontiguousarray(np.asarray(a, dtype=np.float32))
    w_in = f(inp["w_in"])[0]
    roped_cols = np.concatenate([np.arange(c0, c0 + 128) for c0 in FM_COLS[:N_ROPED]])
    perm = np.arange(128)
    for hb in (0, 64):
        perm[hb:hb + 8] = np.arange(hb + 8, hb + 16)
        perm[hb + 8:hb + 16] = np.arange(hb, hb + 8)
    rot_cols = np.concatenate([roped_cols[i * 128:(i + 1) * 128][perm] for i in range(N_ROPED)])
    w = dict(
        attn_norm=f(inp["attn_norm"])[0], w_in=w_in, w_rot=np.ascontiguousarray(w_in[:, rot_cols]),
        lam4=np.stack([f(inp["da_lambda_q1"])[0], f(inp["da_lambda_k1"])[0], f(inp["da_lambda_q2"])[0], f(inp["da_lambda_k2"])[0]]),
        subln=f(inp["da_subln"])[0],
        peT_k=np.ascontiguousarray(f(inp["cmp_pe_k"])[0].T), peT_v=np.ascontiguousarray(f(inp["cmp_pe_v"])[0].T),
        w1k=f(inp["cmp_w1_k"])[0], w1v=f(inp["cmp_w1_v"])[0], w2k=f(inp["cmp_w2_k"])[0], w2v=f(inp["cmp_w2_v"])[0],
        p_da=f(inp["p_da"])[0], p_nsa=f(inp["p_nsa"])[0], w_o=f(inp["w_o"])[0], ffn_norm=f(inp["ffn_norm"])[0],
        wq=f(inp["peer_wq"])[0], k1T=np.ascontiguousarray(f(inp["peer_k1"])[0].T), k2T=np.ascontiguousarray(f(inp["peer_k2"])[0].T),
        peer_u=f(inp["peer_u"])[0], peer_v=f(inp["peer_v"])[0], final_norm=f(inp["final_norm"]),
    )
    return w


def run(inputs, S, dbg=False, cores=None, LIM=99):
    del fin_ops[:]
    xin = np.asarray(inputs["x"], dtype=np.float32)
    B = xin.shape[0]
    nc = build(S, dbg, LIM)
    shared = host_weights(inputs)
    shared.update(host_consts(S))
    in_maps = []
    for b in range(B):
        m = dict(shared)
        m["x"] = np.ascontiguousarray(xin[b])
        in_maps.append(m)
    res = run_bass_kernel_spmd(nc, in_maps, core_ids=list(range(B)))
    return res


def kernel(**inputs):
    S = inputs["x"].shape[1]
    res = run(inputs, S)
    return np.stack([np.asarray(r["y"], dtype=np.float32) for r in res.results], axis=0)
```
